# Optimizing a Trainium2 kernel written in Bass

```python
import jax, jax.numpy as jnp
from jax import lax
import numpy as np

D_MODEL = 1024
BATCH = 8
SEQ = 4096
DEPTH = 1

CONV_CH = D_MODEL
CONV_K = 3
GMLP_CH = D_MODEL
GMLP_GROUPS = 8
GMLP_GROUP_CH = GMLP_CH // GMLP_GROUPS
CHUNK = 128
MEM_LEN = 256
XA_HEADS = 4
XA_HEAD_DIM = D_MODEL // XA_HEADS
XA_WIDTH = XA_HEADS * XA_HEAD_DIM
N_BRANCH = 3
W_IN_SIZES = (CONV_CH, CONV_CH, CONV_CH, GMLP_CH, GMLP_CH, XA_WIDTH, N_BRANCH * D_MODEL)
W_IN_COLS = sum(W_IN_SIZES)
W_IN_SPLITS = tuple(int(s) for s in np.cumsum(W_IN_SIZES)[:-1])
N_EXPERTS = 32
TOP_K = 4
D_FF = D_MODEL
SWIGLU_LIMIT = 7.0
SWIGLU_ALPHA = 1.702
LN_EPS = 1e-5
DEEPNORM_ALPHA = (2 * DEPTH) ** 0.25
DEEPNORM_BETA = (8 * DEPTH) ** -0.25

kernel_name = "hybrid_conv_gmlp_xattn_moe_deepnorm"


def layer_norm(x, g, b):
    xf = x.astype(jnp.float32)
    mu = jnp.mean(xf, axis=-1, keepdims=True)
    xc = xf - mu
    var = jnp.mean(xc * xc, axis=-1, keepdims=True)
    y = xc * lax.rsqrt(var + LN_EPS) * g.astype(jnp.float32) + b.astype(jnp.float32)
    return y.astype(x.dtype)


def causal_depthwise_conv(u, w):
    c = u.shape[-1]
    return lax.conv_general_dilated(
        u, w[:, None, :].astype(u.dtype), window_strides=(1,),
        padding=((CONV_K - 1, 0),), dimension_numbers=("NWC", "WIO", "NWC"),
        feature_group_count=c)


def gmlp_spatial_gate(u, v, ws, bias, ln_g, ln_b):
    bsz, seq, _ = v.shape
    v = layer_norm(v, ln_g, ln_b)
    v = v.reshape(bsz, seq // CHUNK, CHUNK, GMLP_GROUPS, GMLP_GROUP_CH)
    ws_causal = jnp.tril(ws)
    f = jnp.einsum("gts,bnsgc->bntgc", ws_causal.astype(v.dtype), v)
    f = f + bias.T[None, None, :, :, None].astype(v.dtype)
    return u * f.reshape(bsz, seq, GMLP_CH)


def memory_cross_attention(q, mem_n, w_kv):
    bsz, seq, _ = q.shape
    k, v = jnp.split(mem_n @ w_kv, 2, axis=-1)
    q = q.reshape(bsz, seq, XA_HEADS, XA_HEAD_DIM)
    k = k.reshape(bsz, MEM_LEN, XA_HEADS, XA_HEAD_DIM)
    v = v.reshape(bsz, MEM_LEN, XA_HEADS, XA_HEAD_DIM)
    scores = jnp.einsum("bshd,bmhd->bhsm", q, k).astype(jnp.float32) * (XA_HEAD_DIM ** -0.5)
    probs = jax.nn.softmax(scores, axis=-1).astype(v.dtype)
    o = jnp.einsum("bhsm,bmhd->bshd", probs, v)
    return o.reshape(bsz, seq, XA_WIDTH)


def token_mix(h, mem_n, w_in, b_gate, conv_w, gmlp_ws, gmlp_b, gmlp_ln_g, gmlp_ln_b,
              w_kv, w_conv_proj, w_gmlp_proj, w_xa_proj, w_out):
    z = h @ w_in
    cb, cc, ch, gu, gv, q, gates = jnp.split(z, W_IN_SPLITS, axis=-1)
    y_conv = cb * causal_depthwise_conv(cc * ch, conv_w)
    y_gmlp = gmlp_spatial_gate(jax.nn.gelu(gu), jax.nn.gelu(gv), gmlp_ws, gmlp_b, gmlp_ln_g, gmlp_ln_b)
    y_xa = memory_cross_attention(q, mem_n, w_kv)
    g_conv, g_gmlp, g_xa = jnp.split(jax.nn.sigmoid(gates + b_gate), N_BRANCH, axis=-1)
    merged = (g_conv * (y_conv @ w_conv_proj)
              + g_gmlp * (y_gmlp @ w_gmlp_proj)
              + g_xa * (y_xa @ w_xa_proj))
    return merged @ w_out


def moe_ffn(h, w_router, b_router, w_gate_up, b_gate_up, w_down, b_down):
    bsz, seq, d = h.shape
    n_tok = bsz * seq
    hf = h.reshape(n_tok, d)
    logits = (hf @ w_router + b_router).astype(jnp.float32)
    top_vals, top_idx = lax.top_k(logits, TOP_K)
    gate = jax.nn.softmax(top_vals, axis=-1).astype(h.dtype)
    expert_flat = top_idx.reshape(-1)
    order = jnp.argsort(expert_flat)
    sorted_expert = expert_flat[order]
    sorted_token = order // TOP_K
    group_sizes = jnp.bincount(expert_flat, length=N_EXPERTS).astype(jnp.int32)
    xs = hf[sorted_token]
    gu = lax.ragged_dot(xs, w_gate_up, group_sizes) + b_gate_up[sorted_expert]
    g_lin, u_lin = jnp.split(gu, 2, axis=-1)
    g_lin = jnp.minimum(g_lin, SWIGLU_LIMIT)
    u_lin = jnp.clip(u_lin, -SWIGLU_LIMIT, SWIGLU_LIMIT)
    act = (u_lin + 1) * (g_lin * jax.nn.sigmoid(SWIGLU_ALPHA * g_lin))
    eo = lax.ragged_dot(act, w_down, group_sizes) + b_down[sorted_expert]
    eo = eo * gate.reshape(-1)[order][:, None]
    y = jnp.zeros_like(hf).at[sorted_token].add(eo)
    return y.reshape(bsz, seq, d)


def setup_inputs(seed: int = 0) -> dict:
    key = jax.random.key(seed)
    ks = jax.random.split(key, 28)
    L = DEPTH
    beta = DEEPNORM_BETA

    def nrm(k, shape, scale):
        return jax.random.normal(k, shape, jnp.float32) * scale

    w_k = nrm(ks[11], (L, D_MODEL, XA_WIDTH), D_MODEL ** -0.5)
    w_v = nrm(ks[12], (L, D_MODEL, XA_WIDTH), D_MODEL ** -0.5 * beta)
    return {
        "x": nrm(ks[0], (BATCH, SEQ, D_MODEL), 1.0),
        "mem": nrm(ks[1], (BATCH, MEM_LEN, D_MODEL), 1.0),
        "w_in": nrm(ks[2], (L, D_MODEL, W_IN_COLS), D_MODEL ** -0.5),
        "b_gate": nrm(ks[3], (L, N_BRANCH * D_MODEL), 0.01),
        "conv_w": nrm(ks[4], (L, CONV_K, CONV_CH), CONV_K ** -0.5),
        "gmlp_ws": nrm(ks[5], (L, GMLP_GROUPS, CHUNK, CHUNK), CHUNK ** -0.5),
        "gmlp_b": 1.0 + nrm(ks[6], (L, GMLP_GROUPS, CHUNK), 0.01),
        "gmlp_ln_g": 1.0 + nrm(ks[7], (L, GMLP_CH), 0.01),
        "gmlp_ln_b": nrm(ks[8], (L, GMLP_CH), 0.01),
        "mem_ln_g": 1.0 + nrm(ks[9], (L, D_MODEL), 0.01),
        "mem_ln_b": nrm(ks[10], (L, D_MODEL), 0.01),
        "w_kv": jnp.concatenate([w_k, w_v], axis=-1),
        "w_conv_proj": nrm(ks[13], (L, CONV_CH, D_MODEL), CONV_CH ** -0.5 * beta),
        "w_gmlp_proj": nrm(ks[14], (L, GMLP_CH, D_MODEL), GMLP_CH ** -0.5 * beta),
        "w_xa_proj": nrm(ks[15], (L, XA_WIDTH, D_MODEL), XA_WIDTH ** -0.5 * beta),
        "w_out": nrm(ks[16], (L, D_MODEL, D_MODEL), D_MODEL ** -0.5 * beta),
        "ln1_g": 1.0 + nrm(ks[17], (L, D_MODEL), 0.01),
        "ln1_b": nrm(ks[18], (L, D_MODEL), 0.01),
        "w_router": nrm(ks[19], (L, D_MODEL, N_EXPERTS), D_MODEL ** -0.5),
        "b_router": nrm(ks[20], (L, N_EXPERTS), 0.01),
        "w_gate_up": nrm(ks[21], (L, N_EXPERTS, D_MODEL, 2 * D_FF), D_MODEL ** -0.5 * beta),
        "b_gate_up": nrm(ks[22], (L, N_EXPERTS, 2 * D_FF), 0.01),
        "w_down": nrm(ks[23], (L, N_EXPERTS, D_FF, D_MODEL), D_FF ** -0.5 * beta),
        "b_down": nrm(ks[24], (L, N_EXPERTS, D_MODEL), 0.01),
        "ln2_g": 1.0 + nrm(ks[25], (L, D_MODEL), 0.01),
        "ln2_b": nrm(ks[26], (L, D_MODEL), 0.01),
    }


def reference(x, mem, w_in, b_gate, conv_w, gmlp_ws, gmlp_b, gmlp_ln_g, gmlp_ln_b,
              mem_ln_g, mem_ln_b, w_kv, w_conv_proj, w_gmlp_proj, w_xa_proj, w_out,
              ln1_g, ln1_b, w_router, b_router, w_gate_up, b_gate_up, w_down, b_down,
              ln2_g, ln2_b):
    for l in range(DEPTH):
        mem_n = layer_norm(mem, mem_ln_g[l], mem_ln_b[l])
        mix = token_mix(x, mem_n, w_in[l], b_gate[l], conv_w[l], gmlp_ws[l], gmlp_b[l],
                        gmlp_ln_g[l], gmlp_ln_b[l], w_kv[l], w_conv_proj[l],
                        w_gmlp_proj[l], w_xa_proj[l], w_out[l])
        x = layer_norm(DEEPNORM_ALPHA * x + mix, ln1_g[l], ln1_b[l])
        ffn = moe_ffn(x, w_router[l], b_router[l], w_gate_up[l], b_gate_up[l],
                      w_down[l], b_down[l])
        x = layer_norm(DEEPNORM_ALPHA * x + ffn, ln2_g[l], ln2_b[l])
    return x
```

```python
import contextlib
import numpy as np
import concourse.bass as bass
import concourse.mybir as mybir
from concourse.bass_utils import run_bass_kernel_spmd

F32 = mybir.dt.float32
BF16 = mybir.dt.bfloat16
I32 = mybir.dt.int32
U32 = mybir.dt.uint32
AF = mybir.ActivationFunctionType
ALU = mybir.AluOpType

ENGS = ("pe", "act", "dve", "pool", "sp")


class Buf:
    __slots__ = ("name", "writers", "readers")

    def __init__(self, name=""):
        self.name = name
        self.writers = []
        self.readers = []


class Op:
    __slots__ = ("eng", "emit", "deps", "is_dma", "signal", "val", "sem", "idx")

    def __init__(self, eng, emit, is_dma):
        self.eng = eng
        self.emit = emit
        self.deps = []
        self.is_dma = is_dma
        self.signal = False
        self.val = None
        self.sem = None
        self.idx = None


class Sched:
    def __init__(self, nc, n_dma_sems=None):
        self.nc = nc
        self.streams = {e: [] for e in ENGS}
        self.n_dma_sems = n_dma_sems or {"sp": 8, "act": 2, "pool": 4}
        self.last = {}
        self.dmas_since_barrier = []

    @staticmethod
    def _same_inorder(a, b):
        return (not a.is_dma) and (not b.is_dma) and a.eng == b.eng

    def op(self, eng, emit, reads=(), writes=(), dma=False, extra_deps=()):
        o = Op(eng, emit, dma)
        o.idx = len(self.streams[eng])
        deps = list(extra_deps)
        for b in reads:
            deps.extend(b.writers)
        for b in writes:
            deps.extend(b.readers)
            deps.extend(b.writers)
        seen = set()
        for d in deps:
            if d is o or id(d) in seen:
                continue
            seen.add(id(d))
            if eng == "pe" and d.eng == "pe" and not d.is_dma and not dma:
                continue
            o.deps.append(d)
            d.signal = True
        for b in reads:
            b.readers = [r for r in b.readers if not self._same_inorder(r, o)] + [o]
        for b in writes:
            if b.readers:
                b.readers = []
                b.writers = []
            b.writers = [w for w in b.writers if not self._same_inorder(w, o)] + [o]
        self.streams[eng].append(o)
        if dma:
            self.dmas_since_barrier.append(o)
        else:
            self.last[eng] = o
        return o

    def barrier(self):
        deps = list(self.last.values()) + list(self.dmas_since_barrier)
        self.dmas_since_barrier = []
        for e in ENGS:
            self.op(e, lambda eng: eng.nop(), extra_deps=deps)

    def emit_all(self, final_waits=()):
        nc = self.nc
        with contextlib.ExitStack() as st:
            csem = {e: st.enter_context(nc.semaphore("c_" + e)) for e in ("pe", "act", "dve", "pool", "sp")}
            dsems = {e: [st.enter_context(nc.semaphore("d_%s%d" % (e, i))) for i in range(n)]
                     for e, n in self.n_dma_sems.items()}
            for e in ENGS:
                cnt = 0
                nd = self.n_dma_sems.get(e, 0)
                dcnt = [0] * max(nd, 1)
                rr = 0
                for o in self.streams[e]:
                    if o.is_dma:
                        s = rr % nd
                        rr += 1
                        dcnt[s] += 1
                        o.sem = dsems[e][s]
                        o.val = 16 * dcnt[s]
                    elif o.signal:
                        cnt += 1
                        o.sem = csem[e]
                        o.val = cnt
            fw = {}
            for (e, d) in final_waits:
                fw.setdefault(e, []).append(d)
            block = st.enter_context(nc.Block())
            streams = self.streams

            def make(e):
                def body(eng):
                    seen = {}

                    def wait(sem, val):
                        k = id(sem)
                        if seen.get(k, 0) < val:
                            eng.wait_ge(sem, val)
                            seen[k] = val

                    for o in streams[e]:
                        if o.is_dma and o.val > 16:
                            wait(o.sem, o.val - 16)
                        mx = {}
                        for d in o.deps:
                            k = id(d.sem)
                            if k not in mx or mx[k][1] < d.val:
                                mx[k] = (d.sem, d.val)
                        for sem_, val_ in mx.values():
                            wait(sem_, val_)
                        ins = o.emit(eng)
                        if o.is_dma:
                            ins.then_inc(o.sem, 16)
                        elif o.signal:
                            ins.then_inc(o.sem, 1)
                    for d in fw.get(e, ()):
                        wait(d.sem, d.val)
                return body

            for e, deco in (("pe", block.tensor), ("act", block.scalar), ("dve", block.vector),
                            ("pool", block.gpsimd), ("sp", block.sync)):
                if streams[e] or e in fw:
                    deco(make(e))


P = 128
D = 1024
SEQ = 4096
TT = 512
NCH = SEQ // P
NE = 32
CAP = 768
NST = CAP // P
NROWS = NE * CAP
ZROW = NROWS
ALPHA = 2.0 ** 0.25
EPS = 1e-5
SEG_KV = 0
SEG_A = 2048
SEG_B = SEG_A + 1024
SEG_C = SEG_B + 1024
SEG_D = SEG_C + 3072
SEG_E = SEG_D + 1024
SEG_F = SEG_E + 8 * 768
WCOLS = SEG_F + 1024


def build(n_tiles=SEQ // TT, n_exp=NE, debug=False):
    nc = bass.Bass("TRN2", target_bir_lowering=False)

    def din(name, shape, dt=F32):
        return nc.dram_tensor(name, list(shape), dt, kind="ExternalInput").ap()

    xT_d = din("xT", [D, SEQ])
    x_d = din("x", [SEQ, D])
    mem_d = din("mem", [256, D])
    wmix_d = din("wmix", [D, WCOLS])
    bgate_d = din("b_gate_T", [P, 24])
    convw_d = din("conv_w_T", [P, 24])
    wsT_d = din("gmlp_wsT", [P, 8 * P])
    gmlpb_d = din("gmlp_b", [1, D])
    glng_d = din("gmlp_ln_g", [1, D])
    glnb_d = din("gmlp_ln_b", [1, D])
    mlng_d = din("mem_ln_g", [1, D])
    mlnb_d = din("mem_ln_b", [1, D])
    l1g_d = din("ln1_g", [1, D])
    l1b_d = din("ln1_b", [1, D])
    l2g_d = din("ln2_g", [1, D])
    l2b_d = din("ln2_b", [1, D])
    wr_d = din("w_router", [D, NE])
    br_d = din("b_router", [1, NE])
    wgu_d = din("w_gate_up", [NE, D, 2 * D])
    bgu_d = din("b_gate_up_T", [P, NE * 16])
    wd_d = din("w_down", [NE, D, D])
    bd_d = din("b_down", [NE, D])
    out_d = nc.dram_tensor("out", [SEQ, D], F32, kind="ExternalOutput").ap()
    if debug:
        x1_scr = nc.dram_tensor("x1_dbg", [SEQ, D], F32, kind="ExternalOutput").ap()
    else:
        x1_scr = nc.dram_tensor("x1_scr", [SEQ, D], F32).ap()
    wmix_bf = nc.dram_tensor("wmix_bf", [D, WCOLS], BF16).ap()
    xs_scr = nc.dram_tensor("xs_scr", [NROWS, D], BF16).ap()
    eo_scr = nc.dram_tensor("eo_scr", [NROWS + 1, D], F32).ap()

    S = Sched(nc)
    out_dmas = []

    with contextlib.ExitStack() as st:
        NF = 52224
        big = st.enter_context(nc.sbuf_tensor("arena", [P, NF], F32))
        banks = [st.enter_context(nc.psum_tensor("bank%d" % i, [P, 512], F32)) for i in range(8)]
        bank_bufs = [Buf("bank%d" % i) for i in range(8)]
        bank_rr = [0]

        def bank():
            i = bank_rr[0] % 8
            bank_rr[0] += 1
            return banks[i][:, :], bank_bufs[i]

        top = [0]

        def alloc(ncols, dt=F32, shape=None):
            a = big[:, top[0]:top[0] + ncols]
            top[0] += ncols
            assert top[0] <= NF, "SBUF arena overflow %d" % top[0]
            if dt != F32:
                a = a.bitcast(dt)
            if shape is not None:
                names = " ".join("d%d" % i for i in range(len(shape)))
                kw = {"d%d" % i: s for i, s in enumerate(shape)}
                a = a.rearrange("p (%s) -> p %s" % (names, names), **kw)
            return a

        def TT_(eng, out, in0, in1, op, R, W):
            return S.op(eng, lambda e: e.tensor_tensor(out=out, in0=in0, in1=in1, op=op), R, W)

        def TS_(eng, out, in0, s1, s2, op0, op1, R, W):
            if op1 is None:
                return S.op(eng, lambda e: e.tensor_scalar(out=out, in0=in0, scalar1=s1, scalar2=None, op0=op0), R, W)
            return S.op(eng, lambda e: e.tensor_scalar(out=out, in0=in0, scalar1=s1, scalar2=s2, op0=op0, op1=op1), R, W)

        def STT_(eng, out, in0, sc, in1, op0, op1, R, W, accum=None):
            if accum is None:
                return S.op(eng, lambda e: e.scalar_tensor_tensor(out=out, in0=in0, scalar=sc, in1=in1, op0=op0, op1=op1), R, W)
            return S.op(eng, lambda e: e.scalar_tensor_tensor(out=out, in0=in0, scalar=sc, in1=in1, op0=op0, op1=op1,
                                                              accum_out=accum), R, W)

        def ACT_(out, in_, func, R, W, bias=None, scale=1.0, accum=None):
            kw = {}
            if bias is not None:
                kw["bias"] = bias
            if accum is not None:
                kw["accum_out"] = accum
            return S.op("act", lambda e: e.activation(out=out, in_=in_, func=func, scale=scale, **kw), R, W)

        def CP_(eng, out, in_, R, W):
            if eng == "act":
                return S.op("act", lambda e: e.copy(out=out, in_=in_), R, W)
            return S.op(eng, lambda e: e.tensor_copy(out=out, in_=in_), R, W)

        def MS_(eng, ap, val, W):
            return S.op(eng, lambda e: e.memset(ap, val), (), W)

        def MM_(out, lhsT, rhs, start, stop, R, W):
            return S.op("pe", lambda e: e.matmul(out, lhsT=lhsT, rhs=rhs, start=start, stop=stop), R, W)

        def TR_(out, in_, ident, R, W):
            return S.op("pe", lambda e: e.transpose(out=out, in_=in_, identity=ident), R, W)

        def DMA_(eng, out, in_, R, W):
            return S.op(eng, lambda e: e.dma_start(out=out, in_=in_), R, W, dma=True)

        def layer_norm(xin, b_x, g_bc, b_bc, b_par, out, b_out, tmp, b_tmp):
            st6, mv, rstd = tmp
            for h in range(2):
                S.op("dve", lambda e, h=h: e.bn_stats(out=st6[:, h, :], in_=xin[:, h * 512:(h + 1) * 512]), [b_x], [b_tmp])
            S.op("dve", lambda e: e.bn_aggr(out=mv, in_=st6.rearrange("p a b -> p (a b)")), [b_tmp], [b_tmp])
            ACT_(rstd, mv[:, 1:2], AF.Sqrt, [b_tmp, b_eps], [b_tmp], bias=eps_t)
            S.op("dve", lambda e: e.reciprocal(out=rstd, in_=rstd), [b_tmp], [b_tmp])
            TS_("dve", xin, xin, mv[:, 0:1], rstd, ALU.subtract, ALU.mult, [b_x, b_tmp], [b_x])
            TT_("pool", xin, xin, g_bc, ALU.mult, [b_x, b_par], [b_x])
            TT_("pool", out, xin, b_bc, ALU.add, [b_x, b_par], [b_out])

        identf = alloc(128); b_identf = Buf()
        identb = alloc(64, BF16); b_identb = Buf()
        onesb = alloc(64, BF16); b_onesb = Buf()
        ustrb = alloc(64, BF16); b_ustr = Buf()
        eps_t = alloc(1); b_eps = Buf()
        iota32 = alloc(32); b_iota = Buf()
        wr_sb = alloc(8 * NE, F32, (8, NE)); b_wr = Buf()
        br_bc = alloc(NE); b_br = Buf()
        gate_all = alloc(NCH * 4, F32, (NCH, 4)); b_gate_all = [Buf() for _ in range(NCH)]
        dst_all = alloc(NCH * 4, I32, (NCH, 4)); b_dst_all = [Buf() for _ in range(NCH)]
        tot_bc = alloc(NE); b_tot = Buf()
        bgu_sb = alloc(NE * 16); b_bgu = Buf()
        lnC_g = alloc(D); lnC_b = alloc(D); b_lnC = Buf()
        zrow = alloc(D); b_zrow = Buf()
        lnt = (alloc(12, F32, (2, 6)), alloc(2), alloc(1)); b_lnt = Buf()
        persist_top = top[0]
        WcT = alloc(512, BF16, (8, P)); b_WcT = Buf()
        biasbc = alloc(D, F32, (8, P)); b_biasbc = Buf()
        KT = alloc(1024, BF16, (8, 256)); b_KT = Buf()
        Vt = alloc(1024, BF16, (2, D)); b_Vt = Buf()
        bgate = alloc(24); b_bgate = Buf()
        convw = alloc(24); b_convw = Buf()
        halo = alloc(16, F32, (8, 2)); b_halo = [Buf() for _ in range(8)]
        gln_g = alloc(D); gln_b = alloc(D); b_gln = Buf()
        l1_g = alloc(D); l1_b = alloc(D); b_l1 = Buf()
        mixer_base = top[0]

        MS_("pool", identf, 0.0, [b_identf])
        S.op("pool", lambda e: e.affine_select(out=identf, in_=identf, pattern=[[-1, P]], compare_op=ALU.not_equal,
                                               fill=1.0, base=0, channel_multiplier=1), [b_identf], [b_identf])
        CP_("dve", identb, identf, [b_identf], [b_identb])
        MS_("pool", onesb, 1.0, [b_onesb])
        ustrf = alloc(128); b_ustrf = Buf()
        MS_("pool", ustrf, 1.0, [b_ustrf])
        S.op("pool", lambda e: e.affine_select(out=ustrf, in_=ustrf, pattern=[[1, P]], compare_op=ALU.is_gt,
                                               fill=0.0, base=0, channel_multiplier=-1), [b_ustrf], [b_ustrf])
        CP_("dve", ustrb, ustrf, [b_ustrf], [b_ustr])
        MS_("pool", eps_t, EPS, [b_eps])
        S.op("pool", lambda e: e.iota(iota32, pattern=[[1, NE]], base=0, channel_multiplier=0,
                                      allow_small_or_imprecise_dtypes=True), (), [b_iota])
        MS_("pool", tot_bc, 0.0, [b_tot])
        MS_("pool", halo.rearrange("p a b -> p (a b)"), 0.0, b_halo)
        MS_("pool", zrow, 0.0, [b_zrow])
        b_eo = Buf("eo_scr")
        DMA_("sp", eo_scr[ZROW:ZROW + 1, :], zrow[0:1, :], [b_zrow], [b_eo])
        DMA_("sp", wr_sb, wr_d.rearrange("(k p) n -> p k n", p=P), (), [b_wr])
        DMA_("sp", br_bc, br_d.partition_broadcast(P), (), [b_br])
        DMA_("sp", bgu_sb, bgu_d, (), [b_bgu])
        DMA_("sp", lnC_g, mlng_d.partition_broadcast(P), (), [b_lnC])
        DMA_("sp", lnC_b, mlnb_d.partition_broadcast(P), (), [b_lnC])
        DMA_("sp", biasbc.rearrange("p a b -> p (a b)"), gmlpb_d.partition_broadcast(P), (), [b_biasbc])
        DMA_("sp", bgate, bgate_d, (), [b_bgate])
        DMA_("sp", convw, convw_d, (), [b_convw])
        DMA_("sp", gln_g, glng_d.partition_broadcast(P), (), [b_gln])
        DMA_("sp", gln_b, glnb_d.partition_broadcast(P), (), [b_gln])
        DMA_("sp", l1_g, l1g_d.partition_broadcast(P), (), [b_l1])
        DMA_("sp", l1_b, l1b_d.partition_broadcast(P), (), [b_l1])
        wsTf = alloc(8 * P, F32, (8, P)); b_wsTf = Buf()
        DMA_("sp", wsTf.rearrange("p a b -> p (a b)"), wsT_d, (), [b_wsTf])
        S.op("pool", lambda e: e.affine_select(out=wsTf, in_=wsTf, pattern=[[0, 8], [1, P]], compare_op=ALU.is_ge,
                                               fill=0.0, base=0, channel_multiplier=-1), [b_wsTf], [b_wsTf])
        CP_("dve", WcT, wsTf, [b_wsTf], [b_WcT])
        const_top = top[0]

        NSLOT = 4
        ring = [alloc(2048, BF16, (8, 512)) for _ in range(NSLOT)]
        ring_bufs = [Buf("slab%d" % i) for i in range(NSLOT)]
        b_wmixbf = [Buf("wmixbf%d" % i) for i in range(WCOLS // 512)]

        for blk in range(WCOLS // 512):
            sl = blk % NSLOT
            c0 = blk * 512
            DMA_("pool", ring[sl], wmix_d[:, c0:c0 + 512].rearrange("(k p) n -> p k n", p=P), (), [ring_bufs[sl]])
            DMA_("sp", wmix_bf[:, c0:c0 + 512].rearrange("(k p) n -> p k n", p=P), ring[sl], [ring_bufs[sl]], [b_wmixbf[blk]])

        slab_list = []
        for c in range(4):
            slab_list.append((SEG_KV + c * 512, 512))
        for _ in range(n_tiles):
            slab_list += [(SEG_A, 512), (SEG_A + 512, 512), (SEG_B, 512), (SEG_B + 512, 512)]
            slab_list += [(SEG_C + c * 384, 384) for c in range(8)]
            slab_list += [(SEG_D, 512), (SEG_D + 512, 512)]
            for dc in range(8):
                slab_list += [(SEG_E + dc * 768, 384), (SEG_E + dc * 768 + 384, 384)]
            slab_list += [(SEG_F, 512), (SEG_F + 512, 512)]
        slab_issued = [0]
        slab_next = [0]

        def issue_slab():
            i = slab_issued[0]
            if i >= len(slab_list):
                return
            c0, w = slab_list[i]
            sl = i % NSLOT
            rb = [b_wmixbf[b] for b in range(c0 // 512, (c0 + w - 1) // 512 + 1)]
            DMA_("sp", ring[sl][:, :, 0:w], wmix_bf[:, c0:c0 + w].rearrange("(k p) n -> p k n", p=P), rb, [ring_bufs[sl]])
            slab_issued[0] += 1

        def get_slab():
            i = slab_next[0]
            slab_next[0] += 1
            while slab_issued[0] < min(i + NSLOT - 1, len(slab_list)):
                issue_slab()
            sl = i % NSLOT
            return ring[sl], ring_bufs[sl]

        memx = [alloc(D) for _ in range(2)]; b_memx = [Buf() for _ in range(2)]
        memnT = alloc(1024, BF16, (8, 256)); b_memnT = Buf()
        for j in range(2):
            DMA_("sp", memx[j], mem_d[j * P:(j + 1) * P, :], (), [b_memx[j]])
            layer_norm(memx[j], b_memx[j], lnC_g, lnC_b, b_lnC, memx[j], b_memx[j], lnt, b_lnt)
            for hh in range(2):
                pb, bb = bank()
                for kk in range(4):
                    k = hh * 4 + kk
                    TR_(pb[:, kk * P:(kk + 1) * P], memx[j][:, k * P:(k + 1) * P], identf, [b_memx[j], b_identf], [bb])
                CP_("act", memnT[:, hh * 4:(hh + 1) * 4, j * P:(j + 1) * P], pb.rearrange("p (a b) -> p a b", a=4), [bb], [b_memnT])
        for c in range(2):
            slab, sb_ = get_slab()
            for dd in range(4):
                dchunk = c * 4 + dd
                pb, bb = bank()
                for k in range(8):
                    MM_(pb[:, 0:256], slab[:, k, dd * P:(dd + 1) * P], memnT[:, k, :], k == 0, k == 7, [sb_, b_memnT], [bb])
                CP_("act", KT[:, dchunk, :], pb[:, 0:256], [bb], [b_KT])
        for c in range(2):
            slab, sb_ = get_slab()
            for mc in range(2):
                pb, bb = bank()
                for k in range(8):
                    MM_(pb, memnT[:, k, mc * P:(mc + 1) * P], slab[:, k, :], k == 0, k == 7, [sb_, b_memnT], [bb])
                CP_("dve", Vt[:, mc, c * 512:(c + 1) * 512], pb, [bb], [b_Vt])

        top[0] = const_top + NSLOT * 2048
        xT_bf = alloc(2048, BF16, (8, TT)); b_xT = Buf()
        vn_mg = alloc(2048, BF16)
        v_n = vn_mg.rearrange("p (a b) -> p a b", a=4)
        merged = vn_mg.rearrange("p (a b) -> p a b", a=8)
        b_vnmg = Buf()
        vg = [alloc(D) for _ in range(2)]; b_vg = [Buf() for _ in range(2)]
        ug = [alloc(TT) for _ in range(2)]; b_ug = [Buf() for _ in range(2)]
        ftmp = [alloc(TT) for _ in range(2)]; b_ftmp = [Buf() for _ in range(2)]
        y_conv = alloc(2048, BF16, (8, TT)); b_yconv = Buf()
        y_gmlp = alloc(2048, BF16, (8, TT)); b_ygmlp = Buf()
        y_xa = alloc(2048, BF16, (8, TT)); b_yxa = Buf()
        cc_sb = alloc(TT); b_cc = Buf()
        u_t = [alloc(TT + 4) for _ in range(2)]; b_u = [Buf() for _ in range(2)]
        acc = alloc(TT); b_acc = Buf()
        qT = [alloc(512, BF16, (2, TT)) for _ in range(2)]; b_qT = [Buf() for _ in range(2)]
        PT = [alloc(512, BF16, (2, TT)) for _ in range(2)]; b_PT = [Buf() for _ in range(2)]
        rden = [alloc(TT) for _ in range(2)]; b_rden = [Buf() for _ in range(2)]
        sig = [alloc(3 * TT, F32, (3, TT)) for _ in range(2)]; b_sig = [Buf() for _ in range(2)]
        mt = alloc(3 * TT, F32, (3, TT)); b_mt = Buf()
        xc = [alloc(D) for _ in range(2)]; b_xc = [Buf() for _ in range(2)]
        r2 = [alloc(D) for _ in range(2)]; b_r2 = [Buf() for _ in range(2)]
        x1bf = [alloc(512, BF16) for _ in range(2)]; b_x1bf = [Buf() for _ in range(2)]
        x1T = alloc(D, F32, (8, P)); b_x1T = Buf()
        lgt = alloc(NE); mx8 = alloc(8); mi8 = alloc(8, U32); ef4 = alloc(4); negmax = alloc(1)
        ex4 = alloc(4); ssum = alloc(1); Mbf = alloc(NE // 2, BF16); pos = alloc(NE); junk = alloc(NE)
        pos4 = alloc(4); dstf = alloc(4); keep = alloc(4)
        b_rt = Buf()
        b_x1scr = [Buf() for _ in range(NCH)]
        b_xs = Buf("xs_scr")
        bc_reg = {}

        S.barrier()

        for ti in range(n_tiles):
            t0 = ti * TT
            if ti == 0:
                DMA_("pool", xT_bf, xT_d[:, t0:t0 + TT].rearrange("(k p) t -> p k t", p=P), (), [b_xT])

            slabA = [get_slab(), get_slab()]
            for j in range(4):
                vgj, bvg = vg[j % 2], b_vg[j % 2]
                for h in range(2):
                    pb, bb = bank()
                    sl, sb_ = slabA[h]
                    for k in range(8):
                        MM_(pb, xT_bf[:, k, j * P:(j + 1) * P], sl[:, k, :], k == 0, k == 7, [b_xT, sb_], [bb])
                    ACT_(vgj[:, h * 512:(h + 1) * 512], pb, AF.Gelu_apprx_tanh, [bb], [bvg])
                layer_norm(vgj, bvg, gln_g, gln_b, b_gln, v_n[:, j, :], b_vnmg, lnt, b_lnt)

            slabB = [get_slab(), get_slab()]
            for g in range(8):
                sl, sb_ = slabB[g // 4]
                gg = g % 4
                pu, bpu = bank()
                for k in range(8):
                    MM_(pu, sl[:, k, gg * P:(gg + 1) * P], xT_bf[:, k, :], k == 0, k == 7, [sb_, b_xT], [bpu])
                ACT_(ug[g % 2], pu, AF.Gelu_apprx_tanh, [bpu], [b_ug[g % 2]])
                pf, bpf = bank()
                for j in range(4):
                    MM_(pf[:, j * P:(j + 1) * P], v_n[:, j, g * P:(g + 1) * P], WcT[:, g, :], True, True, [b_vnmg, b_WcT], [bpf])
                TT_("dve", ftmp[g % 2].rearrange("p (j t) -> p j t", j=4), pf.rearrange("p (j t) -> p j t", j=4),
                    biasbc[:, g, :].unsqueeze(1).broadcast_to([P, 4, P]), ALU.add, [bpf, b_biasbc], [b_ftmp[g % 2]])
                TT_("pool", y_gmlp[:, g, :], ftmp[g % 2], ug[g % 2], ALU.mult, [b_ftmp[g % 2], b_ug[g % 2]], [b_ygmlp])

            for c in range(8):
                sl, sb_ = get_slab()
                pcs = []
                for r in range(3):
                    pb, bb = bank()
                    for k in range(8):
                        MM_(pb, sl[:, k, r * P:(r + 1) * P], xT_bf[:, k, :], k == 0, k == 7, [sb_, b_xT], [bb])
                    pcs.append((pb, bb))
                (pcb, bcb), (pcc, bcc), (pch, bch) = pcs
                u, bu = u_t[c % 2], b_u[c % 2]
                CP_("act", cc_sb, pcc, [bcc], [b_cc])
                CP_("pool", u[:, 0:2], halo[:, c, :], [b_halo[c]], [bu])
                TT_("dve", u[:, 2:TT + 2], cc_sb, pch, ALU.mult, [b_cc, bch], [bu])
                CP_("pool", halo[:, c, :], u[:, TT:TT + 2], [bu], [b_halo[c]])
                TS_("dve", acc, u[:, 2:TT + 2], convw[:, 16 + c:17 + c], None, ALU.mult, None, [bu, b_convw], [b_acc])
                STT_("dve", acc, u[:, 1:TT + 1], convw[:, 8 + c:9 + c], acc, ALU.mult, ALU.add, [bu, b_convw, b_acc], [b_acc])
                STT_("dve", acc, u[:, 0:TT], convw[:, c:c + 1], acc, ALU.mult, ALU.add, [bu, b_convw, b_acc], [b_acc])
                TT_("dve", y_conv[:, c, :], acc, pcb, ALU.mult, [b_acc, bcb], [b_yconv])

            slabD = [get_slab(), get_slab()]
            for h in range(4):
                sl, sb_ = slabD[h // 2]
                q, bq = qT[h % 2], b_qT[h % 2]
                for dc in range(2):
                    cc = (h % 2) * 2 + dc
                    pb, bb = bank()
                    for k in range(8):
                        MM_(pb, sl[:, k, cc * P:(cc + 1) * P], xT_bf[:, k, :], k == 0, k == 7, [sb_, b_xT], [bb])
                    CP_("act", q[:, dc, :], pb, [bb], [bq])
                pt, bpt = PT[h % 2], b_PT[h % 2]
                for mc in range(2):
                    pb, bb = bank()
                    for dc in range(2):
                        MM_(pb, KT[:, h * 2 + dc, mc * P:(mc + 1) * P], q[:, dc, :], dc == 0, dc == 1, [b_KT, bq], [bb])
                    ACT_(pt[:, mc, :], pb, AF.Exp, [bb], [bpt], scale=0.0625)
                pden, bden = bank()
                for mc in range(2):
                    MM_(pden, onesb, pt[:, mc, :], mc == 0, mc == 1, [b_onesb, bpt], [bden])
                rd, brd = rden[h % 2], b_rden[h % 2]
                S.op("dve", lambda e, rd=rd, pden=pden: e.reciprocal(out=rd, in_=pden), [bden], [brd])
                for dc in range(2):
                    po, bpo = bank()
                    for mc in range(2):
                        MM_(po, Vt[:, mc, (h * 2 + dc) * P:(h * 2 + dc + 1) * P], pt[:, mc, :], mc == 0, mc == 1, [b_Vt, bpt], [bpo])
                    TT_("dve", y_xa[:, h * 2 + dc, :], rd, po, ALU.mult, [brd, bpo], [b_yxa])

            ys = [(y_conv, b_yconv), (y_gmlp, b_ygmlp), (y_xa, b_yxa)]
            for dc in range(8):
                slg, sbg = get_slab()
                slp, sbp = get_slab()
                sg, bsg = sig[dc % 2], b_sig[dc % 2]
                for r in range(3):
                    pb, bb = bank()
                    for k in range(8):
                        MM_(pb, slg[:, k, r * P:(r + 1) * P], xT_bf[:, k, :], k == 0, k == 7, [sbg, b_xT], [bb])
                    ACT_(sg[:, r, :], pb, AF.Sigmoid, [bb, b_bgate], [bsg], bias=bgate[:, r * 8 + dc:r * 8 + dc + 1])
                for r in range(3):
                    pb, bb = bank()
                    yr, byr = ys[r]
                    for k in range(8):
                        MM_(pb, slp[:, k, r * P:(r + 1) * P], yr[:, k, :], k == 0, k == 7, [sbp, byr], [bb])
                    TT_("dve", mt[:, r, :], sg[:, r, :], pb, ALU.mult, [bsg, bb], [b_mt])
                TT_("pool", mt[:, 0, :], mt[:, 0, :], mt[:, 1, :], ALU.add, [b_mt], [b_mt])
                TT_("pool", merged[:, dc, :], mt[:, 0, :], mt[:, 2, :], ALU.add, [b_mt], [b_vnmg])

            if ti + 1 < n_tiles:
                DMA_("pool", xT_bf, xT_d[:, t0 + TT:t0 + 2 * TT].rearrange("(k p) t -> p k t", p=P), (), [b_xT])

            slabF = [get_slab(), get_slab()]

            def stage1(j):
                ci = ti * 4 + j
                r_t, b_r = r2[j % 2], b_r2[j % 2]
                xcj, bxc = xc[j % 2], b_xc[j % 2]
                DMA_("sp", xcj, x_d[ci * P:(ci + 1) * P, :], (), [bxc])
                for h in range(2):
                    pb, bb = bank()
                    sl, sb_ = slabF[h]
                    for k in range(8):
                        MM_(pb, merged[:, k, j * P:(j + 1) * P], sl[:, k, :], k == 0, k == 7, [b_vnmg, sb_], [bb])
                    STT_("dve", r_t[:, h * 512:(h + 1) * 512], xcj[:, h * 512:(h + 1) * 512], ALPHA, pb, ALU.mult, ALU.add,
                         [bxc, bb], [b_r])
                layer_norm(r_t, b_r, l1_g, l1_b, b_l1, r_t, b_r, lnt, b_lnt)
                DMA_("sp", x1_scr[ci * P:(ci + 1) * P, :], r_t, [b_r], [b_x1scr[ci]])
                xb, bxb = x1bf[j % 2], b_x1bf[j % 2]
                CP_("act", xb, r_t, [b_r], [bxb])

            def stage2(j):
                ci = ti * 4 + j
                r_t, b_r = r2[j % 2], b_r2[j % 2]
                xb, bxb = x1bf[j % 2], b_x1bf[j % 2]
                for hh in range(2):
                    pb, bb = bank()
                    for kk in range(4):
                        k = hh * 4 + kk
                        TR_(pb[:, kk * P:(kk + 1) * P], r_t[:, k * P:(k + 1) * P], identf, [b_r, b_identf], [bb])
                    CP_("act", x1T[:, hh * 4:(hh + 1) * 4, :], pb.rearrange("p (a b) -> p a b", a=4), [bb], [b_x1T])
                pl, bpl = bank()
                for k in range(8):
                    MM_(pl[:, 0:NE], x1T[:, k, :], wr_sb[:, k, :], k == 0, k == 7, [b_x1T, b_wr], [bpl])
                TT_("dve", lgt, pl[:, 0:NE], br_bc, ALU.add, [bpl, b_br], [b_rt])
                S.op("dve", lambda e: e.max(out=mx8, in_=lgt), [b_rt], [b_rt])
                S.op("dve", lambda e: e.max_index(out=mi8, in_max=mx8, in_values=lgt), [b_rt], [b_rt])
                TS_("dve", negmax, mx8[:, 0:1], -1.0, None, ALU.mult, None, [b_rt], [b_rt])
                MS_("pool", ssum, 0.0, [b_rt])
                MS_("pool", pos4, 0.0, [b_rt])
                ACT_(ex4, mx8[:, 0:4], AF.Exp, [b_rt], [b_rt], bias=negmax, accum=ssum)
                S.op("dve", lambda e: e.reciprocal(out=ssum, in_=ssum), [b_rt], [b_rt])
                TS_("dve", gate_all[:, ci, :], ex4, ssum, None, ALU.mult, None, [b_rt], [b_gate_all[ci]])
                TS_("dve", Mbf, lgt, mx8[:, 3:4], None, ALU.is_ge, None, [b_rt], [b_rt])
                pp, bpp = bank()
                MM_(pp[:, 0:NE], ustrb, Mbf, True, True, [b_ustr, b_rt], [bpp])
                MM_(pp[:, NE:2 * NE], onesb, Mbf, True, True, [b_onesb, b_rt], [bpp])
                TT_("dve", pos, pp[:, 0:NE], tot_bc, ALU.add, [bpp, b_tot], [b_rt])
                TT_("dve", tot_bc, pp[:, NE:2 * NE], tot_bc, ALU.add, [bpp, b_tot], [b_tot])
                CP_("dve", ef4, mi8[:, 0:4], [b_rt], [b_rt])
                for k in range(4):
                    STT_("dve", junk, iota32, ef4[:, k:k + 1], pos, ALU.is_equal, ALU.mult, [b_iota, b_rt], [b_rt],
                         accum=pos4[:, k:k + 1])
                STT_("dve", dstf, ef4, float(CAP), pos4, ALU.mult, ALU.add, [b_rt], [b_rt])
                TS_("dve", keep, pos4, float(CAP), None, ALU.is_lt, None, [b_rt], [b_rt])
                TS_("dve", dstf, dstf, -float(ZROW), None, ALU.add, None, [b_rt], [b_rt])
                TT_("dve", dstf, dstf, keep, ALU.mult, [b_rt], [b_rt])
                TS_("dve", dstf, dstf, float(ZROW), None, ALU.add, None, [b_rt], [b_rt])
                CP_("dve", dst_all[:, ci, :], dstf, [b_rt], [b_dst_all[ci]])
                for k in range(4):
                    def scat(e, ci=ci, k=k, xb=xb):
                        if "r" not in bc_reg:
                            bc_reg["r"] = e.to_reg(NROWS - 1)
                        return e.indirect_dma_start(
                            out=xs_scr, out_offset=bass.IndirectOffsetOnAxis(ap=dst_all[:, ci, k:k + 1], axis=0),
                            in_=xb, in_offset=None, bounds_check=bc_reg["r"], oob_is_err=False)
                    S.op("pool", scat, [bxb, b_dst_all[ci]], [b_xs], dma=True)

            stage1(0)
            for j in range(4):
                if j + 1 < 4:
                    stage1(j + 1)
                stage2(j)

        S.barrier()

        top[0] = persist_top
        DMA_("sp", lnC_g, l2g_d.partition_broadcast(P), (), [b_lnC])
        DMA_("sp", lnC_b, l2b_d.partition_broadcast(P), (), [b_lnC])
        wgu = [alloc(8192, BF16, (8, 2 * D)) for _ in range(2)]
        b_wgu = [[Buf() for _ in range(8)] for _ in range(2)]
        wdn = [alloc(4096, BF16, (8, D)) for _ in range(2)]
        b_wdn = [[Buf() for _ in range(8)] for _ in range(2)]
        xs_sm = [alloc(512, BF16) for _ in range(2)]; b_xssm = [Buf() for _ in range(2)]
        xsT2 = [alloc(4 * CAP, BF16, (8, CAP)) for _ in range(2)]; b_xsT2 = [[Buf() for _ in range(NST)] for _ in range(2)]
        actT = alloc(4 * CAP, BF16, (8, CAP)); b_actT = Buf()
        g_sb = [alloc(512) for _ in range(2)]; b_g = [Buf() for _ in range(2)]
        s_sb = [alloc(512) for _ in range(2)]; b_s = [Buf() for _ in range(2)]
        u_sb = [alloc(512) for _ in range(2)]; b_us = [Buf() for _ in range(2)]
        eo_sb = [alloc(D) for _ in range(2)]; b_eosb = [Buf() for _ in range(2)]
        bdbc = [alloc(D) for _ in range(2)]; b_bdbc = [Buf() for _ in range(2)]
        moe_top = top[0]
        bankbf2 = [banks[6][:, :].bitcast(BF16), banks[7][:, :].bitcast(BF16)]

        def load_expert(e):
            bi = e % 2
            for k in range(8):
                DMA_("pool", wgu[bi][:, k, :], wgu_d[e, k * P:(k + 1) * P, :], (), [b_wgu[bi][k]])
            for k in range(8):
                DMA_("pool", wdn[bi][:, k, :], wd_d[e, k * P:(k + 1) * P, :], (), [b_wdn[bi][k]])
            DMA_("sp", bdbc[bi], bd_d[e:e + 1, :].partition_broadcast(P), (), [b_bdbc[bi]])

        splits = [(0, 512), (512, CAP)]
        split_tiles = [[s for s in range(NST) if n0 <= s * P < n1] for (n0, n1) in splits]
        if n_exp > 0:
            load_expert(0)
        cnt = 0
        for e in range(n_exp):
            bi = e % 2
            if e + 1 < n_exp:
                load_expert(e + 1)
            xsT, b_xsT = xsT2[bi], b_xsT2[bi]
            for s in range(NST):
                xm, bxm = xs_sm[s % 2], b_xssm[s % 2]
                DMA_("sp", xm, xs_scr[e * CAP + s * P:e * CAP + (s + 1) * P, :], [b_xs], [bxm])
                bankbf, bbf = bankbf2[s % 2], bank_bufs[6 + s % 2]
                for k in range(8):
                    TR_(bankbf[:, k * P:(k + 1) * P], xm[:, k * P:(k + 1) * P], identb, [bxm, b_identb], [bbf])
                CP_("act" if s % 2 == 0 else "dve", xsT[:, :, s * P:(s + 1) * P], bankbf.rearrange("p (a b) -> p a b", a=8),
                    [bbf], [b_xsT[s]])
            for (n0, n1), tiles in zip(splits, split_tiles):
                w = n1 - n0
                rb = [b_xsT[s] for s in tiles]
                for f in range(8):
                    pgs = []
                    for m in (f, 8 + f):
                        i = bank_rr[0] % 6
                        bank_rr[0] += 1
                        pb, bb = banks[i][:, :], bank_bufs[i]
                        for k in range(8):
                            MM_(pb[:, 0:w], wgu[bi][:, k, m * P:(m + 1) * P], xsT[:, k, n0:n1], k == 0, k == 7,
                                [b_wgu[bi][k]] + rb, [bb])
                        pgs.append((pb, bb))
                    (pg, bpg), (pu, bpu) = pgs
                    gi = cnt % 2
                    cnt += 1
                    gs, us, ss = g_sb[gi][:, 0:w], u_sb[gi][:, 0:w], s_sb[gi][:, 0:w]
                    TS_("dve", gs, pg[:, 0:w], bgu_sb[:, e * 16 + f:e * 16 + f + 1], 7.0, ALU.add, ALU.min, [bpg, b_bgu], [b_g[gi]])
                    ACT_(ss, gs, AF.Sigmoid, [b_g[gi]], [b_s[gi]], scale=1.702)
                    TS_("dve", us, pu[:, 0:w], bgu_sb[:, e * 16 + 8 + f:e * 16 + 9 + f], 7.0, ALU.add, ALU.min, [bpu, b_bgu], [b_us[gi]])
                    TS_("dve", us, us, -7.0, 1.0, ALU.max, ALU.add, [b_us[gi]], [b_us[gi]])
                    TT_("pool", gs, gs, ss, ALU.mult, [b_g[gi], b_s[gi]], [b_g[gi]])
                    TT_("pool", actT[:, f, n0:n1], gs, us, ALU.mult, [b_g[gi], b_us[gi]], [b_actT])
            for s in range(NST):
                eo, beo = eo_sb[s % 2], b_eosb[s % 2]
                for h in range(2):
                    i = bank_rr[0] % 6
                    bank_rr[0] += 1
                    pb, bb = banks[i][:, :], bank_bufs[i]
                    for f in range(8):
                        MM_(pb, actT[:, f, s * P:(s + 1) * P], wdn[bi][:, f, h * 512:(h + 1) * 512], f == 0, f == 7,
                            [b_actT, b_wdn[bi][f]], [bb])
                    TT_("dve", eo[:, h * 512:(h + 1) * 512], pb, bdbc[bi][:, h * 512:(h + 1) * 512], ALU.add, [bb, b_bdbc[bi]], [beo])
                DMA_("sp", eo_scr[e * CAP + s * P:e * CAP + (s + 1) * P, :], eo, [beo], [b_eo])

        S.barrier()

        top[0] = persist_top
        NB4 = 3
        eo4 = [alloc(4 * D, F32, (4, D)) for _ in range(NB4)]; b_eo4 = [Buf() for _ in range(NB4)]
        x1c = [alloc(D) for _ in range(NB4)]; b_x1c = [Buf() for _ in range(NB4)]
        accd = [alloc(D) for _ in range(2)]; b_accd = [Buf() for _ in range(2)]
        lnt2 = (alloc(12, F32, (2, 6)), alloc(2), alloc(1)); b_lnt2 = Buf()
        n_ch = n_tiles * 4

        def fetch(ci):
            e4, be4 = eo4[ci % NB4], b_eo4[ci % NB4]
            for k in range(4):
                S.op("pool", lambda e, ci=ci, k=k, e4=e4: e.indirect_dma_start(
                    out=e4[:, k, :], out_offset=None, in_=eo_scr,
                    in_offset=bass.IndirectOffsetOnAxis(ap=dst_all[:, ci, k:k + 1], axis=0)),
                    [b_eo, b_dst_all[ci]], [be4], dma=True)
            DMA_("sp", x1c[ci % NB4], x1_scr[ci * P:(ci + 1) * P, :], [b_x1scr[ci]], [b_x1c[ci % NB4]])

        for ci in range(min(NB4 - 1, n_ch)):
            fetch(ci)
        for ci in range(n_ch):
            if ci + NB4 - 1 < n_ch:
                fetch(ci + NB4 - 1)
            e4, be4 = eo4[ci % NB4], b_eo4[ci % NB4]
            xx, bxx = x1c[ci % NB4], b_x1c[ci % NB4]
            a, ba = accd[ci % 2], b_accd[ci % 2]
            TS_("dve", a, e4[:, 0, :], gate_all[:, ci, 0:1], None, ALU.mult, None, [be4, b_gate_all[ci]], [ba])
            for k in range(1, 4):
                STT_("dve", a, e4[:, k, :], gate_all[:, ci, k:k + 1], a, ALU.mult, ALU.add, [be4, b_gate_all[ci], ba], [ba])
            STT_("dve", a, xx, ALPHA, a, ALU.mult, ALU.add, [bxx, ba], [ba])
            layer_norm(a, ba, lnC_g, lnC_b, b_lnC, a, ba, lnt2, b_lnt2)
            out_dmas.append(DMA_("sp", out_d[ci * P:(ci + 1) * P, :], a, [ba], ()))

        S.emit_all(final_waits=[("sp", d) for d in out_dmas])
    return nc


def _host_layout(inp, b):
    f = np.float32
    w_in = inp["w_in"][0]
    cb, cc, ch, gu, gv, q, gates = np.split(w_in, np.cumsum([1024, 1024, 1024, 1024, 1024, 1024])[:], axis=1)
    segs = [inp["w_kv"][0], gv, gu]
    for c in range(8):
        sl = slice(c * 128, (c + 1) * 128)
        segs += [cb[:, sl], cc[:, sl], ch[:, sl]]
    segs.append(q)
    wcp, wgp, wxp = inp["w_conv_proj"][0], inp["w_gmlp_proj"][0], inp["w_xa_proj"][0]
    for dc in range(8):
        sl = slice(dc * 128, (dc + 1) * 128)
        segs += [gates[:, 0:1024][:, sl], gates[:, 1024:2048][:, sl], gates[:, 2048:3072][:, sl], wcp[:, sl], wgp[:, sl], wxp[:, sl]]
    segs.append(inp["w_out"][0])
    wmix = np.ascontiguousarray(np.concatenate(segs, axis=1), dtype=f)
    assert wmix.shape == (D, WCOLS)
    shared = {
        "wmix": wmix,
        "b_gate_T": np.ascontiguousarray(inp["b_gate"][0].reshape(24, 128).T, dtype=f),
        "conv_w_T": np.ascontiguousarray(inp["conv_w"][0].reshape(3, 8, 128).transpose(2, 0, 1).reshape(128, 24), dtype=f),
        "gmlp_wsT": np.ascontiguousarray(inp["gmlp_ws"][0].transpose(2, 0, 1).reshape(128, 8 * 128), dtype=f),
        "gmlp_b": np.ascontiguousarray(inp["gmlp_b"][0].reshape(1, D), dtype=f),
        "gmlp_ln_g": np.ascontiguousarray(inp["gmlp_ln_g"], dtype=f), "gmlp_ln_b": np.ascontiguousarray(inp["gmlp_ln_b"], dtype=f),
        "mem_ln_g": np.ascontiguousarray(inp["mem_ln_g"], dtype=f), "mem_ln_b": np.ascontiguousarray(inp["mem_ln_b"], dtype=f),
        "ln1_g": np.ascontiguousarray(inp["ln1_g"], dtype=f), "ln1_b": np.ascontiguousarray(inp["ln1_b"], dtype=f),
        "ln2_g": np.ascontiguousarray(inp["ln2_g"], dtype=f), "ln2_b": np.ascontiguousarray(inp["ln2_b"], dtype=f),
        "w_router": np.ascontiguousarray(inp["w_router"][0], dtype=f),
        "b_router": np.ascontiguousarray(inp["b_router"], dtype=f),
        "w_gate_up": np.ascontiguousarray(inp["w_gate_up"][0], dtype=f),
        "b_gate_up_T": np.ascontiguousarray(inp["b_gate_up"][0].reshape(32, 16, 128).transpose(2, 0, 1).reshape(128, 512), dtype=f),
        "w_down": np.ascontiguousarray(inp["w_down"][0], dtype=f),
        "b_down": np.ascontiguousarray(inp["b_down"][0], dtype=f),
    }
    return shared


def _core_inputs(inp, shared, b):
    m = dict(shared)
    xb = np.ascontiguousarray(inp["x"][b], dtype=np.float32)
    m["x"] = xb
    m["xT"] = np.ascontiguousarray(xb.T)
    m["mem"] = np.ascontiguousarray(inp["mem"][b], dtype=np.float32)
    return m


def kernel(**inputs):
    inp = {k: np.asarray(v) for k, v in inputs.items()}
    nb = inp["x"].shape[0]
    shared = _host_layout(inp, 0)
    in_maps = [_core_inputs(inp, shared, b) for b in range(nb)]
    nc = build()
    res = run_bass_kernel_spmd(nc, in_maps, core_ids=list(range(nb)))
    out = np.stack([np.asarray(r["out"]) for r in res.results], axis=0)
    return out.astype(np.float32, copy=False)
```

```python
import contextlib
import numpy as np
import concourse.bass as bass
import concourse.mybir as mybir
from concourse.bass_utils import run_bass_kernel_spmd

F32 = mybir.dt.float32
BF16 = mybir.dt.bfloat16
I32 = mybir.dt.int32
U32 = mybir.dt.uint32
AF = mybir.ActivationFunctionType
ALU = mybir.AluOpType

ENGS = ("pe", "act", "dve", "pool", "sp")


class Buf:
    __slots__ = ("name", "writers", "readers")

    def __init__(self, name=""):
        self.name = name
        self.writers = []
        self.readers = []


class Op:
    __slots__ = ("eng", "emit", "deps", "is_dma", "signal", "val", "sem", "idx")

    def __init__(self, eng, emit, is_dma):
        self.eng = eng
        self.emit = emit
        self.deps = []
        self.is_dma = is_dma
        self.signal = False
        self.val = None
        self.sem = None
        self.idx = None


class Sched:
    def __init__(self, nc, n_dma_sems=None):
        self.nc = nc
        self.streams = {e: [] for e in ENGS}
        self.n_dma_sems = n_dma_sems or {"sp": 8, "act": 2, "pool": 4}
        self.last = {}
        self.dmas_since_barrier = []

    @staticmethod
    def _same_inorder(a, b):
        return (not a.is_dma) and (not b.is_dma) and a.eng == b.eng

    def op(self, eng, emit, reads=(), writes=(), dma=False, extra_deps=()):
        o = Op(eng, emit, dma)
        o.idx = len(self.streams[eng])
        deps = list(extra_deps)
        for b in reads:
            deps.extend(b.writers)
        for b in writes:
            deps.extend(b.readers)
            deps.extend(b.writers)
        seen = set()
        for d in deps:
            if d is o or id(d) in seen:
                continue
            seen.add(id(d))
            if eng == "pe" and d.eng == "pe" and not d.is_dma and not dma:
                continue
            o.deps.append(d)
            d.signal = True
        for b in reads:
            b.readers = [r for r in b.readers if not self._same_inorder(r, o)] + [o]
        for b in writes:
            if b.readers:
                b.readers = []
                b.writers = []
            b.writers = [w for w in b.writers if not self._same_inorder(w, o)] + [o]
        self.streams[eng].append(o)
        if dma:
            self.dmas_since_barrier.append(o)
        else:
            self.last[eng] = o
        return o

    def barrier(self):
        deps = list(self.last.values()) + list(self.dmas_since_barrier)
        self.dmas_since_barrier = []
        for e in ENGS:
            self.op(e, lambda eng: eng.nop(), extra_deps=deps)

    def emit_all(self, final_waits=()):
        nc = self.nc
        with contextlib.ExitStack() as st:
            csem = {e: st.enter_context(nc.semaphore("c_" + e)) for e in ("pe", "act", "dve", "pool", "sp")}
            dsems = {e: [st.enter_context(nc.semaphore("d_%s%d" % (e, i))) for i in range(n)]
                     for e, n in self.n_dma_sems.items()}
            for e in ENGS:
                cnt = 0
                nd = self.n_dma_sems.get(e, 0)
                dcnt = [0] * max(nd, 1)
                rr = 0
                for o in self.streams[e]:
                    if o.is_dma:
                        s = rr % nd
                        rr += 1
                        dcnt[s] += 1
                        o.sem = dsems[e][s]
                        o.val = 16 * dcnt[s]
                    elif o.signal:
                        cnt += 1
                        o.sem = csem[e]
                        o.val = cnt
            fw = {}
            for (e, d) in final_waits:
                fw.setdefault(e, []).append(d)
            block = st.enter_context(nc.Block())
            streams = self.streams

            def make(e):
                def body(eng):
                    seen = {}

                    def wait(sem, val):
                        k = id(sem)
                        if seen.get(k, 0) < val:
                            eng.wait_ge(sem, val)
                            seen[k] = val

                    for o in streams[e]:
                        if o.is_dma and o.val > 16:
                            wait(o.sem, o.val - 16)
                        mx = {}
                        for d in o.deps:
                            k = id(d.sem)
                            if k not in mx or mx[k][1] < d.val:
                                mx[k] = (d.sem, d.val)
                        for sem_, val_ in mx.values():
                            wait(sem_, val_)
                        ins = o.emit(eng)
                        if o.is_dma:
                            ins.then_inc(o.sem, 16)
                        elif o.signal:
                            ins.then_inc(o.sem, 1)
                    for d in fw.get(e, ()):
                        wait(d.sem, d.val)
                return body

            for e, deco in (("pe", block.tensor), ("act", block.scalar), ("dve", block.vector),
                            ("pool", block.gpsimd), ("sp", block.sync)):
                if streams[e] or e in fw:
                    deco(make(e))


P = 128
D = 1024
SEQ = 4096
TT = 512
NCH = SEQ // P
NE = 32
CAP = 768
NST = CAP // P
NROWS = NE * CAP
ZROW = NROWS
ALPHA = 2.0 ** 0.25
EPS = 1e-5
SEG_KV = 0
SEG_A = 2048
SEG_B = SEG_A + 1024
SEG_C = SEG_B + 1024
SEG_D = SEG_C + 3072
SEG_E = SEG_D + 1024
SEG_F = SEG_E + 8 * 768
WCOLS = SEG_F + 1024


def build(n_tiles=SEQ // TT, n_exp=NE, debug=False):
    nc = bass.Bass("TRN2", target_bir_lowering=False)

    def din(name, shape, dt=F32):
        return nc.dram_tensor(name, list(shape), dt, kind="ExternalInput").ap()

    xT_d = din("xT", [D, SEQ])
    x_d = din("x", [SEQ, D])
    mem_d = din("mem", [256, D])
    wmix_d = din("wmix", [D, WCOLS])
    bgate_d = din("b_gate_T", [P, 24])
    convw_d = din("conv_w_T", [P, 24])
    wsT_d = din("gmlp_wsT", [P, 8 * P])
    gmlpb_d = din("gmlp_b", [1, D])
    glng_d = din("gmlp_ln_g", [1, D])
    glnb_d = din("gmlp_ln_b", [1, D])
    mlng_d = din("mem_ln_g", [1, D])
    mlnb_d = din("mem_ln_b", [1, D])
    l1g_d = din("ln1_g", [1, D])
    l1b_d = din("ln1_b", [1, D])
    l2g_d = din("ln2_g", [1, D])
    l2b_d = din("ln2_b", [1, D])
    wr_d = din("w_router", [D, NE])
    br_d = din("b_router", [1, NE])
    wgu_d = din("w_gate_up", [NE, D, 2 * D])
    bgu_d = din("b_gate_up_T", [P, NE * 16])
    wd_d = din("w_down", [NE, D, D])
    bd_d = din("b_down", [NE, D])
    out_d = nc.dram_tensor("out", [SEQ, D], F32, kind="ExternalOutput").ap()
    if debug:
        x1_scr = nc.dram_tensor("x1_dbg", [SEQ, D], F32, kind="ExternalOutput").ap()
    else:
        x1_scr = nc.dram_tensor("x1_scr", [SEQ, D], F32).ap()
    wmix_bf = nc.dram_tensor("wmix_bf", [D, WCOLS], BF16).ap()
    xs_scr = nc.dram_tensor("xs_scr", [NROWS, D], BF16).ap()
    eo_scr = nc.dram_tensor("eo_scr", [NROWS + 1, D], F32).ap()

    S = Sched(nc)
    out_dmas = []

    with contextlib.ExitStack() as st:
        NF = 52224
        big = st.enter_context(nc.sbuf_tensor("arena", [P, NF], F32))
        banks = [st.enter_context(nc.psum_tensor("bank%d" % i, [P, 512], F32)) for i in range(8)]
        bank_bufs = [Buf("bank%d" % i) for i in range(8)]
        bank_rr = [0]

        def bank():
            i = bank_rr[0] % 8
            bank_rr[0] += 1
            return banks[i][:, :], bank_bufs[i]

        top = [0]

        def alloc(ncols, dt=F32, shape=None):
            a = big[:, top[0]:top[0] + ncols]
            top[0] += ncols
            assert top[0] <= NF, "SBUF arena overflow %d" % top[0]
            if dt != F32:
                a = a.bitcast(dt)
            if shape is not None:
                names = " ".join("d%d" % i for i in range(len(shape)))
                kw = {"d%d" % i: s for i, s in enumerate(shape)}
                a = a.rearrange("p (%s) -> p %s" % (names, names), **kw)
            return a

        def TT_(eng, out, in0, in1, op, R, W):
            return S.op(eng, lambda e: e.tensor_tensor(out=out, in0=in0, in1=in1, op=op), R, W)

        def TS_(eng, out, in0, s1, s2, op0, op1, R, W):
            if op1 is None:
                return S.op(eng, lambda e: e.tensor_scalar(out=out, in0=in0, scalar1=s1, scalar2=None, op0=op0), R, W)
            return S.op(eng, lambda e: e.tensor_scalar(out=out, in0=in0, scalar1=s1, scalar2=s2, op0=op0, op1=op1), R, W)

        def STT_(eng, out, in0, sc, in1, op0, op1, R, W, accum=None):
            if accum is None:
                return S.op(eng, lambda e: e.scalar_tensor_tensor(out=out, in0=in0, scalar=sc, in1=in1, op0=op0, op1=op1), R, W)
            return S.op(eng, lambda e: e.scalar_tensor_tensor(out=out, in0=in0, scalar=sc, in1=in1, op0=op0, op1=op1,
                                                              accum_out=accum), R, W)

        def ACT_(out, in_, func, R, W, bias=None, scale=1.0, accum=None):
            kw = {}
            if bias is not None:
                kw["bias"] = bias
            if accum is not None:
                kw["accum_out"] = accum
            return S.op("act", lambda e: e.activation(out=out, in_=in_, func=func, scale=scale, **kw), R, W)

        def CP_(eng, out, in_, R, W):
            if eng == "act":
                return S.op("act", lambda e: e.copy(out=out, in_=in_), R, W)
            return S.op(eng, lambda e: e.tensor_copy(out=out, in_=in_), R, W)

        def MS_(eng, ap, val, W):
            return S.op(eng, lambda e: e.memset(ap, val), (), W)

        def MM_(out, lhsT, rhs, start, stop, R, W):
            return S.op("pe", lambda e: e.matmul(out, lhsT=lhsT, rhs=rhs, start=start, stop=stop), R, W)

        def TR_(out, in_, ident, R, W):
            return S.op("pe", lambda e: e.transpose(out=out, in_=in_, identity=ident), R, W)

        def DMA_(eng, out, in_, R, W):
            return S.op(eng, lambda e: e.dma_start(out=out, in_=in_), R, W, dma=True)

        def layer_norm(xin, b_x, g_bc, b_bc, b_par, out, b_out, tmp, b_tmp, aff="pool"):
            st6, mv, rstd = tmp
            for h in range(2):
                S.op("dve", lambda e, h=h: e.bn_stats(out=st6[:, h, :], in_=xin[:, h * 512:(h + 1) * 512]), [b_x], [b_tmp])
            S.op("dve", lambda e: e.bn_aggr(out=mv, in_=st6.rearrange("p a b -> p (a b)")), [b_tmp], [b_tmp])
            ACT_(rstd, mv[:, 1:2], AF.Sqrt, [b_tmp, b_eps], [b_tmp], bias=eps_t)
            S.op("dve", lambda e: e.reciprocal(out=rstd, in_=rstd), [b_tmp], [b_tmp])
            TS_("dve", xin, xin, mv[:, 0:1], rstd, ALU.subtract, ALU.mult, [b_x, b_tmp], [b_x])
            TT_(aff, xin, xin, g_bc, ALU.mult, [b_x, b_par], [b_x])
            TT_(aff, out, xin, b_bc, ALU.add, [b_x, b_par], [b_out])

        identf = alloc(128); b_identf = Buf()
        identb = alloc(64, BF16); b_identb = Buf()
        onesb = alloc(64, BF16); b_onesb = Buf()
        ustrb = alloc(64, BF16); b_ustr = Buf()
        eps_t = alloc(1); b_eps = Buf()
        iota32 = alloc(32); b_iota = Buf()
        wr_sb = alloc(8 * NE, F32, (8, NE)); b_wr = Buf()
        br_bc = alloc(NE); b_br = Buf()
        gate_all = alloc(NCH * 4, F32, (NCH, 4)); b_gate_all = [Buf() for _ in range(NCH)]
        dst_all = alloc(NCH * 4, I32, (NCH, 4)); b_dst_all = [Buf() for _ in range(NCH)]
        tot_bc = alloc(NE); b_tot = Buf()
        bgu_sb = alloc(NE * 16); b_bgu = Buf()
        lnC_g = alloc(D); lnC_b = alloc(D); b_lnC = Buf()
        zrow = alloc(D); b_zrow = Buf()
        lnt = (alloc(12, F32, (2, 6)), alloc(2), alloc(1)); b_lnt = Buf()
        persist_top = top[0]
        WcT = alloc(512, BF16, (8, P)); b_WcT = Buf()
        biasbc = alloc(D, F32, (8, P)); b_biasbc = Buf()
        KT = alloc(1024, BF16, (8, 256)); b_KT = Buf()
        Vt = alloc(1024, BF16, (2, D)); b_Vt = Buf()
        bgate = alloc(24); b_bgate = Buf()
        convw = alloc(24); b_convw = Buf()
        halo = alloc(16, F32, (8, 2)); b_halo = [Buf() for _ in range(8)]
        gln_g = alloc(D); gln_b = alloc(D); b_gln = Buf()
        l1_g = alloc(D); l1_b = alloc(D); b_l1 = Buf()
        mixer_base = top[0]

        MS_("pool", identf, 0.0, [b_identf])
        S.op("pool", lambda e: e.affine_select(out=identf, in_=identf, pattern=[[-1, P]], compare_op=ALU.not_equal,
                                               fill=1.0, base=0, channel_multiplier=1), [b_identf], [b_identf])
        CP_("dve", identb, identf, [b_identf], [b_identb])
        MS_("pool", onesb, 1.0, [b_onesb])
        ustrf = alloc(128); b_ustrf = Buf()
        MS_("pool", ustrf, 1.0, [b_ustrf])
        S.op("pool", lambda e: e.affine_select(out=ustrf, in_=ustrf, pattern=[[1, P]], compare_op=ALU.is_gt,
                                               fill=0.0, base=0, channel_multiplier=-1), [b_ustrf], [b_ustrf])
        CP_("dve", ustrb, ustrf, [b_ustrf], [b_ustr])
        MS_("pool", eps_t, EPS, [b_eps])
        S.op("pool", lambda e: e.iota(iota32, pattern=[[1, NE]], base=0, channel_multiplier=0,
                                      allow_small_or_imprecise_dtypes=True), (), [b_iota])
        MS_("pool", tot_bc, 0.0, [b_tot])
        MS_("pool", halo.rearrange("p a b -> p (a b)"), 0.0, b_halo)
        MS_("pool", zrow, 0.0, [b_zrow])
        b_eo = Buf("eo_scr")
        DMA_("sp", eo_scr[ZROW:ZROW + 1, :], zrow[0:1, :], [b_zrow], [b_eo])
        DMA_("sp", wr_sb, wr_d.rearrange("(k p) n -> p k n", p=P), (), [b_wr])
        DMA_("sp", br_bc, br_d.partition_broadcast(P), (), [b_br])
        DMA_("sp", bgu_sb, bgu_d, (), [b_bgu])
        DMA_("sp", lnC_g, mlng_d.partition_broadcast(P), (), [b_lnC])
        DMA_("sp", lnC_b, mlnb_d.partition_broadcast(P), (), [b_lnC])
        DMA_("sp", biasbc.rearrange("p a b -> p (a b)"), gmlpb_d.partition_broadcast(P), (), [b_biasbc])
        DMA_("sp", bgate, bgate_d, (), [b_bgate])
        DMA_("sp", convw, convw_d, (), [b_convw])
        DMA_("sp", gln_g, glng_d.partition_broadcast(P), (), [b_gln])
        DMA_("sp", gln_b, glnb_d.partition_broadcast(P), (), [b_gln])
        DMA_("sp", l1_g, l1g_d.partition_broadcast(P), (), [b_l1])
        DMA_("sp", l1_b, l1b_d.partition_broadcast(P), (), [b_l1])
        wsTf = alloc(8 * P, F32, (8, P)); b_wsTf = Buf()
        DMA_("sp", wsTf.rearrange("p a b -> p (a b)"), wsT_d, (), [b_wsTf])
        S.op("pool", lambda e: e.affine_select(out=wsTf, in_=wsTf, pattern=[[0, 8], [1, P]], compare_op=ALU.is_ge,
                                               fill=0.0, base=0, channel_multiplier=-1), [b_wsTf], [b_wsTf])
        CP_("dve", WcT, wsTf, [b_wsTf], [b_WcT])
        const_top = top[0]

        NSLOT = 4
        ring = [alloc(2048, BF16, (8, 512)) for _ in range(NSLOT)]
        ring_bufs = [Buf("slab%d" % i) for i in range(NSLOT)]
        b_wmixbf = [Buf("wmixbf%d" % i) for i in range(WCOLS // 512)]

        for blk in range(WCOLS // 512):
            sl = blk % NSLOT
            c0 = blk * 512
            DMA_("pool", ring[sl], wmix_d[:, c0:c0 + 512].rearrange("(k p) n -> p k n", p=P), (), [ring_bufs[sl]])
            DMA_("sp", wmix_bf[:, c0:c0 + 512].rearrange("(k p) n -> p k n", p=P), ring[sl], [ring_bufs[sl]], [b_wmixbf[blk]])

        slab_list = []
        for c in range(4):
            slab_list.append((SEG_KV + c * 512, 512))
        for _ in range(n_tiles):
            slab_list += [(SEG_A, 512), (SEG_A + 512, 512), (SEG_B, 512), (SEG_B + 512, 512)]
            slab_list += [(SEG_C + c * 384, 384) for c in range(8)]
            slab_list += [(SEG_D, 512), (SEG_D + 512, 512)]
            for dc in range(8):
                slab_list += [(SEG_E + dc * 768, 384), (SEG_E + dc * 768 + 384, 384)]
            slab_list += [(SEG_F, 512), (SEG_F + 512, 512)]
        slab_issued = [0]
        slab_next = [0]

        def issue_slab():
            i = slab_issued[0]
            if i >= len(slab_list):
                return
            c0, w = slab_list[i]
            sl = i % NSLOT
            rb = [b_wmixbf[b] for b in range(c0 // 512, (c0 + w - 1) // 512 + 1)]
            DMA_("sp", ring[sl][:, :, 0:w], wmix_bf[:, c0:c0 + w].rearrange("(k p) n -> p k n", p=P), rb, [ring_bufs[sl]])
            slab_issued[0] += 1

        def get_slab():
            i = slab_next[0]
            slab_next[0] += 1
            while slab_issued[0] < min(i + NSLOT - 1, len(slab_list)):
                issue_slab()
            sl = i % NSLOT
            return ring[sl], ring_bufs[sl]

        memx = [alloc(D) for _ in range(2)]; b_memx = [Buf() for _ in range(2)]
        memnT = alloc(1024, BF16, (8, 256)); b_memnT = Buf()
        for j in range(2):
            DMA_("sp", memx[j], mem_d[j * P:(j + 1) * P, :], (), [b_memx[j]])
            layer_norm(memx[j], b_memx[j], lnC_g, lnC_b, b_lnC, memx[j], b_memx[j], lnt, b_lnt)
            for hh in range(2):
                pb, bb = bank()
                for kk in range(4):
                    k = hh * 4 + kk
                    TR_(pb[:, kk * P:(kk + 1) * P], memx[j][:, k * P:(k + 1) * P], identf, [b_memx[j], b_identf], [bb])
                CP_("act", memnT[:, hh * 4:(hh + 1) * 4, j * P:(j + 1) * P], pb.rearrange("p (a b) -> p a b", a=4), [bb], [b_memnT])
        for c in range(2):
            slab, sb_ = get_slab()
            for dd in range(4):
                dchunk = c * 4 + dd
                pb, bb = bank()
                for k in range(8):
                    MM_(pb[:, 0:256], slab[:, k, dd * P:(dd + 1) * P], memnT[:, k, :], k == 0, k == 7, [sb_, b_memnT], [bb])
                CP_("act", KT[:, dchunk, :], pb[:, 0:256], [bb], [b_KT])
        for c in range(2):
            slab, sb_ = get_slab()
            for mc in range(2):
                pb, bb = bank()
                for k in range(8):
                    MM_(pb, memnT[:, k, mc * P:(mc + 1) * P], slab[:, k, :], k == 0, k == 7, [sb_, b_memnT], [bb])
                CP_("dve", Vt[:, mc, c * 512:(c + 1) * 512], pb, [bb], [b_Vt])

        top[0] = const_top + NSLOT * 2048
        xT_bf = alloc(2048, BF16, (8, TT)); b_xT = Buf()
        vn_mg = alloc(2048, BF16)
        v_n = vn_mg.rearrange("p (a b) -> p a b", a=4)
        merged = vn_mg.rearrange("p (a b) -> p a b", a=8)
        b_vnmg = Buf()
        vg = [alloc(D) for _ in range(2)]; b_vg = [Buf() for _ in range(2)]
        ug = [alloc(TT) for _ in range(2)]; b_ug = [Buf() for _ in range(2)]
        ftmp = [alloc(TT) for _ in range(2)]; b_ftmp = [Buf() for _ in range(2)]
        y_conv = alloc(2048, BF16, (8, TT)); b_yconv = Buf()
        y_gmlp = alloc(2048, BF16, (8, TT)); b_ygmlp = Buf()
        y_xa = alloc(2048, BF16, (8, TT)); b_yxa = Buf()
        cc_sb = alloc(TT); b_cc = Buf()
        u_t = [alloc(TT + 4) for _ in range(2)]; b_u = [Buf() for _ in range(2)]
        acc = alloc(TT); b_acc = Buf()
        qT = [alloc(512, BF16, (2, TT)) for _ in range(2)]; b_qT = [Buf() for _ in range(2)]
        PT = [alloc(512, BF16, (2, TT)) for _ in range(2)]; b_PT = [Buf() for _ in range(2)]
        rden = [alloc(TT) for _ in range(2)]; b_rden = [Buf() for _ in range(2)]
        sig = [alloc(3 * TT, F32, (3, TT)) for _ in range(2)]; b_sig = [Buf() for _ in range(2)]
        mt = alloc(3 * TT, F32, (3, TT)); b_mt = Buf()
        xc = [alloc(D) for _ in range(2)]; b_xc = [Buf() for _ in range(2)]
        r2 = [alloc(D) for _ in range(2)]; b_r2 = [Buf() for _ in range(2)]
        x1bf = [alloc(512, BF16) for _ in range(2)]; b_x1bf = [Buf() for _ in range(2)]
        x1T = alloc(D, F32, (8, P)); b_x1T = Buf()
        lgt = alloc(NE); mx8 = alloc(8); mi8 = alloc(8, U32); ef4 = alloc(4); negmax = alloc(1)
        ex4 = alloc(4); ssum = alloc(1); Mbf = alloc(NE // 2, BF16); pos = alloc(NE); junk = alloc(NE)
        pos4 = alloc(4); dstf = alloc(4); keep = alloc(4)
        b_rt = Buf()
        b_x1scr = [Buf() for _ in range(NCH)]
        b_xs = Buf("xs_scr")
        bc_reg = {}

        S.barrier()

        for ti in range(n_tiles):
            t0 = ti * TT
            if ti == 0:
                DMA_("pool", xT_bf, xT_d[:, t0:t0 + TT].rearrange("(k p) t -> p k t", p=P), (), [b_xT])

            slabA = [get_slab(), get_slab()]
            for j in range(4):
                vgj, bvg = vg[j % 2], b_vg[j % 2]
                for h in range(2):
                    pb, bb = bank()
                    sl, sb_ = slabA[h]
                    for k in range(8):
                        MM_(pb, xT_bf[:, k, j * P:(j + 1) * P], sl[:, k, :], k == 0, k == 7, [b_xT, sb_], [bb])
                    ACT_(vgj[:, h * 512:(h + 1) * 512], pb, AF.Gelu_apprx_tanh, [bb], [bvg])
                layer_norm(vgj, bvg, gln_g, gln_b, b_gln, v_n[:, j, :], b_vnmg, lnt, b_lnt)

            slabB = [get_slab(), get_slab()]
            for g in range(8):
                sl, sb_ = slabB[g // 4]
                gg = g % 4
                pu, bpu = bank()
                for k in range(8):
                    MM_(pu, sl[:, k, gg * P:(gg + 1) * P], xT_bf[:, k, :], k == 0, k == 7, [sb_, b_xT], [bpu])
                ACT_(ug[g % 2], pu, AF.Gelu_apprx_tanh, [bpu], [b_ug[g % 2]])
                pf, bpf = bank()
                for j in range(4):
                    MM_(pf[:, j * P:(j + 1) * P], v_n[:, j, g * P:(g + 1) * P], WcT[:, g, :], True, True, [b_vnmg, b_WcT], [bpf])
                TT_("dve", ftmp[g % 2].rearrange("p (j t) -> p j t", j=4), pf.rearrange("p (j t) -> p j t", j=4),
                    biasbc[:, g, :].unsqueeze(1).broadcast_to([P, 4, P]), ALU.add, [bpf, b_biasbc], [b_ftmp[g % 2]])
                TT_("pool", y_gmlp[:, g, :], ftmp[g % 2], ug[g % 2], ALU.mult, [b_ftmp[g % 2], b_ug[g % 2]], [b_ygmlp])

            for c in range(8):
                sl, sb_ = get_slab()
                pcs = []
                for r in range(3):
                    pb, bb = bank()
                    for k in range(8):
                        MM_(pb, sl[:, k, r * P:(r + 1) * P], xT_bf[:, k, :], k == 0, k == 7, [sb_, b_xT], [bb])
                    pcs.append((pb, bb))
                (pcb, bcb), (pcc, bcc), (pch, bch) = pcs
                u, bu = u_t[c % 2], b_u[c % 2]
                CP_("act", cc_sb, pcc, [bcc], [b_cc])
                CP_("pool", u[:, 0:2], halo[:, c, :], [b_halo[c]], [bu])
                TT_("dve", u[:, 2:TT + 2], cc_sb, pch, ALU.mult, [b_cc, bch], [bu])
                CP_("pool", halo[:, c, :], u[:, TT:TT + 2], [bu], [b_halo[c]])
                TS_("dve", acc, u[:, 2:TT + 2], convw[:, 16 + c:17 + c], None, ALU.mult, None, [bu, b_convw], [b_acc])
                STT_("dve", acc, u[:, 1:TT + 1], convw[:, 8 + c:9 + c], acc, ALU.mult, ALU.add, [bu, b_convw, b_acc], [b_acc])
                STT_("dve", acc, u[:, 0:TT], convw[:, c:c + 1], acc, ALU.mult, ALU.add, [bu, b_convw, b_acc], [b_acc])
                TT_("dve", y_conv[:, c, :], acc, pcb, ALU.mult, [b_acc, bcb], [b_yconv])

            slabD = [get_slab(), get_slab()]
            for h in range(4):
                sl, sb_ = slabD[h // 2]
                q, bq = qT[h % 2], b_qT[h % 2]
                for dc in range(2):
                    cc = (h % 2) * 2 + dc
                    pb, bb = bank()
                    for k in range(8):
                        MM_(pb, sl[:, k, cc * P:(cc + 1) * P], xT_bf[:, k, :], k == 0, k == 7, [sb_, b_xT], [bb])
                    CP_("act", q[:, dc, :], pb, [bb], [bq])
                pt, bpt = PT[h % 2], b_PT[h % 2]
                for mc in range(2):
                    pb, bb = bank()
                    for dc in range(2):
                        MM_(pb, KT[:, h * 2 + dc, mc * P:(mc + 1) * P], q[:, dc, :], dc == 0, dc == 1, [b_KT, bq], [bb])
                    ACT_(pt[:, mc, :], pb, AF.Exp, [bb], [bpt], scale=0.0625)
                pden, bden = bank()
                for mc in range(2):
                    MM_(pden, onesb, pt[:, mc, :], mc == 0, mc == 1, [b_onesb, bpt], [bden])
                rd, brd = rden[h % 2], b_rden[h % 2]
                S.op("dve", lambda e, rd=rd, pden=pden: e.reciprocal(out=rd, in_=pden), [bden], [brd])
                for dc in range(2):
                    po, bpo = bank()
                    for mc in range(2):
                        MM_(po, Vt[:, mc, (h * 2 + dc) * P:(h * 2 + dc + 1) * P], pt[:, mc, :], mc == 0, mc == 1, [b_Vt, bpt], [bpo])
                    TT_("dve", y_xa[:, h * 2 + dc, :], rd, po, ALU.mult, [brd, bpo], [b_yxa])

            ys = [(y_conv, b_yconv), (y_gmlp, b_ygmlp), (y_xa, b_yxa)]
            for dc in range(8):
                slg, sbg = get_slab()
                slp, sbp = get_slab()
                sg, bsg = sig[dc % 2], b_sig[dc % 2]
                for r in range(3):
                    pb, bb = bank()
                    for k in range(8):
                        MM_(pb, slg[:, k, r * P:(r + 1) * P], xT_bf[:, k, :], k == 0, k == 7, [sbg, b_xT], [bb])
                    ACT_(sg[:, r, :], pb, AF.Sigmoid, [bb, b_bgate], [bsg], bias=bgate[:, r * 8 + dc:r * 8 + dc + 1])
                for r in range(3):
                    pb, bb = bank()
                    yr, byr = ys[r]
                    for k in range(8):
                        MM_(pb, slp[:, k, r * P:(r + 1) * P], yr[:, k, :], k == 0, k == 7, [sbp, byr], [bb])
                    TT_("dve", mt[:, r, :], sg[:, r, :], pb, ALU.mult, [bsg, bb], [b_mt])
                TT_("pool", mt[:, 0, :], mt[:, 0, :], mt[:, 1, :], ALU.add, [b_mt], [b_mt])
                TT_("pool", merged[:, dc, :], mt[:, 0, :], mt[:, 2, :], ALU.add, [b_mt], [b_vnmg])

            if ti + 1 < n_tiles:
                DMA_("pool", xT_bf, xT_d[:, t0 + TT:t0 + 2 * TT].rearrange("(k p) t -> p k t", p=P), (), [b_xT])

            slabF = [get_slab(), get_slab()]

            def stage1(j):
                ci = ti * 4 + j
                r_t, b_r = r2[j % 2], b_r2[j % 2]
                xcj, bxc = xc[j % 2], b_xc[j % 2]
                DMA_("sp", xcj, x_d[ci * P:(ci + 1) * P, :], (), [bxc])
                for h in range(2):
                    pb, bb = bank()
                    sl, sb_ = slabF[h]
                    for k in range(8):
                        MM_(pb, merged[:, k, j * P:(j + 1) * P], sl[:, k, :], k == 0, k == 7, [b_vnmg, sb_], [bb])
                    STT_("dve", r_t[:, h * 512:(h + 1) * 512], xcj[:, h * 512:(h + 1) * 512], ALPHA, pb, ALU.mult, ALU.add,
                         [bxc, bb], [b_r])
                layer_norm(r_t, b_r, l1_g, l1_b, b_l1, r_t, b_r, lnt, b_lnt)
                DMA_("sp", x1_scr[ci * P:(ci + 1) * P, :], r_t, [b_r], [b_x1scr[ci]])
                xb, bxb = x1bf[j % 2], b_x1bf[j % 2]
                CP_("act", xb, r_t, [b_r], [bxb])

            def stage2(j):
                ci = ti * 4 + j
                r_t, b_r = r2[j % 2], b_r2[j % 2]
                xb, bxb = x1bf[j % 2], b_x1bf[j % 2]
                for hh in range(2):
                    pb, bb = bank()
                    for kk in range(4):
                        k = hh * 4 + kk
                        TR_(pb[:, kk * P:(kk + 1) * P], r_t[:, k * P:(k + 1) * P], identf, [b_r, b_identf], [bb])
                    CP_("act", x1T[:, hh * 4:(hh + 1) * 4, :], pb.rearrange("p (a b) -> p a b", a=4), [bb], [b_x1T])
                pl, bpl = bank()
                for k in range(8):
                    MM_(pl[:, 0:NE], x1T[:, k, :], wr_sb[:, k, :], k == 0, k == 7, [b_x1T, b_wr], [bpl])
                TT_("dve", lgt, pl[:, 0:NE], br_bc, ALU.add, [bpl, b_br], [b_rt])
                S.op("dve", lambda e: e.max(out=mx8, in_=lgt), [b_rt], [b_rt])
                S.op("dve", lambda e: e.max_index(out=mi8, in_max=mx8, in_values=lgt), [b_rt], [b_rt])
                TS_("dve", negmax, mx8[:, 0:1], -1.0, None, ALU.mult, None, [b_rt], [b_rt])
                MS_("pool", ssum, 0.0, [b_rt])
                MS_("pool", pos4, 0.0, [b_rt])
                ACT_(ex4, mx8[:, 0:4], AF.Exp, [b_rt], [b_rt], bias=negmax, accum=ssum)
                S.op("dve", lambda e: e.reciprocal(out=ssum, in_=ssum), [b_rt], [b_rt])
                TS_("dve", gate_all[:, ci, :], ex4, ssum, None, ALU.mult, None, [b_rt], [b_gate_all[ci]])
                TS_("dve", Mbf, lgt, mx8[:, 3:4], None, ALU.is_ge, None, [b_rt], [b_rt])
                pp, bpp = bank()
                MM_(pp[:, 0:NE], ustrb, Mbf, True, True, [b_ustr, b_rt], [bpp])
                MM_(pp[:, NE:2 * NE], onesb, Mbf, True, True, [b_onesb, b_rt], [bpp])
                TT_("dve", pos, pp[:, 0:NE], tot_bc, ALU.add, [bpp, b_tot], [b_rt])
                TT_("dve", tot_bc, pp[:, NE:2 * NE], tot_bc, ALU.add, [bpp, b_tot], [b_tot])
                CP_("dve", ef4, mi8[:, 0:4], [b_rt], [b_rt])
                for k in range(4):
                    STT_("dve", junk, iota32, ef4[:, k:k + 1], pos, ALU.is_equal, ALU.mult, [b_iota, b_rt], [b_rt],
                         accum=pos4[:, k:k + 1])
                STT_("dve", dstf, ef4, float(CAP), pos4, ALU.mult, ALU.add, [b_rt], [b_rt])
                TS_("dve", keep, pos4, float(CAP), None, ALU.is_lt, None, [b_rt], [b_rt])
                TS_("dve", dstf, dstf, -float(ZROW), None, ALU.add, None, [b_rt], [b_rt])
                TT_("dve", dstf, dstf, keep, ALU.mult, [b_rt], [b_rt])
                TS_("dve", dstf, dstf, float(ZROW), None, ALU.add, None, [b_rt], [b_rt])
                CP_("dve", dst_all[:, ci, :], dstf, [b_rt], [b_dst_all[ci]])
                for k in range(4):
                    def scat(e, ci=ci, k=k, xb=xb):
                        if "r" not in bc_reg:
                            bc_reg["r"] = e.to_reg(NROWS - 1)
                        return e.indirect_dma_start(
                            out=xs_scr, out_offset=bass.IndirectOffsetOnAxis(ap=dst_all[:, ci, k:k + 1], axis=0),
                            in_=xb, in_offset=None, bounds_check=bc_reg["r"], oob_is_err=False)
                    S.op("pool", scat, [bxb, b_dst_all[ci]], [b_xs], dma=True)

            stage1(0)
            for j in range(4):
                if j + 1 < 4:
                    stage1(j + 1)
                stage2(j)

        S.barrier()

        top[0] = persist_top
        DMA_("sp", lnC_g, l2g_d.partition_broadcast(P), (), [b_lnC])
        DMA_("sp", lnC_b, l2b_d.partition_broadcast(P), (), [b_lnC])
        wgu = [alloc(8192, BF16, (8, 2 * D)) for _ in range(2)]
        b_wgu = [[Buf() for _ in range(8)] for _ in range(2)]
        wdn = [alloc(4096, BF16, (8, D)) for _ in range(2)]
        b_wdn = [[Buf() for _ in range(8)] for _ in range(2)]
        xs_sm = [alloc(512, BF16) for _ in range(NST)]; b_xssm = [Buf() for _ in range(NST)]
        xsT2 = [alloc(4 * CAP, BF16, (8, CAP)) for _ in range(2)]; b_xsT2 = [[Buf() for _ in range(NST)] for _ in range(2)]
        actT = alloc(4 * CAP, BF16, (8, CAP)); b_actT = Buf()
        g_sb = [alloc(512) for _ in range(2)]; b_g = [Buf() for _ in range(2)]
        s_sb = [alloc(512) for _ in range(2)]; b_s = [Buf() for _ in range(2)]
        u_sb = [alloc(512) for _ in range(2)]; b_us = [Buf() for _ in range(2)]
        eo_sb = [alloc(D) for _ in range(2)]; b_eosb = [Buf() for _ in range(2)]
        bdbc = [alloc(D) for _ in range(2)]; b_bdbc = [Buf() for _ in range(2)]
        bgu1 = alloc(NE * 16); b_bgu1 = Buf()
        TS_("dve", bgu1, bgu_sb, 1.0, None, ALU.add, None, [b_bgu], [b_bgu1])
        bankbf2 = [banks[6][:, :].bitcast(BF16), banks[7][:, :].bitcast(BF16)]

        def load_expert(e):
            bi = e % 2
            for k in range(8):
                DMA_("pool", wgu[bi][:, k, :], wgu_d[e, k * P:(k + 1) * P, :], (), [b_wgu[bi][k]])
            for k in range(8):
                DMA_("pool", wdn[bi][:, k, :], wd_d[e, k * P:(k + 1) * P, :], (), [b_wdn[bi][k]])
            DMA_("sp", bdbc[bi], bd_d[e:e + 1, :].partition_broadcast(P), (), [b_bdbc[bi]])

        def prefetch_xs(e):
            for s in range(NST):
                DMA_("sp", xs_sm[s], xs_scr[e * CAP + s * P:e * CAP + (s + 1) * P, :], [b_xs], [b_xssm[s]])

        def transposes(e):
            xsT, b_xsT = xsT2[e % 2], b_xsT2[e % 2]
            for s in range(NST):
                xm, bxm = xs_sm[s], b_xssm[s]
                bankbf, bbf = bankbf2[s % 2], bank_bufs[6 + s % 2]
                for k in range(8):
                    TR_(bankbf[:, k * P:(k + 1) * P], xm[:, k * P:(k + 1) * P], identb, [bxm, b_identb], [bbf])
                CP_("act", xsT[:, :, s * P:(s + 1) * P], bankbf.rearrange("p (a b) -> p a b", a=8), [bbf], [b_xsT[s]])

        splits = [(0, 512), (512, CAP)]
        split_tiles = [[s for s in range(NST) if n0 <= s * P < n1] for (n0, n1) in splits]
        if n_exp > 0:
            load_expert(0)
            prefetch_xs(0)
            transposes(0)
        cnt = 0
        for e in range(n_exp):
            bi = e % 2
            if e + 1 < n_exp:
                load_expert(e + 1)
                prefetch_xs(e + 1)
            xsT, b_xsT = xsT2[bi], b_xsT2[bi]
            for (n0, n1), tiles in zip(splits, split_tiles):
                w = n1 - n0
                rb = [b_xsT[s] for s in tiles]
                for f in range(8):
                    pgs = []
                    for m in (f, 8 + f):
                        i = bank_rr[0] % 6
                        bank_rr[0] += 1
                        pb, bb = banks[i][:, :], bank_bufs[i]
                        for k in range(8):
                            MM_(pb[:, 0:w], wgu[bi][:, k, m * P:(m + 1) * P], xsT[:, k, n0:n1], k == 0, k == 7,
                                [b_wgu[bi][k]] + rb, [bb])
                        pgs.append((pb, bb))
                    (pg, bpg), (pu, bpu) = pgs
                    gi = cnt % 2
                    cnt += 1
                    gs, us, ss = g_sb[gi][:, 0:w], u_sb[gi][:, 0:w], s_sb[gi][:, 0:w]
                    TS_("dve", gs, pg[:, 0:w], bgu_sb[:, e * 16 + f:e * 16 + f + 1], 7.0, ALU.add, ALU.min, [bpg, b_bgu], [b_g[gi]])
                    ACT_(ss, gs, AF.Sigmoid, [b_g[gi]], [b_s[gi]], scale=1.702)
                    TS_("dve", us, pu[:, 0:w], bgu1[:, e * 16 + 8 + f:e * 16 + 9 + f], 8.0, ALU.add, ALU.min, [bpu, b_bgu1], [b_us[gi]])
                    TT_("dve", gs, gs, ss, ALU.mult, [b_g[gi], b_s[gi]], [b_g[gi]])
                    STT_("dve", actT[:, f, n0:n1], us, -6.0, gs, ALU.max, ALU.mult, [b_us[gi], b_g[gi]], [b_actT])
            if e + 1 < n_exp:
                transposes(e + 1)
            for s in range(NST):
                eo, beo = eo_sb[s % 2], b_eosb[s % 2]
                for h in range(2):
                    i = bank_rr[0] % 6
                    bank_rr[0] += 1
                    pb, bb = banks[i][:, :], bank_bufs[i]
                    for f in range(8):
                        MM_(pb, actT[:, f, s * P:(s + 1) * P], wdn[bi][:, f, h * 512:(h + 1) * 512], f == 0, f == 7,
                            [b_actT, b_wdn[bi][f]], [bb])
                    TT_("dve", eo[:, h * 512:(h + 1) * 512], pb, bdbc[bi][:, h * 512:(h + 1) * 512], ALU.add, [bb, b_bdbc[bi]], [beo])
                DMA_("sp", eo_scr[e * CAP + s * P:e * CAP + (s + 1) * P, :], eo, [beo], [b_eo])

        S.barrier()

        top[0] = persist_top
        NB4 = 3
        eo4 = [alloc(4 * D, F32, (4, D)) for _ in range(NB4)]; b_eo4 = [Buf() for _ in range(NB4)]
        x1c = [alloc(D) for _ in range(NB4)]; b_x1c = [Buf() for _ in range(NB4)]
        accd = [alloc(D) for _ in range(2)]; b_accd = [Buf() for _ in range(2)]
        lnt2 = (alloc(12, F32, (2, 6)), alloc(2), alloc(1)); b_lnt2 = Buf()
        n_ch = n_tiles * 4

        def fetch(ci):
            e4, be4 = eo4[ci % NB4], b_eo4[ci % NB4]
            for k in range(4):
                S.op("pool", lambda e, ci=ci, k=k, e4=e4: e.indirect_dma_start(
                    out=e4[:, k, :], out_offset=None, in_=eo_scr,
                    in_offset=bass.IndirectOffsetOnAxis(ap=dst_all[:, ci, k:k + 1], axis=0)),
                    [b_eo, b_dst_all[ci]], [be4], dma=True)
            DMA_("sp", x1c[ci % NB4], x1_scr[ci * P:(ci + 1) * P, :], [b_x1scr[ci]], [b_x1c[ci % NB4]])

        for ci in range(min(NB4 - 1, n_ch)):
            fetch(ci)
        for ci in range(n_ch):
            if ci + NB4 - 1 < n_ch:
                fetch(ci + NB4 - 1)
            e4, be4 = eo4[ci % NB4], b_eo4[ci % NB4]
            xx, bxx = x1c[ci % NB4], b_x1c[ci % NB4]
            a, ba = accd[ci % 2], b_accd[ci % 2]
            TS_("dve", a, e4[:, 0, :], gate_all[:, ci, 0:1], None, ALU.mult, None, [be4, b_gate_all[ci]], [ba])
            for k in range(1, 4):
                STT_("dve", a, e4[:, k, :], gate_all[:, ci, k:k + 1], a, ALU.mult, ALU.add, [be4, b_gate_all[ci], ba], [ba])
            STT_("dve", a, xx, ALPHA, a, ALU.mult, ALU.add, [bxx, ba], [ba])
            layer_norm(a, ba, lnC_g, lnC_b, b_lnC, a, ba, lnt2, b_lnt2, aff="dve")
            out_dmas.append(DMA_("sp", out_d[ci * P:(ci + 1) * P, :], a, [ba], ()))

        S.emit_all(final_waits=[("sp", d) for d in out_dmas])
    return nc


def _host_layout(inp, b):
    f = np.float32
    w_in = inp["w_in"][0]
    cb, cc, ch, gu, gv, q, gates = np.split(w_in, np.cumsum([1024, 1024, 1024, 1024, 1024, 1024])[:], axis=1)
    segs = [inp["w_kv"][0], gv, gu]
    for c in range(8):
        sl = slice(c * 128, (c + 1) * 128)
        segs += [cb[:, sl], cc[:, sl], ch[:, sl]]
    segs.append(q)
    wcp, wgp, wxp = inp["w_conv_proj"][0], inp["w_gmlp_proj"][0], inp["w_xa_proj"][0]
    for dc in range(8):
        sl = slice(dc * 128, (dc + 1) * 128)
        segs += [gates[:, 0:1024][:, sl], gates[:, 1024:2048][:, sl], gates[:, 2048:3072][:, sl], wcp[:, sl], wgp[:, sl], wxp[:, sl]]
    segs.append(inp["w_out"][0])
    wmix = np.ascontiguousarray(np.concatenate(segs, axis=1), dtype=f)
    assert wmix.shape == (D, WCOLS)
    shared = {
        "wmix": wmix,
        "b_gate_T": np.ascontiguousarray(inp["b_gate"][0].reshape(24, 128).T, dtype=f),
        "conv_w_T": np.ascontiguousarray(inp["conv_w"][0].reshape(3, 8, 128).transpose(2, 0, 1).reshape(128, 24), dtype=f),
        "gmlp_wsT": np.ascontiguousarray(inp["gmlp_ws"][0].transpose(2, 0, 1).reshape(128, 8 * 128), dtype=f),
        "gmlp_b": np.ascontiguousarray(inp["gmlp_b"][0].reshape(1, D), dtype=f),
        "gmlp_ln_g": np.ascontiguousarray(inp["gmlp_ln_g"], dtype=f), "gmlp_ln_b": np.ascontiguousarray(inp["gmlp_ln_b"], dtype=f),
        "mem_ln_g": np.ascontiguousarray(inp["mem_ln_g"], dtype=f), "mem_ln_b": np.ascontiguousarray(inp["mem_ln_b"], dtype=f),
        "ln1_g": np.ascontiguousarray(inp["ln1_g"], dtype=f), "ln1_b": np.ascontiguousarray(inp["ln1_b"], dtype=f),
        "ln2_g": np.ascontiguousarray(inp["ln2_g"], dtype=f), "ln2_b": np.ascontiguousarray(inp["ln2_b"], dtype=f),
        "w_router": np.ascontiguousarray(inp["w_router"][0], dtype=f),
        "b_router": np.ascontiguousarray(inp["b_router"], dtype=f),
        "w_gate_up": np.ascontiguousarray(inp["w_gate_up"][0], dtype=f),
        "b_gate_up_T": np.ascontiguousarray(inp["b_gate_up"][0].reshape(32, 16, 128).transpose(2, 0, 1).reshape(128, 512), dtype=f),
        "w_down": np.ascontiguousarray(inp["w_down"][0], dtype=f),
        "b_down": np.ascontiguousarray(inp["b_down"][0], dtype=f),
    }
    return shared


def _core_inputs(inp, shared, b):
    m = dict(shared)
    xb = np.ascontiguousarray(inp["x"][b], dtype=np.float32)
    m["x"] = xb
    m["xT"] = np.ascontiguousarray(xb.T)
    m["mem"] = np.ascontiguousarray(inp["mem"][b], dtype=np.float32)
    return m


def kernel(**inputs):
    inp = {k: np.asarray(v) for k, v in inputs.items()}
    nb = inp["x"].shape[0]
    shared = _host_layout(inp, 0)
    in_maps = [_core_inputs(inp, shared, b) for b in range(nb)]
    nc = build()
    res = run_bass_kernel_spmd(nc, in_maps, core_ids=list(range(nb)))
    out = np.stack([np.asarray(r["out"]) for r in res.results], axis=0)
    return out.astype(np.float32, copy=False)
```

```python
import contextlib
import numpy as np
import concourse.bass as bass
import concourse.mybir as mybir
from concourse.bass_utils import run_bass_kernel_spmd

F32 = mybir.dt.float32
BF16 = mybir.dt.bfloat16
I32 = mybir.dt.int32
U32 = mybir.dt.uint32
AF = mybir.ActivationFunctionType
ALU = mybir.AluOpType

ENGS = ("pe", "act", "dve", "pool", "sp")


class Buf:
    __slots__ = ("name", "writers", "readers")

    def __init__(self, name=""):
        self.name = name
        self.writers = []
        self.readers = []


class Op:
    __slots__ = ("eng", "emit", "deps", "is_dma", "signal", "val", "sem", "idx")

    def __init__(self, eng, emit, is_dma):
        self.eng = eng
        self.emit = emit
        self.deps = []
        self.is_dma = is_dma
        self.signal = False
        self.val = None
        self.sem = None
        self.idx = None


class Sched:
    def __init__(self, nc, n_dma_sems=None):
        self.nc = nc
        self.streams = {e: [] for e in ENGS}
        self.n_dma_sems = n_dma_sems or {"sp": 8, "act": 2, "pool": 4}
        self.last = {}
        self.dmas_since_barrier = []

    @staticmethod
    def _same_inorder(a, b):
        return (not a.is_dma) and (not b.is_dma) and a.eng == b.eng

    def op(self, eng, emit, reads=(), writes=(), dma=False, extra_deps=()):
        o = Op(eng, emit, dma)
        o.idx = len(self.streams[eng])
        deps = list(extra_deps)
        for b in reads:
            deps.extend(b.writers)
        for b in writes:
            deps.extend(b.readers)
            deps.extend(b.writers)
        seen = set()
        for d in deps:
            if d is o or id(d) in seen:
                continue
            seen.add(id(d))
            if eng == "pe" and d.eng == "pe" and not d.is_dma and not dma:
                continue
            o.deps.append(d)
            d.signal = True
        for b in reads:
            b.readers = [r for r in b.readers if not self._same_inorder(r, o)] + [o]
        for b in writes:
            if b.readers:
                b.readers = []
                b.writers = []
            b.writers = [w for w in b.writers if not self._same_inorder(w, o)] + [o]
        self.streams[eng].append(o)
        if dma:
            self.dmas_since_barrier.append(o)
        else:
            self.last[eng] = o
        return o

    def barrier(self):
        deps = list(self.last.values()) + list(self.dmas_since_barrier)
        self.dmas_since_barrier = []
        for e in ENGS:
            self.op(e, lambda eng: eng.nop(), extra_deps=deps)

    def emit_all(self, final_waits=()):
        nc = self.nc
        with contextlib.ExitStack() as st:
            csem = {e: st.enter_context(nc.semaphore("c_" + e)) for e in ("pe", "act", "dve", "pool", "sp")}
            dsems = {e: [st.enter_context(nc.semaphore("d_%s%d" % (e, i))) for i in range(n)]
                     for e, n in self.n_dma_sems.items()}
            for e in ENGS:
                cnt = 0
                nd = self.n_dma_sems.get(e, 0)
                dcnt = [0] * max(nd, 1)
                rr = 0
                for o in self.streams[e]:
                    if o.is_dma:
                        s = rr % nd
                        rr += 1
                        dcnt[s] += 1
                        o.sem = dsems[e][s]
                        o.val = 16 * dcnt[s]
                    elif o.signal:
                        cnt += 1
                        o.sem = csem[e]
                        o.val = cnt
            fw = {}
            for (e, d) in final_waits:
                fw.setdefault(e, []).append(d)
            block = st.enter_context(nc.Block())
            streams = self.streams

            def make(e):
                def body(eng):
                    seen = {}

                    def wait(sem, val):
                        k = id(sem)
                        if seen.get(k, 0) < val:
                            eng.wait_ge(sem, val)
                            seen[k] = val

                    for o in streams[e]:
                        if o.is_dma and o.val > 16:
                            wait(o.sem, o.val - 16)
                        mx = {}
                        for d in o.deps:
                            k = id(d.sem)
                            if k not in mx or mx[k][1] < d.val:
                                mx[k] = (d.sem, d.val)
                        for sem_, val_ in mx.values():
                            wait(sem_, val_)
                        ins = o.emit(eng)
                        if o.is_dma:
                            ins.then_inc(o.sem, 16)
                        elif o.signal:
                            ins.then_inc(o.sem, 1)
                    for d in fw.get(e, ()):
                        wait(d.sem, d.val)
                return body

            for e, deco in (("pe", block.tensor), ("act", block.scalar), ("dve", block.vector),
                            ("pool", block.gpsimd), ("sp", block.sync)):
                if streams[e] or e in fw:
                    deco(make(e))


P = 128
D = 1024
SEQ = 4096
TT = 512
NCH = SEQ // P
NE = 32
CAP = 768
NST = CAP // P
NROWS = NE * CAP
ZROW = NROWS
ALPHA = 2.0 ** 0.25
EPS = 1e-5
SEG_KV = 0
SEG_A = 2048
SEG_B = SEG_A + 1024
SEG_C = SEG_B + 1024
SEG_D = SEG_C + 3072
SEG_E = SEG_D + 1024
SEG_F = SEG_E + 8 * 768
WCOLS = SEG_F + 1024


def build(n_tiles=SEQ // TT, n_exp=NE, debug=False):
    nc = bass.Bass("TRN2", target_bir_lowering=False)

    def din(name, shape, dt=F32):
        return nc.dram_tensor(name, list(shape), dt, kind="ExternalInput").ap()

    xT_d = din("xT", [D, SEQ])
    x_d = din("x", [SEQ, D])
    mem_d = din("mem", [256, D])
    wmix_d = din("wmix", [D, WCOLS])
    bgate_d = din("b_gate_T", [P, 24])
    convw_d = din("conv_w_T", [P, 24])
    wsT_d = din("gmlp_wsT", [P, 8 * P])
    gmlpb_d = din("gmlp_b", [1, D])
    glng_d = din("gmlp_ln_g", [1, D])
    glnb_d = din("gmlp_ln_b", [1, D])
    mlng_d = din("mem_ln_g", [1, D])
    mlnb_d = din("mem_ln_b", [1, D])
    l1g_d = din("ln1_g", [1, D])
    l1b_d = din("ln1_b", [1, D])
    l2g_d = din("ln2_g", [1, D])
    l2b_d = din("ln2_b", [1, D])
    wr_d = din("w_router", [D, NE])
    br_d = din("b_router", [1, NE])
    wgu_d = din("w_gate_up", [NE, D, 2 * D])
    bgu_d = din("b_gate_up_T", [P, NE * 16])
    wd_d = din("w_down", [NE, D, D])
    bd_d = din("b_down", [NE, D])
    out_d = nc.dram_tensor("out", [SEQ, D], F32, kind="ExternalOutput").ap()
    if debug:
        x1_scr = nc.dram_tensor("x1_dbg", [SEQ, D], F32, kind="ExternalOutput").ap()
    else:
        x1_scr = nc.dram_tensor("x1_scr", [SEQ, D], F32).ap()
    wmix_bf = nc.dram_tensor("wmix_bf", [D, WCOLS], BF16).ap()
    xs_scr = nc.dram_tensor("xs_scr", [NROWS, D], BF16).ap()
    eo_scr = nc.dram_tensor("eo_scr", [NROWS + 1, D], F32).ap()

    S = Sched(nc)
    out_dmas = []

    with contextlib.ExitStack() as st:
        NF = 52224
        big = st.enter_context(nc.sbuf_tensor("arena", [P, NF], F32))
        banks = [st.enter_context(nc.psum_tensor("bank%d" % i, [P, 512], F32)) for i in range(8)]
        bank_bufs = [Buf("bank%d" % i) for i in range(8)]
        bank_rr = [0]

        def bank():
            i = bank_rr[0] % 8
            bank_rr[0] += 1
            return banks[i][:, :], bank_bufs[i]

        top = [0]

        def alloc(ncols, dt=F32, shape=None):
            a = big[:, top[0]:top[0] + ncols]
            top[0] += ncols
            assert top[0] <= NF, "SBUF arena overflow %d" % top[0]
            if dt != F32:
                a = a.bitcast(dt)
            if shape is not None:
                names = " ".join("d%d" % i for i in range(len(shape)))
                kw = {"d%d" % i: s for i, s in enumerate(shape)}
                a = a.rearrange("p (%s) -> p %s" % (names, names), **kw)
            return a

        def TT_(eng, out, in0, in1, op, R, W):
            return S.op(eng, lambda e: e.tensor_tensor(out=out, in0=in0, in1=in1, op=op), R, W)

        def TS_(eng, out, in0, s1, s2, op0, op1, R, W):
            if op1 is None:
                return S.op(eng, lambda e: e.tensor_scalar(out=out, in0=in0, scalar1=s1, scalar2=None, op0=op0), R, W)
            return S.op(eng, lambda e: e.tensor_scalar(out=out, in0=in0, scalar1=s1, scalar2=s2, op0=op0, op1=op1), R, W)

        def STT_(eng, out, in0, sc, in1, op0, op1, R, W, accum=None):
            if accum is None:
                return S.op(eng, lambda e: e.scalar_tensor_tensor(out=out, in0=in0, scalar=sc, in1=in1, op0=op0, op1=op1), R, W)
            return S.op(eng, lambda e: e.scalar_tensor_tensor(out=out, in0=in0, scalar=sc, in1=in1, op0=op0, op1=op1,
                                                              accum_out=accum), R, W)

        def ACT_(out, in_, func, R, W, bias=None, scale=1.0, accum=None):
            kw = {}
            if bias is not None:
                kw["bias"] = bias
            if accum is not None:
                kw["accum_out"] = accum
            return S.op("act", lambda e: e.activation(out=out, in_=in_, func=func, scale=scale, **kw), R, W)

        def CP_(eng, out, in_, R, W):
            if eng == "act":
                return S.op("act", lambda e: e.copy(out=out, in_=in_), R, W)
            return S.op(eng, lambda e: e.tensor_copy(out=out, in_=in_), R, W)

        def MS_(eng, ap, val, W):
            return S.op(eng, lambda e: e.memset(ap, val), (), W)

        def MM_(out, lhsT, rhs, start, stop, R, W):
            return S.op("pe", lambda e: e.matmul(out, lhsT=lhsT, rhs=rhs, start=start, stop=stop), R, W)

        def TR_(out, in_, ident, R, W):
            return S.op("pe", lambda e: e.transpose(out=out, in_=in_, identity=ident), R, W)

        def DMA_(eng, out, in_, R, W):
            return S.op(eng, lambda e: e.dma_start(out=out, in_=in_), R, W, dma=True)

        def layer_norm(xin, b_x, g_bc, b_bc, b_par, out, b_out, tmp, b_tmp, aff="pool"):
            st6, mv, rstd = tmp
            for h in range(2):
                S.op("dve", lambda e, h=h: e.bn_stats(out=st6[:, h, :], in_=xin[:, h * 512:(h + 1) * 512]), [b_x], [b_tmp])
            S.op("dve", lambda e: e.bn_aggr(out=mv, in_=st6.rearrange("p a b -> p (a b)")), [b_tmp], [b_tmp])
            ACT_(rstd, mv[:, 1:2], AF.Sqrt, [b_tmp, b_eps], [b_tmp], bias=eps_t)
            S.op("dve", lambda e: e.reciprocal(out=rstd, in_=rstd), [b_tmp], [b_tmp])
            TS_("dve", xin, xin, mv[:, 0:1], rstd, ALU.subtract, ALU.mult, [b_x, b_tmp], [b_x])
            TT_(aff, xin, xin, g_bc, ALU.mult, [b_x, b_par], [b_x])
            TT_(aff, out, xin, b_bc, ALU.add, [b_x, b_par], [b_out])

        identf = alloc(128); b_identf = Buf()
        identb = alloc(64, BF16); b_identb = Buf()
        onesb = alloc(64, BF16); b_onesb = Buf()
        ustrb = alloc(64, BF16); b_ustr = Buf()
        eps_t = alloc(1); b_eps = Buf()
        iota32 = alloc(32); b_iota = Buf()
        wr_sb = alloc(8 * NE, F32, (8, NE)); b_wr = Buf()
        br_bc = alloc(NE); b_br = Buf()
        gate_all = alloc(NCH * 4, F32, (NCH, 4)); b_gate_all = [Buf() for _ in range(NCH)]
        dst_all = alloc(NCH * 4, I32, (NCH, 4)); b_dst_all = [Buf() for _ in range(NCH)]
        tot_bc = alloc(NE); b_tot = Buf()
        bgu_sb = alloc(NE * 16); b_bgu = Buf()
        lnC_g = alloc(D); lnC_b = alloc(D); b_lnC = Buf()
        zrow = alloc(D); b_zrow = Buf()
        lnt = (alloc(12, F32, (2, 6)), alloc(2), alloc(1)); b_lnt = Buf()
        persist_top = top[0]
        WcT = alloc(512, BF16, (8, P)); b_WcT = Buf()
        biasbc = alloc(D, F32, (8, P)); b_biasbc = Buf()
        KT = alloc(1024, BF16, (8, 256)); b_KT = Buf()
        Vt = alloc(1024, BF16, (2, D)); b_Vt = Buf()
        bgate = alloc(24); b_bgate = Buf()
        convw = alloc(24); b_convw = Buf()
        halo = alloc(16, F32, (8, 2)); b_halo = [Buf() for _ in range(8)]
        gln_g = alloc(D); gln_b = alloc(D); b_gln = Buf()
        l1_g = alloc(D); l1_b = alloc(D); b_l1 = Buf()
        mixer_base = top[0]

        MS_("pool", identf, 0.0, [b_identf])
        S.op("pool", lambda e: e.affine_select(out=identf, in_=identf, pattern=[[-1, P]], compare_op=ALU.not_equal,
                                               fill=1.0, base=0, channel_multiplier=1), [b_identf], [b_identf])
        CP_("dve", identb, identf, [b_identf], [b_identb])
        MS_("pool", onesb, 1.0, [b_onesb])
        ustrf = alloc(128); b_ustrf = Buf()
        MS_("pool", ustrf, 1.0, [b_ustrf])
        S.op("pool", lambda e: e.affine_select(out=ustrf, in_=ustrf, pattern=[[1, P]], compare_op=ALU.is_gt,
                                               fill=0.0, base=0, channel_multiplier=-1), [b_ustrf], [b_ustrf])
        CP_("dve", ustrb, ustrf, [b_ustrf], [b_ustr])
        MS_("pool", eps_t, EPS, [b_eps])
        S.op("pool", lambda e: e.iota(iota32, pattern=[[1, NE]], base=0, channel_multiplier=0,
                                      allow_small_or_imprecise_dtypes=True), (), [b_iota])
        MS_("pool", tot_bc, 0.0, [b_tot])
        MS_("pool", halo.rearrange("p a b -> p (a b)"), 0.0, b_halo)
        MS_("pool", zrow, 0.0, [b_zrow])
        b_eo = Buf("eo_scr")
        DMA_("sp", eo_scr[ZROW:ZROW + 1, :], zrow[0:1, :], [b_zrow], [b_eo])
        DMA_("sp", wr_sb, wr_d.rearrange("(k p) n -> p k n", p=P), (), [b_wr])
        DMA_("sp", br_bc, br_d.partition_broadcast(P), (), [b_br])
        DMA_("sp", bgu_sb, bgu_d, (), [b_bgu])
        DMA_("sp", lnC_g, mlng_d.partition_broadcast(P), (), [b_lnC])
        DMA_("sp", lnC_b, mlnb_d.partition_broadcast(P), (), [b_lnC])
        DMA_("sp", biasbc.rearrange("p a b -> p (a b)"), gmlpb_d.partition_broadcast(P), (), [b_biasbc])
        DMA_("sp", bgate, bgate_d, (), [b_bgate])
        DMA_("sp", convw, convw_d, (), [b_convw])
        DMA_("sp", gln_g, glng_d.partition_broadcast(P), (), [b_gln])
        DMA_("sp", gln_b, glnb_d.partition_broadcast(P), (), [b_gln])
        DMA_("sp", l1_g, l1g_d.partition_broadcast(P), (), [b_l1])
        DMA_("sp", l1_b, l1b_d.partition_broadcast(P), (), [b_l1])
        wsTf = alloc(8 * P, F32, (8, P)); b_wsTf = Buf()
        DMA_("sp", wsTf.rearrange("p a b -> p (a b)"), wsT_d, (), [b_wsTf])
        S.op("pool", lambda e: e.affine_select(out=wsTf, in_=wsTf, pattern=[[0, 8], [1, P]], compare_op=ALU.is_ge,
                                               fill=0.0, base=0, channel_multiplier=-1), [b_wsTf], [b_wsTf])
        CP_("dve", WcT, wsTf, [b_wsTf], [b_WcT])
        const_top = top[0]

        NSLOT = 4
        ring = [alloc(2048, BF16, (8, 512)) for _ in range(NSLOT)]
        ring_bufs = [Buf("slab%d" % i) for i in range(NSLOT)]
        b_wmixbf = [Buf("wmixbf%d" % i) for i in range(WCOLS // 512)]

        for blk in range(WCOLS // 512):
            sl = blk % NSLOT
            c0 = blk * 512
            DMA_("pool", ring[sl], wmix_d[:, c0:c0 + 512].rearrange("(k p) n -> p k n", p=P), (), [ring_bufs[sl]])
            DMA_("sp", wmix_bf[:, c0:c0 + 512].rearrange("(k p) n -> p k n", p=P), ring[sl], [ring_bufs[sl]], [b_wmixbf[blk]])

        slab_list = []
        for c in range(4):
            slab_list.append((SEG_KV + c * 512, 512))
        for _ in range(n_tiles):
            slab_list += [(SEG_A, 512), (SEG_A + 512, 512)]
            slab_list += [(SEG_C + c * 384, 384) for c in range(8)]
            slab_list += [(SEG_B, 512), (SEG_B + 512, 512)]
            slab_list += [(SEG_D, 512), (SEG_D + 512, 512)]
            for dc in range(8):
                slab_list += [(SEG_E + dc * 768, 384), (SEG_E + dc * 768 + 384, 384)]
            slab_list += [(SEG_F, 512), (SEG_F + 512, 512)]
        slab_issued = [0]
        slab_next = [0]

        def issue_slab():
            i = slab_issued[0]
            if i >= len(slab_list):
                return
            c0, w = slab_list[i]
            sl = i % NSLOT
            rb = [b_wmixbf[b] for b in range(c0 // 512, (c0 + w - 1) // 512 + 1)]
            DMA_("sp", ring[sl][:, :, 0:w], wmix_bf[:, c0:c0 + w].rearrange("(k p) n -> p k n", p=P), rb, [ring_bufs[sl]])
            slab_issued[0] += 1

        def get_slab():
            i = slab_next[0]
            slab_next[0] += 1
            while slab_issued[0] < min(i + NSLOT - 1, len(slab_list)):
                issue_slab()
            sl = i % NSLOT
            return ring[sl], ring_bufs[sl]

        memx = [alloc(D) for _ in range(2)]; b_memx = [Buf() for _ in range(2)]
        memnT = alloc(1024, BF16, (8, 256)); b_memnT = Buf()
        for j in range(2):
            DMA_("sp", memx[j], mem_d[j * P:(j + 1) * P, :], (), [b_memx[j]])
            layer_norm(memx[j], b_memx[j], lnC_g, lnC_b, b_lnC, memx[j], b_memx[j], lnt, b_lnt)
            for hh in range(2):
                pb, bb = bank()
                for kk in range(4):
                    k = hh * 4 + kk
                    TR_(pb[:, kk * P:(kk + 1) * P], memx[j][:, k * P:(k + 1) * P], identf, [b_memx[j], b_identf], [bb])
                CP_("act", memnT[:, hh * 4:(hh + 1) * 4, j * P:(j + 1) * P], pb.rearrange("p (a b) -> p a b", a=4), [bb], [b_memnT])
        for c in range(2):
            slab, sb_ = get_slab()
            for dd in range(4):
                dchunk = c * 4 + dd
                pb, bb = bank()
                for k in range(8):
                    MM_(pb[:, 0:256], slab[:, k, dd * P:(dd + 1) * P], memnT[:, k, :], k == 0, k == 7, [sb_, b_memnT], [bb])
                CP_("act", KT[:, dchunk, :], pb[:, 0:256], [bb], [b_KT])
        for c in range(2):
            slab, sb_ = get_slab()
            for mc in range(2):
                pb, bb = bank()
                for k in range(8):
                    MM_(pb, memnT[:, k, mc * P:(mc + 1) * P], slab[:, k, :], k == 0, k == 7, [sb_, b_memnT], [bb])
                CP_("dve", Vt[:, mc, c * 512:(c + 1) * 512], pb, [bb], [b_Vt])

        top[0] = const_top + NSLOT * 2048
        xT_bf = alloc(2048, BF16, (8, TT)); b_xT = Buf()
        vn_mg = alloc(2048, BF16)
        v_n = vn_mg.rearrange("p (a b) -> p a b", a=4)
        merged = vn_mg.rearrange("p (a b) -> p a b", a=8)
        b_vnmg = Buf()
        vg = [alloc(D) for _ in range(2)]; b_vg = [Buf() for _ in range(2)]
        ug = [alloc(TT) for _ in range(2)]; b_ug = [Buf() for _ in range(2)]
        ftmp = [alloc(TT) for _ in range(2)]; b_ftmp = [Buf() for _ in range(2)]
        y_conv = alloc(2048, BF16, (8, TT)); b_yconv = Buf()
        y_gmlp = alloc(2048, BF16, (8, TT)); b_ygmlp = Buf()
        y_xa = alloc(2048, BF16, (8, TT)); b_yxa = Buf()
        cc_sb = alloc(TT); b_cc = Buf()
        u_t = [alloc(TT + 4) for _ in range(2)]; b_u = [Buf() for _ in range(2)]
        acc = alloc(TT); b_acc = Buf()
        qT = [alloc(512, BF16, (2, TT)) for _ in range(2)]; b_qT = [Buf() for _ in range(2)]
        PT = [alloc(512, BF16, (2, TT)) for _ in range(2)]; b_PT = [Buf() for _ in range(2)]
        rden = [alloc(TT) for _ in range(2)]; b_rden = [Buf() for _ in range(2)]
        sig = [alloc(3 * TT, F32, (3, TT)) for _ in range(2)]; b_sig = [Buf() for _ in range(2)]
        mt = alloc(3 * TT, F32, (3, TT)); b_mt = Buf()
        xc = [alloc(D) for _ in range(2)]; b_xc = [Buf() for _ in range(2)]
        r2 = [alloc(D) for _ in range(2)]; b_r2 = [Buf() for _ in range(2)]
        x1bf = [alloc(512, BF16) for _ in range(2)]; b_x1bf = [Buf() for _ in range(2)]
        x1T = alloc(D, F32, (8, P)); b_x1T = Buf()
        lgt = alloc(NE); mx8 = alloc(8); mi8 = alloc(8, U32); ef4 = alloc(4); negmax = alloc(1)
        ex4 = alloc(4); ssum = alloc(1); Mbf = alloc(NE // 2, BF16); pos = alloc(NE); junk = alloc(NE)
        pos4 = alloc(4); dstf = alloc(4); keep = alloc(4)
        b_rt = Buf()
        b_x1scr = [Buf() for _ in range(NCH)]
        b_xs = Buf("xs_scr")
        bc_reg = {}

        S.barrier()

        for ti in range(n_tiles):
            t0 = ti * TT
            if ti == 0:
                DMA_("pool", xT_bf, xT_d[:, t0:t0 + TT].rearrange("(k p) t -> p k t", p=P), (), [b_xT])

            slabA = [get_slab(), get_slab()]
            for j in range(4):
                vgj, bvg = vg[j % 2], b_vg[j % 2]
                for h in range(2):
                    pb, bb = bank()
                    sl, sb_ = slabA[h]
                    for k in range(8):
                        MM_(pb, xT_bf[:, k, j * P:(j + 1) * P], sl[:, k, :], k == 0, k == 7, [b_xT, sb_], [bb])
                    ACT_(vgj[:, h * 512:(h + 1) * 512], pb, AF.Gelu_apprx_tanh, [bb], [bvg])
                layer_norm(vgj, bvg, gln_g, gln_b, b_gln, v_n[:, j, :], b_vnmg, lnt, b_lnt)

            for c in range(8):
                sl, sb_ = get_slab()
                pcs = []
                for r in range(3):
                    pb, bb = bank()
                    for k in range(8):
                        MM_(pb, sl[:, k, r * P:(r + 1) * P], xT_bf[:, k, :], k == 0, k == 7, [sb_, b_xT], [bb])
                    pcs.append((pb, bb))
                (pcb, bcb), (pcc, bcc), (pch, bch) = pcs
                u, bu = u_t[c % 2], b_u[c % 2]
                CP_("act", cc_sb, pcc, [bcc], [b_cc])
                CP_("pool", u[:, 0:2], halo[:, c, :], [b_halo[c]], [bu])
                TT_("dve", u[:, 2:TT + 2], cc_sb, pch, ALU.mult, [b_cc, bch], [bu])
                CP_("pool", halo[:, c, :], u[:, TT:TT + 2], [bu], [b_halo[c]])
                TS_("dve", acc, u[:, 2:TT + 2], convw[:, 16 + c:17 + c], None, ALU.mult, None, [bu, b_convw], [b_acc])
                STT_("dve", acc, u[:, 1:TT + 1], convw[:, 8 + c:9 + c], acc, ALU.mult, ALU.add, [bu, b_convw, b_acc], [b_acc])
                STT_("dve", acc, u[:, 0:TT], convw[:, c:c + 1], acc, ALU.mult, ALU.add, [bu, b_convw, b_acc], [b_acc])
                TT_("dve", y_conv[:, c, :], acc, pcb, ALU.mult, [b_acc, bcb], [b_yconv])

            slabB = [get_slab(), get_slab()]
            for g in range(8):
                sl, sb_ = slabB[g // 4]
                gg = g % 4
                pu, bpu = bank()
                for k in range(8):
                    MM_(pu, sl[:, k, gg * P:(gg + 1) * P], xT_bf[:, k, :], k == 0, k == 7, [sb_, b_xT], [bpu])
                ACT_(ug[g % 2], pu, AF.Gelu_apprx_tanh, [bpu], [b_ug[g % 2]])
                pf, bpf = bank()
                for j in range(4):
                    MM_(pf[:, j * P:(j + 1) * P], v_n[:, j, g * P:(g + 1) * P], WcT[:, g, :], True, True, [b_vnmg, b_WcT], [bpf])
                TT_("dve", ftmp[g % 2].rearrange("p (j t) -> p j t", j=4), pf.rearrange("p (j t) -> p j t", j=4),
                    biasbc[:, g, :].unsqueeze(1).broadcast_to([P, 4, P]), ALU.add, [bpf, b_biasbc], [b_ftmp[g % 2]])
                TT_("pool", y_gmlp[:, g, :], ftmp[g % 2], ug[g % 2], ALU.mult, [b_ftmp[g % 2], b_ug[g % 2]], [b_ygmlp])

            slabD = [get_slab(), get_slab()]
            for h in range(4):
                sl, sb_ = slabD[h // 2]
                q, bq = qT[h % 2], b_qT[h % 2]
                for dc in range(2):
                    cc = (h % 2) * 2 + dc
                    pb, bb = bank()
                    for k in range(8):
                        MM_(pb, sl[:, k, cc * P:(cc + 1) * P], xT_bf[:, k, :], k == 0, k == 7, [sb_, b_xT], [bb])
                    CP_("act", q[:, dc, :], pb, [bb], [bq])
                pt, bpt = PT[h % 2], b_PT[h % 2]
                for mc in range(2):
                    pb, bb = bank()
                    for dc in range(2):
                        MM_(pb, KT[:, h * 2 + dc, mc * P:(mc + 1) * P], q[:, dc, :], dc == 0, dc == 1, [b_KT, bq], [bb])
                    ACT_(pt[:, mc, :], pb, AF.Exp, [bb], [bpt], scale=0.0625)
                pden, bden = bank()
                for mc in range(2):
                    MM_(pden, onesb, pt[:, mc, :], mc == 0, mc == 1, [b_onesb, bpt], [bden])
                rd, brd = rden[h % 2], b_rden[h % 2]
                S.op("dve", lambda e, rd=rd, pden=pden: e.reciprocal(out=rd, in_=pden), [bden], [brd])
                for dc in range(2):
                    po, bpo = bank()
                    for mc in range(2):
                        MM_(po, Vt[:, mc, (h * 2 + dc) * P:(h * 2 + dc + 1) * P], pt[:, mc, :], mc == 0, mc == 1, [b_Vt, bpt], [bpo])
                    TT_("dve", y_xa[:, h * 2 + dc, :], rd, po, ALU.mult, [brd, bpo], [b_yxa])

            ys = [(y_conv, b_yconv), (y_gmlp, b_ygmlp), (y_xa, b_yxa)]
            for dc in range(8):
                slg, sbg = get_slab()
                slp, sbp = get_slab()
                sg, bsg = sig[dc % 2], b_sig[dc % 2]
                for r in range(3):
                    pb, bb = bank()
                    for k in range(8):
                        MM_(pb, slg[:, k, r * P:(r + 1) * P], xT_bf[:, k, :], k == 0, k == 7, [sbg, b_xT], [bb])
                    ACT_(sg[:, r, :], pb, AF.Sigmoid, [bb, b_bgate], [bsg], bias=bgate[:, r * 8 + dc:r * 8 + dc + 1])
                for r in range(3):
                    pb, bb = bank()
                    yr, byr = ys[r]
                    for k in range(8):
                        MM_(pb, slp[:, k, r * P:(r + 1) * P], yr[:, k, :], k == 0, k == 7, [sbp, byr], [bb])
                    TT_("dve", mt[:, r, :], sg[:, r, :], pb, ALU.mult, [bsg, bb], [b_mt])
                TT_("pool", mt[:, 0, :], mt[:, 0, :], mt[:, 1, :], ALU.add, [b_mt], [b_mt])
                TT_("pool", merged[:, dc, :], mt[:, 0, :], mt[:, 2, :], ALU.add, [b_mt], [b_vnmg])

            if ti + 1 < n_tiles:
                DMA_("pool", xT_bf, xT_d[:, t0 + TT:t0 + 2 * TT].rearrange("(k p) t -> p k t", p=P), (), [b_xT])

            slabF = [get_slab(), get_slab()]

            wbanks = {}

            def stage1_mm(j):
                ci = ti * 4 + j
                xcj, bxc = xc[j % 2], b_xc[j % 2]
                DMA_("sp", xcj, x_d[ci * P:(ci + 1) * P, :], (), [bxc])
                wbanks[j] = []
                for h in range(2):
                    pb, bb = bank()
                    sl, sb_ = slabF[h]
                    for k in range(8):
                        MM_(pb, merged[:, k, j * P:(j + 1) * P], sl[:, k, :], k == 0, k == 7, [b_vnmg, sb_], [bb])
                    wbanks[j].append((pb, bb))

            def stage1_ew(j):
                ci = ti * 4 + j
                r_t, b_r = r2[j % 2], b_r2[j % 2]
                xcj, bxc = xc[j % 2], b_xc[j % 2]
                for h in range(2):
                    pb, bb = wbanks[j][h]
                    STT_("dve", r_t[:, h * 512:(h + 1) * 512], xcj[:, h * 512:(h + 1) * 512], ALPHA, pb, ALU.mult, ALU.add,
                         [bxc, bb], [b_r])
                layer_norm(r_t, b_r, l1_g, l1_b, b_l1, r_t, b_r, lnt, b_lnt)
                DMA_("sp", x1_scr[ci * P:(ci + 1) * P, :], r_t, [b_r], [b_x1scr[ci]])
                xb, bxb = x1bf[j % 2], b_x1bf[j % 2]
                CP_("act", xb, r_t, [b_r], [bxb])

            def stage2(j):
                ci = ti * 4 + j
                r_t, b_r = r2[j % 2], b_r2[j % 2]
                xb, bxb = x1bf[j % 2], b_x1bf[j % 2]
                for hh in range(2):
                    pb, bb = bank()
                    for kk in range(4):
                        k = hh * 4 + kk
                        TR_(pb[:, kk * P:(kk + 1) * P], r_t[:, k * P:(k + 1) * P], identf, [b_r, b_identf], [bb])
                    CP_("act", x1T[:, hh * 4:(hh + 1) * 4, :], pb.rearrange("p (a b) -> p a b", a=4), [bb], [b_x1T])
                pl, bpl = bank()
                for k in range(8):
                    MM_(pl[:, 0:NE], x1T[:, k, :], wr_sb[:, k, :], k == 0, k == 7, [b_x1T, b_wr], [bpl])
                TT_("dve", lgt, pl[:, 0:NE], br_bc, ALU.add, [bpl, b_br], [b_rt])
                S.op("dve", lambda e: e.max(out=mx8, in_=lgt), [b_rt], [b_rt])
                S.op("dve", lambda e: e.max_index(out=mi8, in_max=mx8, in_values=lgt), [b_rt], [b_rt])
                TS_("dve", negmax, mx8[:, 0:1], -1.0, None, ALU.mult, None, [b_rt], [b_rt])
                MS_("pool", ssum, 0.0, [b_rt])
                MS_("pool", pos4, 0.0, [b_rt])
                ACT_(ex4, mx8[:, 0:4], AF.Exp, [b_rt], [b_rt], bias=negmax, accum=ssum)
                S.op("dve", lambda e: e.reciprocal(out=ssum, in_=ssum), [b_rt], [b_rt])
                TS_("dve", gate_all[:, ci, :], ex4, ssum, None, ALU.mult, None, [b_rt], [b_gate_all[ci]])
                TS_("dve", Mbf, lgt, mx8[:, 3:4], None, ALU.is_ge, None, [b_rt], [b_rt])
                pp, bpp = bank()
                MM_(pp[:, 0:NE], ustrb, Mbf, True, True, [b_ustr, b_rt], [bpp])
                MM_(pp[:, NE:2 * NE], onesb, Mbf, True, True, [b_onesb, b_rt], [bpp])
                TT_("dve", pos, pp[:, 0:NE], tot_bc, ALU.add, [bpp, b_tot], [b_rt])
                TT_("dve", tot_bc, pp[:, NE:2 * NE], tot_bc, ALU.add, [bpp, b_tot], [b_tot])
                CP_("dve", ef4, mi8[:, 0:4], [b_rt], [b_rt])
                for k in range(4):
                    STT_("dve", junk, iota32, ef4[:, k:k + 1], pos, ALU.is_equal, ALU.mult, [b_iota, b_rt], [b_rt],
                         accum=pos4[:, k:k + 1])
                STT_("dve", dstf, ef4, float(CAP), pos4, ALU.mult, ALU.add, [b_rt], [b_rt])
                TS_("dve", keep, pos4, float(CAP), None, ALU.is_lt, None, [b_rt], [b_rt])
                TS_("dve", dstf, dstf, -float(ZROW), None, ALU.add, None, [b_rt], [b_rt])
                TT_("dve", dstf, dstf, keep, ALU.mult, [b_rt], [b_rt])
                TS_("dve", dstf, dstf, float(ZROW), None, ALU.add, None, [b_rt], [b_rt])
                CP_("dve", dst_all[:, ci, :], dstf, [b_rt], [b_dst_all[ci]])
                for k in range(4):
                    def scat(e, ci=ci, k=k, xb=xb):
                        if "r" not in bc_reg:
                            bc_reg["r"] = e.to_reg(NROWS - 1)
                        return e.indirect_dma_start(
                            out=xs_scr, out_offset=bass.IndirectOffsetOnAxis(ap=dst_all[:, ci, k:k + 1], axis=0),
                            in_=xb, in_offset=None, bounds_check=bc_reg["r"], oob_is_err=False)
                    S.op("pool", scat, [bxb, b_dst_all[ci]], [b_xs], dma=True)

            stage1_mm(0)
            stage1_ew(0)
            for j in range(4):
                if j + 1 < 4:
                    stage1_mm(j + 1)
                stage2(j)
                if j + 1 < 4:
                    stage1_ew(j + 1)

        S.barrier()

        top[0] = persist_top
        DMA_("sp", lnC_g, l2g_d.partition_broadcast(P), (), [b_lnC])
        DMA_("sp", lnC_b, l2b_d.partition_broadcast(P), (), [b_lnC])
        wgu = [alloc(8192, BF16, (8, 2 * D)) for _ in range(2)]
        b_wgu = [[Buf() for _ in range(8)] for _ in range(2)]
        wdn = [alloc(4096, BF16, (8, D)) for _ in range(2)]
        b_wdn = [[Buf() for _ in range(8)] for _ in range(2)]
        xs_sm = [alloc(512, BF16) for _ in range(NST)]; b_xssm = [Buf() for _ in range(NST)]
        xsT2 = [alloc(4 * CAP, BF16, (8, CAP)) for _ in range(2)]; b_xsT2 = [[Buf() for _ in range(NST)] for _ in range(2)]
        actT = alloc(4 * CAP, BF16, (8, CAP)); b_actT = Buf()
        g_sb = [alloc(512) for _ in range(2)]; b_g = [Buf() for _ in range(2)]
        s_sb = [alloc(512) for _ in range(2)]; b_s = [Buf() for _ in range(2)]
        u_sb = [alloc(512) for _ in range(2)]; b_us = [Buf() for _ in range(2)]
        eo_sb = [alloc(D) for _ in range(2)]; b_eosb = [Buf() for _ in range(2)]
        bdbc = [alloc(D) for _ in range(2)]; b_bdbc = [Buf() for _ in range(2)]
        bgu1 = alloc(NE * 16); b_bgu1 = Buf()
        TS_("dve", bgu1, bgu_sb, 1.0, None, ALU.add, None, [b_bgu], [b_bgu1])
        bankbf2 = [banks[6][:, :].bitcast(BF16), banks[7][:, :].bitcast(BF16)]

        def load_expert(e):
            bi = e % 2
            for k in range(8):
                DMA_("pool", wgu[bi][:, k, :], wgu_d[e, k * P:(k + 1) * P, :], (), [b_wgu[bi][k]])
            for k in range(8):
                DMA_("pool", wdn[bi][:, k, :], wd_d[e, k * P:(k + 1) * P, :], (), [b_wdn[bi][k]])
            DMA_("sp", bdbc[bi], bd_d[e:e + 1, :].partition_broadcast(P), (), [b_bdbc[bi]])

        def prefetch_xs(e):
            for s in range(NST):
                DMA_("sp", xs_sm[s], xs_scr[e * CAP + s * P:e * CAP + (s + 1) * P, :], [b_xs], [b_xssm[s]])

        def transposes(e):
            xsT, b_xsT = xsT2[e % 2], b_xsT2[e % 2]
            for s in range(NST):
                xm, bxm = xs_sm[s], b_xssm[s]
                bankbf, bbf = bankbf2[s % 2], bank_bufs[6 + s % 2]
                for k in range(8):
                    TR_(bankbf[:, k * P:(k + 1) * P], xm[:, k * P:(k + 1) * P], identb, [bxm, b_identb], [bbf])
                CP_("act", xsT[:, :, s * P:(s + 1) * P], bankbf.rearrange("p (a b) -> p a b", a=8), [bbf], [b_xsT[s]])

        splits = [(0, 512), (512, CAP)]
        split_tiles = [[s for s in range(NST) if n0 <= s * P < n1] for (n0, n1) in splits]
        if n_exp > 0:
            load_expert(0)
            prefetch_xs(0)
            transposes(0)
        cnt = 0
        for e in range(n_exp):
            bi = e % 2
            if e + 1 < n_exp:
                load_expert(e + 1)
                prefetch_xs(e + 1)
            xsT, b_xsT = xsT2[bi], b_xsT2[bi]
            for (n0, n1), tiles in zip(splits, split_tiles):
                w = n1 - n0
                rb = [b_xsT[s] for s in tiles]
                for f in range(8):
                    pgs = []
                    for m in (f, 8 + f):
                        i = bank_rr[0] % 6
                        bank_rr[0] += 1
                        pb, bb = banks[i][:, :], bank_bufs[i]
                        for k in range(8):
                            MM_(pb[:, 0:w], wgu[bi][:, k, m * P:(m + 1) * P], xsT[:, k, n0:n1], k == 0, k == 7,
                                [b_wgu[bi][k]] + rb, [bb])
                        pgs.append((pb, bb))
                    (pg, bpg), (pu, bpu) = pgs
                    gi = cnt % 2
                    cnt += 1
                    gs, us, ss = g_sb[gi][:, 0:w], u_sb[gi][:, 0:w], s_sb[gi][:, 0:w]
                    TS_("dve", gs, pg[:, 0:w], bgu_sb[:, e * 16 + f:e * 16 + f + 1], 7.0, ALU.add, ALU.min, [bpg, b_bgu], [b_g[gi]])
                    ACT_(ss, gs, AF.Sigmoid, [b_g[gi]], [b_s[gi]], scale=1.702)
                    TS_("dve", us, pu[:, 0:w], bgu1[:, e * 16 + 8 + f:e * 16 + 9 + f], 8.0, ALU.add, ALU.min, [bpu, b_bgu1], [b_us[gi]])
                    TT_("dve", gs, gs, ss, ALU.mult, [b_g[gi], b_s[gi]], [b_g[gi]])
                    STT_("dve", actT[:, f, n0:n1], us, -6.0, gs, ALU.max, ALU.mult, [b_us[gi], b_g[gi]], [b_actT])
            if e + 1 < n_exp:
                transposes(e + 1)
            for s in range(NST):
                eo, beo = eo_sb[s % 2], b_eosb[s % 2]
                for h in range(2):
                    i = bank_rr[0] % 6
                    bank_rr[0] += 1
                    pb, bb = banks[i][:, :], bank_bufs[i]
                    for f in range(8):
                        MM_(pb, actT[:, f, s * P:(s + 1) * P], wdn[bi][:, f, h * 512:(h + 1) * 512], f == 0, f == 7,
                            [b_actT, b_wdn[bi][f]], [bb])
                    TT_("dve", eo[:, h * 512:(h + 1) * 512], pb, bdbc[bi][:, h * 512:(h + 1) * 512], ALU.add, [bb, b_bdbc[bi]], [beo])
                DMA_("sp", eo_scr[e * CAP + s * P:e * CAP + (s + 1) * P, :], eo, [beo], [b_eo])

        S.barrier()

        top[0] = persist_top
        NB4 = 3
        eo4 = [alloc(4 * D, F32, (4, D)) for _ in range(NB4)]; b_eo4 = [Buf() for _ in range(NB4)]
        x1c = [alloc(D) for _ in range(NB4)]; b_x1c = [Buf() for _ in range(NB4)]
        accd = [alloc(D) for _ in range(2)]; b_accd = [Buf() for _ in range(2)]
        lnt2 = (alloc(12, F32, (2, 6)), alloc(2), alloc(1)); b_lnt2 = Buf()
        n_ch = n_tiles * 4

        def fetch(ci):
            e4, be4 = eo4[ci % NB4], b_eo4[ci % NB4]
            for k in range(4):
                S.op("pool", lambda e, ci=ci, k=k, e4=e4: e.indirect_dma_start(
                    out=e4[:, k, :], out_offset=None, in_=eo_scr,
                    in_offset=bass.IndirectOffsetOnAxis(ap=dst_all[:, ci, k:k + 1], axis=0)),
                    [b_eo, b_dst_all[ci]], [be4], dma=True)
            DMA_("sp", x1c[ci % NB4], x1_scr[ci * P:(ci + 1) * P, :], [b_x1scr[ci]], [b_x1c[ci % NB4]])

        for ci in range(min(NB4 - 1, n_ch)):
            fetch(ci)
        for ci in range(n_ch):
            if ci + NB4 - 1 < n_ch:
                fetch(ci + NB4 - 1)
            e4, be4 = eo4[ci % NB4], b_eo4[ci % NB4]
            xx, bxx = x1c[ci % NB4], b_x1c[ci % NB4]
            a, ba = accd[ci % 2], b_accd[ci % 2]
            TS_("dve", a, e4[:, 0, :], gate_all[:, ci, 0:1], None, ALU.mult, None, [be4, b_gate_all[ci]], [ba])
            for k in range(1, 4):
                STT_("dve", a, e4[:, k, :], gate_all[:, ci, k:k + 1], a, ALU.mult, ALU.add, [be4, b_gate_all[ci], ba], [ba])
            STT_("dve", a, xx, ALPHA, a, ALU.mult, ALU.add, [bxx, ba], [ba])
            layer_norm(a, ba, lnC_g, lnC_b, b_lnC, a, ba, lnt2, b_lnt2, aff="dve")
            out_dmas.append(DMA_("sp", out_d[ci * P:(ci + 1) * P, :], a, [ba], ()))

        S.emit_all(final_waits=[("sp", d) for d in out_dmas])
    return nc


def _host_layout(inp, b):
    f = np.float32
    w_in = inp["w_in"][0]
    cb, cc, ch, gu, gv, q, gates = np.split(w_in, np.cumsum([1024, 1024, 1024, 1024, 1024, 1024])[:], axis=1)
    segs = [inp["w_kv"][0], gv, gu]
    for c in range(8):
        sl = slice(c * 128, (c + 1) * 128)
        segs += [cb[:, sl], cc[:, sl], ch[:, sl]]
    segs.append(q)
    wcp, wgp, wxp = inp["w_conv_proj"][0], inp["w_gmlp_proj"][0], inp["w_xa_proj"][0]
    for dc in range(8):
        sl = slice(dc * 128, (dc + 1) * 128)
        segs += [gates[:, 0:1024][:, sl], gates[:, 1024:2048][:, sl], gates[:, 2048:3072][:, sl], wcp[:, sl], wgp[:, sl], wxp[:, sl]]
    segs.append(inp["w_out"][0])
    wmix = np.ascontiguousarray(np.concatenate(segs, axis=1), dtype=f)
    assert wmix.shape == (D, WCOLS)
    shared = {
        "wmix": wmix,
        "b_gate_T": np.ascontiguousarray(inp["b_gate"][0].reshape(24, 128).T, dtype=f),
        "conv_w_T": np.ascontiguousarray(inp["conv_w"][0].reshape(3, 8, 128).transpose(2, 0, 1).reshape(128, 24), dtype=f),
        "gmlp_wsT": np.ascontiguousarray(inp["gmlp_ws"][0].transpose(2, 0, 1).reshape(128, 8 * 128), dtype=f),
        "gmlp_b": np.ascontiguousarray(inp["gmlp_b"][0].reshape(1, D), dtype=f),
        "gmlp_ln_g": np.ascontiguousarray(inp["gmlp_ln_g"], dtype=f), "gmlp_ln_b": np.ascontiguousarray(inp["gmlp_ln_b"], dtype=f),
        "mem_ln_g": np.ascontiguousarray(inp["mem_ln_g"], dtype=f), "mem_ln_b": np.ascontiguousarray(inp["mem_ln_b"], dtype=f),
        "ln1_g": np.ascontiguousarray(inp["ln1_g"], dtype=f), "ln1_b": np.ascontiguousarray(inp["ln1_b"], dtype=f),
        "ln2_g": np.ascontiguousarray(inp["ln2_g"], dtype=f), "ln2_b": np.ascontiguousarray(inp["ln2_b"], dtype=f),
        "w_router": np.ascontiguousarray(inp["w_router"][0], dtype=f),
        "b_router": np.ascontiguousarray(inp["b_router"], dtype=f),
        "w_gate_up": np.ascontiguousarray(inp["w_gate_up"][0], dtype=f),
        "b_gate_up_T": np.ascontiguousarray(inp["b_gate_up"][0].reshape(32, 16, 128).transpose(2, 0, 1).reshape(128, 512), dtype=f),
        "w_down": np.ascontiguousarray(inp["w_down"][0], dtype=f),
        "b_down": np.ascontiguousarray(inp["b_down"][0], dtype=f),
    }
    return shared


def _core_inputs(inp, shared, b):
    m = dict(shared)
    xb = np.ascontiguousarray(inp["x"][b], dtype=np.float32)
    m["x"] = xb
    m["xT"] = np.ascontiguousarray(xb.T)
    m["mem"] = np.ascontiguousarray(inp["mem"][b], dtype=np.float32)
    return m


def kernel(**inputs):
    inp = {k: np.asarray(v) for k, v in inputs.items()}
    nb = inp["x"].shape[0]
    shared = _host_layout(inp, 0)
    in_maps = [_core_inputs(inp, shared, b) for b in range(nb)]
    nc = build()
    res = run_bass_kernel_spmd(nc, in_maps, core_ids=list(range(nb)))
    out = np.stack([np.asarray(r["out"]) for r in res.results], axis=0)
    return out.astype(np.float32, copy=False)
```

```python
import contextlib
import numpy as np
import concourse.bass as bass
import concourse.mybir as mybir
from concourse.bass_utils import run_bass_kernel_spmd

F32 = mybir.dt.float32
BF16 = mybir.dt.bfloat16
I32 = mybir.dt.int32
U32 = mybir.dt.uint32
AF = mybir.ActivationFunctionType
ALU = mybir.AluOpType

ENGS = ("pe", "act", "dve", "pool", "sp")


class Buf:
    __slots__ = ("name", "writers", "readers")

    def __init__(self, name=""):
        self.name = name
        self.writers = []
        self.readers = []


class Op:
    __slots__ = ("eng", "emit", "deps", "is_dma", "signal", "val", "sem", "idx")

    def __init__(self, eng, emit, is_dma):
        self.eng = eng
        self.emit = emit
        self.deps = []
        self.is_dma = is_dma
        self.signal = False
        self.val = None
        self.sem = None
        self.idx = None


class Sched:
    def __init__(self, nc, n_dma_sems=None):
        self.nc = nc
        self.streams = {e: [] for e in ENGS}
        self.n_dma_sems = n_dma_sems or {"sp": 8, "act": 2, "pool": 4}
        self.last = {}
        self.dmas_since_barrier = []

    @staticmethod
    def _same_inorder(a, b):
        return (not a.is_dma) and (not b.is_dma) and a.eng == b.eng

    def op(self, eng, emit, reads=(), writes=(), dma=False, extra_deps=()):
        o = Op(eng, emit, dma)
        o.idx = len(self.streams[eng])
        deps = list(extra_deps)
        for b in reads:
            deps.extend(b.writers)
        for b in writes:
            deps.extend(b.readers)
            deps.extend(b.writers)
        seen = set()
        for d in deps:
            if d is o or id(d) in seen:
                continue
            seen.add(id(d))
            if eng == "pe" and d.eng == "pe" and not d.is_dma and not dma:
                continue
            o.deps.append(d)
            d.signal = True
        for b in reads:
            b.readers = [r for r in b.readers if not self._same_inorder(r, o)] + [o]
        for b in writes:
            if b.readers:
                b.readers = []
                b.writers = []
            b.writers = [w for w in b.writers if not self._same_inorder(w, o)] + [o]
        self.streams[eng].append(o)
        if dma:
            self.dmas_since_barrier.append(o)
        else:
            self.last[eng] = o
        return o

    def barrier(self):
        deps = list(self.last.values()) + list(self.dmas_since_barrier)
        self.dmas_since_barrier = []
        for e in ENGS:
            self.op(e, lambda eng: eng.nop(), extra_deps=deps)

    def emit_all(self, final_waits=()):
        nc = self.nc
        with contextlib.ExitStack() as st:
            csem = {e: st.enter_context(nc.semaphore("c_" + e)) for e in ("pe", "act", "dve", "pool", "sp")}
            dsems = {e: [st.enter_context(nc.semaphore("d_%s%d" % (e, i))) for i in range(n)]
                     for e, n in self.n_dma_sems.items()}
            for e in ENGS:
                cnt = 0
                nd = self.n_dma_sems.get(e, 0)
                dcnt = [0] * max(nd, 1)
                rr = 0
                for o in self.streams[e]:
                    if o.is_dma:
                        s = rr % nd
                        rr += 1
                        dcnt[s] += 1
                        o.sem = dsems[e][s]
                        o.val = 16 * dcnt[s]
                    elif o.signal:
                        cnt += 1
                        o.sem = csem[e]
                        o.val = cnt
            fw = {}
            for (e, d) in final_waits:
                fw.setdefault(e, []).append(d)
            block = st.enter_context(nc.Block())
            streams = self.streams

            def make(e):
                def body(eng):
                    seen = {}

                    def wait(sem, val):
                        k = id(sem)
                        if seen.get(k, 0) < val:
                            eng.wait_ge(sem, val)
                            seen[k] = val

                    for o in streams[e]:
                        if o.is_dma and o.val > 16:
                            wait(o.sem, o.val - 16)
                        mx = {}
                        for d in o.deps:
                            k = id(d.sem)
                            if k not in mx or mx[k][1] < d.val:
                                mx[k] = (d.sem, d.val)
                        for sem_, val_ in mx.values():
                            wait(sem_, val_)
                        ins = o.emit(eng)
                        if o.is_dma:
                            ins.then_inc(o.sem, 16)
                        elif o.signal:
                            ins.then_inc(o.sem, 1)
                    for d in fw.get(e, ()):
                        wait(d.sem, d.val)
                return body

            for e, deco in (("pe", block.tensor), ("act", block.scalar), ("dve", block.vector),
                            ("pool", block.gpsimd), ("sp", block.sync)):
                if streams[e] or e in fw:
                    deco(make(e))


P = 128
D = 1024
SEQ = 4096
TT = 512
NCH = SEQ // P
NE = 32
CAP = 768
NST = CAP // P
NROWS = NE * CAP
ZROW = NROWS
ALPHA = 2.0 ** 0.25
EPS = 1e-5
SEG_KV = 0
SEG_A = 2048
SEG_B = SEG_A + 1024
SEG_C = SEG_B + 1024
SEG_D = SEG_C + 3072
SEG_E = SEG_D + 1024
SEG_F = SEG_E + 8 * 768
WCOLS = SEG_F + 1024


def build(n_tiles=SEQ // TT, n_exp=NE, debug=False):
    nc = bass.Bass("TRN2", target_bir_lowering=False)

    def din(name, shape, dt=F32):
        return nc.dram_tensor(name, list(shape), dt, kind="ExternalInput").ap()

    xT_d = din("xT", [D, SEQ])
    x_d = din("x", [SEQ, D])
    mem_d = din("mem", [256, D])
    wmix_d = din("wmix", [D, WCOLS])
    bgate_d = din("b_gate_T", [P, 24])
    convw_d = din("conv_w_T", [P, 24])
    wsT_d = din("gmlp_wsT", [P, 8 * P])
    gmlpb_d = din("gmlp_b", [1, D])
    glng_d = din("gmlp_ln_g", [1, D])
    glnb_d = din("gmlp_ln_b", [1, D])
    mlng_d = din("mem_ln_g", [1, D])
    mlnb_d = din("mem_ln_b", [1, D])
    l1g_d = din("ln1_g", [1, D])
    l1b_d = din("ln1_b", [1, D])
    l2g_d = din("ln2_g", [1, D])
    l2b_d = din("ln2_b", [1, D])
    wr_d = din("w_router", [D, NE])
    br_d = din("b_router", [1, NE])
    wgu_d = din("w_gate_up", [NE, D, 2 * D])
    bgu_d = din("b_gate_up_T", [P, NE * 16])
    wd_d = din("w_down", [NE, D, D])
    bd_d = din("b_down", [NE, D])
    out_d = nc.dram_tensor("out", [SEQ, D], F32, kind="ExternalOutput").ap()
    if debug:
        x1_scr = nc.dram_tensor("x1_dbg", [SEQ, D], F32, kind="ExternalOutput").ap()
    else:
        x1_scr = nc.dram_tensor("x1_scr", [SEQ, D], F32).ap()
    wmix_bf = nc.dram_tensor("wmix_bf", [D, WCOLS], BF16).ap()
    xs_scr = nc.dram_tensor("xs_scr", [NROWS, D], BF16).ap()
    eo_scr = nc.dram_tensor("eo_scr", [NROWS + 1, D], F32).ap()

    S = Sched(nc)
    out_dmas = []

    with contextlib.ExitStack() as st:
        NF = 52224
        big = st.enter_context(nc.sbuf_tensor("arena", [P, NF], F32))
        banks = [st.enter_context(nc.psum_tensor("bank%d" % i, [P, 512], F32)) for i in range(8)]
        bank_bufs = [Buf("bank%d" % i) for i in range(8)]
        bank_rr = [0]

        def bank():
            i = bank_rr[0] % 8
            bank_rr[0] += 1
            return banks[i][:, :], bank_bufs[i]

        top = [0]

        def alloc(ncols, dt=F32, shape=None):
            a = big[:, top[0]:top[0] + ncols]
            top[0] += ncols
            assert top[0] <= NF, "SBUF arena overflow %d" % top[0]
            if dt != F32:
                a = a.bitcast(dt)
            if shape is not None:
                names = " ".join("d%d" % i for i in range(len(shape)))
                kw = {"d%d" % i: s for i, s in enumerate(shape)}
                a = a.rearrange("p (%s) -> p %s" % (names, names), **kw)
            return a

        def TT_(eng, out, in0, in1, op, R, W):
            return S.op(eng, lambda e: e.tensor_tensor(out=out, in0=in0, in1=in1, op=op), R, W)

        def TS_(eng, out, in0, s1, s2, op0, op1, R, W):
            if op1 is None:
                return S.op(eng, lambda e: e.tensor_scalar(out=out, in0=in0, scalar1=s1, scalar2=None, op0=op0), R, W)
            return S.op(eng, lambda e: e.tensor_scalar(out=out, in0=in0, scalar1=s1, scalar2=s2, op0=op0, op1=op1), R, W)

        def STT_(eng, out, in0, sc, in1, op0, op1, R, W, accum=None):
            if accum is None:
                return S.op(eng, lambda e: e.scalar_tensor_tensor(out=out, in0=in0, scalar=sc, in1=in1, op0=op0, op1=op1), R, W)
            return S.op(eng, lambda e: e.scalar_tensor_tensor(out=out, in0=in0, scalar=sc, in1=in1, op0=op0, op1=op1,
                                                              accum_out=accum), R, W)

        def ACT_(out, in_, func, R, W, bias=None, scale=1.0, accum=None):
            kw = {}
            if bias is not None:
                kw["bias"] = bias
            if accum is not None:
                kw["accum_out"] = accum
            return S.op("act", lambda e: e.activation(out=out, in_=in_, func=func, scale=scale, **kw), R, W)

        def CP_(eng, out, in_, R, W):
            if eng == "act":
                return S.op("act", lambda e: e.copy(out=out, in_=in_), R, W)
            return S.op(eng, lambda e: e.tensor_copy(out=out, in_=in_), R, W)

        def MS_(eng, ap, val, W):
            return S.op(eng, lambda e: e.memset(ap, val), (), W)

        def MM_(out, lhsT, rhs, start, stop, R, W):
            return S.op("pe", lambda e: e.matmul(out, lhsT=lhsT, rhs=rhs, start=start, stop=stop), R, W)

        def TR_(out, in_, ident, R, W):
            return S.op("pe", lambda e: e.transpose(out=out, in_=in_, identity=ident), R, W)

        def DMA_(eng, out, in_, R, W):
            return S.op(eng, lambda e: e.dma_start(out=out, in_=in_), R, W, dma=True)

        def layer_norm(xin, b_x, g_bc, b_bc, b_par, out, b_out, tmp, b_tmp, aff="pool"):
            st6, mv, rstd = tmp
            for h in range(2):
                S.op("dve", lambda e, h=h: e.bn_stats(out=st6[:, h, :], in_=xin[:, h * 512:(h + 1) * 512]), [b_x], [b_tmp])
            S.op("dve", lambda e: e.bn_aggr(out=mv, in_=st6.rearrange("p a b -> p (a b)")), [b_tmp], [b_tmp])
            ACT_(rstd, mv[:, 1:2], AF.Sqrt, [b_tmp, b_eps], [b_tmp], bias=eps_t)
            S.op("dve", lambda e: e.reciprocal(out=rstd, in_=rstd), [b_tmp], [b_tmp])
            TS_("dve", xin, xin, mv[:, 0:1], rstd, ALU.subtract, ALU.mult, [b_x, b_tmp], [b_x])
            TT_(aff, xin, xin, g_bc, ALU.mult, [b_x, b_par], [b_x])
            TT_(aff, out, xin, b_bc, ALU.add, [b_x, b_par], [b_out])

        identf = alloc(128); b_identf = Buf()
        identb = alloc(64, BF16); b_identb = Buf()
        onesb = alloc(64, BF16); b_onesb = Buf()
        ustrb = alloc(64, BF16); b_ustr = Buf()
        eps_t = alloc(1); b_eps = Buf()
        iota32 = alloc(32); b_iota = Buf()
        wr_sb = alloc(8 * NE, F32, (8, NE)); b_wr = Buf()
        br_bc = alloc(NE); b_br = Buf()
        gate_all = alloc(NCH * 4, F32, (NCH, 4)); b_gate_all = [Buf() for _ in range(NCH)]
        dst_all = alloc(NCH * 4, I32, (NCH, 4)); b_dst_all = [Buf() for _ in range(NCH)]
        tot_bc = alloc(NE); b_tot = Buf()
        bgu_sb = alloc(NE * 16); b_bgu = Buf()
        lnC_g = alloc(D); lnC_b = alloc(D); b_lnC = Buf()
        zrow = alloc(D); b_zrow = Buf()
        lnt = (alloc(12, F32, (2, 6)), alloc(2), alloc(1)); b_lnt = Buf()
        persist_top = top[0]
        WcT = alloc(512, BF16, (8, P)); b_WcT = Buf()
        biasbc = alloc(D, F32, (8, P)); b_biasbc = Buf()
        KT = alloc(1024, BF16, (8, 256)); b_KT = Buf()
        Vt = alloc(1024, BF16, (2, D)); b_Vt = Buf()
        bgate = alloc(24); b_bgate = Buf()
        convw = alloc(24); b_convw = Buf()
        halo = alloc(16, F32, (8, 2)); b_halo = [Buf() for _ in range(8)]
        gln_g = alloc(D); gln_b = alloc(D); b_gln = Buf()
        l1_g = alloc(D); l1_b = alloc(D); b_l1 = Buf()
        mixer_base = top[0]

        MS_("pool", identf, 0.0, [b_identf])
        S.op("pool", lambda e: e.affine_select(out=identf, in_=identf, pattern=[[-1, P]], compare_op=ALU.not_equal,
                                               fill=1.0, base=0, channel_multiplier=1), [b_identf], [b_identf])
        CP_("dve", identb, identf, [b_identf], [b_identb])
        MS_("pool", onesb, 1.0, [b_onesb])
        ustrf = alloc(128); b_ustrf = Buf()
        MS_("pool", ustrf, 1.0, [b_ustrf])
        S.op("pool", lambda e: e.affine_select(out=ustrf, in_=ustrf, pattern=[[1, P]], compare_op=ALU.is_gt,
                                               fill=0.0, base=0, channel_multiplier=-1), [b_ustrf], [b_ustrf])
        CP_("dve", ustrb, ustrf, [b_ustrf], [b_ustr])
        MS_("pool", eps_t, EPS, [b_eps])
        S.op("pool", lambda e: e.iota(iota32, pattern=[[1, NE]], base=0, channel_multiplier=0,
                                      allow_small_or_imprecise_dtypes=True), (), [b_iota])
        MS_("pool", tot_bc, 0.0, [b_tot])
        MS_("pool", halo.rearrange("p a b -> p (a b)"), 0.0, b_halo)
        MS_("pool", zrow, 0.0, [b_zrow])
        b_eo = Buf("eo_scr")
        DMA_("sp", eo_scr[ZROW:ZROW + 1, :], zrow[0:1, :], [b_zrow], [b_eo])
        DMA_("sp", wr_sb, wr_d.rearrange("(k p) n -> p k n", p=P), (), [b_wr])
        DMA_("sp", br_bc, br_d.partition_broadcast(P), (), [b_br])
        DMA_("sp", bgu_sb, bgu_d, (), [b_bgu])
        DMA_("sp", lnC_g, mlng_d.partition_broadcast(P), (), [b_lnC])
        DMA_("sp", lnC_b, mlnb_d.partition_broadcast(P), (), [b_lnC])
        DMA_("sp", biasbc.rearrange("p a b -> p (a b)"), gmlpb_d.partition_broadcast(P), (), [b_biasbc])
        DMA_("sp", bgate, bgate_d, (), [b_bgate])
        DMA_("sp", convw, convw_d, (), [b_convw])
        DMA_("sp", gln_g, glng_d.partition_broadcast(P), (), [b_gln])
        DMA_("sp", gln_b, glnb_d.partition_broadcast(P), (), [b_gln])
        DMA_("sp", l1_g, l1g_d.partition_broadcast(P), (), [b_l1])
        DMA_("sp", l1_b, l1b_d.partition_broadcast(P), (), [b_l1])
        wsTf = alloc(8 * P, F32, (8, P)); b_wsTf = Buf()
        DMA_("sp", wsTf.rearrange("p a b -> p (a b)"), wsT_d, (), [b_wsTf])
        S.op("pool", lambda e: e.affine_select(out=wsTf, in_=wsTf, pattern=[[0, 8], [1, P]], compare_op=ALU.is_ge,
                                               fill=0.0, base=0, channel_multiplier=-1), [b_wsTf], [b_wsTf])
        CP_("dve", WcT, wsTf, [b_wsTf], [b_WcT])
        const_top = top[0]

        NSLOT = 4
        ring = [alloc(2048, BF16, (8, 512)) for _ in range(NSLOT)]
        ring_bufs = [Buf("slab%d" % i) for i in range(NSLOT)]

        slab_list = []
        for c in range(4):
            slab_list.append((SEG_KV + c * 512, 512))
        for _ in range(n_tiles):
            slab_list += [(SEG_A, 512), (SEG_A + 512, 512)]
            slab_list += [(SEG_C + c * 384, 384) for c in range(8)]
            slab_list += [(SEG_B, 512), (SEG_B + 512, 512)]
            slab_list += [(SEG_D, 512), (SEG_D + 512, 512)]
            for dc in range(8):
                slab_list += [(SEG_E + dc * 768, 384), (SEG_E + dc * 768 + 384, 384)]
            slab_list += [(SEG_F, 512), (SEG_F + 512, 512)]
        slab_issued = [0]
        slab_next = [0]

        b_conv = {}

        def issue_slab():
            i = slab_issued[0]
            if i >= len(slab_list):
                return
            c0, w = slab_list[i]
            sl = i % NSLOT
            if c0 not in b_conv:
                DMA_("pool", ring[sl][:, :, 0:w], wmix_d[:, c0:c0 + w].rearrange("(k p) n -> p k n", p=P), (), [ring_bufs[sl]])
                b_conv[c0] = Buf()
                if c0 >= SEG_A and n_tiles > 1:
                    DMA_("sp", wmix_bf[:, c0:c0 + w].rearrange("(k p) n -> p k n", p=P), ring[sl][:, :, 0:w],
                         [ring_bufs[sl]], [b_conv[c0]])
            else:
                DMA_("sp", ring[sl][:, :, 0:w], wmix_bf[:, c0:c0 + w].rearrange("(k p) n -> p k n", p=P), [b_conv[c0]], [ring_bufs[sl]])
            slab_issued[0] += 1

        def get_slab():
            i = slab_next[0]
            slab_next[0] += 1
            while slab_issued[0] < min(i + NSLOT - 1, len(slab_list)):
                issue_slab()
            sl = i % NSLOT
            return ring[sl], ring_bufs[sl]

        memx = [alloc(D) for _ in range(2)]; b_memx = [Buf() for _ in range(2)]
        memnT = alloc(1024, BF16, (8, 256)); b_memnT = Buf()
        for j in range(2):
            DMA_("sp", memx[j], mem_d[j * P:(j + 1) * P, :], (), [b_memx[j]])
            layer_norm(memx[j], b_memx[j], lnC_g, lnC_b, b_lnC, memx[j], b_memx[j], lnt, b_lnt)
            for hh in range(2):
                pb, bb = bank()
                for kk in range(4):
                    k = hh * 4 + kk
                    TR_(pb[:, kk * P:(kk + 1) * P], memx[j][:, k * P:(k + 1) * P], identf, [b_memx[j], b_identf], [bb])
                CP_("act", memnT[:, hh * 4:(hh + 1) * 4, j * P:(j + 1) * P], pb.rearrange("p (a b) -> p a b", a=4), [bb], [b_memnT])
        for c in range(2):
            slab, sb_ = get_slab()
            for dd in range(4):
                dchunk = c * 4 + dd
                pb, bb = bank()
                for k in range(8):
                    MM_(pb[:, 0:256], slab[:, k, dd * P:(dd + 1) * P], memnT[:, k, :], k == 0, k == 7, [sb_, b_memnT], [bb])
                CP_("act", KT[:, dchunk, :], pb[:, 0:256], [bb], [b_KT])
        for c in range(2):
            slab, sb_ = get_slab()
            for mc in range(2):
                pb, bb = bank()
                for k in range(8):
                    MM_(pb, memnT[:, k, mc * P:(mc + 1) * P], slab[:, k, :], k == 0, k == 7, [sb_, b_memnT], [bb])
                CP_("dve", Vt[:, mc, c * 512:(c + 1) * 512], pb, [bb], [b_Vt])

        top[0] = const_top + NSLOT * 2048
        xT_bf = alloc(2048, BF16, (8, TT)); b_xT = Buf()
        vn_mg = alloc(2048, BF16)
        v_n = vn_mg.rearrange("p (a b) -> p a b", a=4)
        merged = vn_mg.rearrange("p (a b) -> p a b", a=8)
        b_vnmg = Buf()
        vg = [alloc(D) for _ in range(2)]; b_vg = [Buf() for _ in range(2)]
        ug = [alloc(TT) for _ in range(2)]; b_ug = [Buf() for _ in range(2)]
        ftmp = [alloc(TT) for _ in range(2)]; b_ftmp = [Buf() for _ in range(2)]
        y_conv = alloc(2048, BF16, (8, TT)); b_yconv = Buf()
        y_gmlp = alloc(2048, BF16, (8, TT)); b_ygmlp = Buf()
        y_xa = alloc(2048, BF16, (8, TT)); b_yxa = Buf()
        cc_sb = alloc(TT); b_cc = Buf()
        u_t = [alloc(TT + 4) for _ in range(2)]; b_u = [Buf() for _ in range(2)]
        acc = alloc(TT); b_acc = Buf()
        qT = [alloc(512, BF16, (2, TT)) for _ in range(2)]; b_qT = [Buf() for _ in range(2)]
        PT = [alloc(512, BF16, (2, TT)) for _ in range(2)]; b_PT = [Buf() for _ in range(2)]
        rden = [alloc(TT) for _ in range(2)]; b_rden = [Buf() for _ in range(2)]
        sig = [alloc(3 * TT, F32, (3, TT)) for _ in range(2)]; b_sig = [Buf() for _ in range(2)]
        mt = alloc(3 * TT, F32, (3, TT)); b_mt = Buf()
        xc = [alloc(D) for _ in range(2)]; b_xc = [Buf() for _ in range(2)]
        r2 = [alloc(D) for _ in range(2)]; b_r2 = [Buf() for _ in range(2)]
        x1bf = [alloc(512, BF16) for _ in range(2)]; b_x1bf = [Buf() for _ in range(2)]
        x1T = alloc(D, F32, (8, P)); b_x1T = Buf()
        lgt = alloc(NE); mx8 = alloc(8); mi8 = alloc(8, U32); ef4 = alloc(4); negmax = alloc(1)
        ex4 = alloc(4); ssum = alloc(1); Mbf = alloc(NE // 2, BF16); pos = alloc(NE); junk = alloc(NE)
        pos4 = alloc(4); dstf = alloc(4); keep = alloc(4)
        b_rt = Buf()
        b_x1scr = [Buf() for _ in range(NCH)]
        b_xs = Buf("xs_scr")
        bc_reg = {}

        S.barrier()

        for ti in range(n_tiles):
            t0 = ti * TT
            if ti == 0:
                DMA_("pool", xT_bf, xT_d[:, t0:t0 + TT].rearrange("(k p) t -> p k t", p=P), (), [b_xT])

            slabA = [get_slab(), get_slab()]
            for j in range(4):
                vgj, bvg = vg[j % 2], b_vg[j % 2]
                for h in range(2):
                    pb, bb = bank()
                    sl, sb_ = slabA[h]
                    for k in range(8):
                        MM_(pb, xT_bf[:, k, j * P:(j + 1) * P], sl[:, k, :], k == 0, k == 7, [b_xT, sb_], [bb])
                    ACT_(vgj[:, h * 512:(h + 1) * 512], pb, AF.Gelu_apprx_tanh, [bb], [bvg])
                layer_norm(vgj, bvg, gln_g, gln_b, b_gln, v_n[:, j, :], b_vnmg, lnt, b_lnt)

            for c in range(8):
                sl, sb_ = get_slab()
                pcs = []
                for r in range(3):
                    pb, bb = bank()
                    for k in range(8):
                        MM_(pb, sl[:, k, r * P:(r + 1) * P], xT_bf[:, k, :], k == 0, k == 7, [sb_, b_xT], [bb])
                    pcs.append((pb, bb))
                (pcb, bcb), (pcc, bcc), (pch, bch) = pcs
                u, bu = u_t[c % 2], b_u[c % 2]
                CP_("act", cc_sb, pcc, [bcc], [b_cc])
                CP_("pool", u[:, 0:2], halo[:, c, :], [b_halo[c]], [bu])
                TT_("dve", u[:, 2:TT + 2], cc_sb, pch, ALU.mult, [b_cc, bch], [bu])
                CP_("pool", halo[:, c, :], u[:, TT:TT + 2], [bu], [b_halo[c]])
                TS_("dve", acc, u[:, 2:TT + 2], convw[:, 16 + c:17 + c], None, ALU.mult, None, [bu, b_convw], [b_acc])
                STT_("dve", acc, u[:, 1:TT + 1], convw[:, 8 + c:9 + c], acc, ALU.mult, ALU.add, [bu, b_convw, b_acc], [b_acc])
                STT_("dve", acc, u[:, 0:TT], convw[:, c:c + 1], acc, ALU.mult, ALU.add, [bu, b_convw, b_acc], [b_acc])
                TT_("dve", y_conv[:, c, :], acc, pcb, ALU.mult, [b_acc, bcb], [b_yconv])

            slabB = [get_slab(), get_slab()]
            for g in range(8):
                sl, sb_ = slabB[g // 4]
                gg = g % 4
                pu, bpu = bank()
                for k in range(8):
                    MM_(pu, sl[:, k, gg * P:(gg + 1) * P], xT_bf[:, k, :], k == 0, k == 7, [sb_, b_xT], [bpu])
                ACT_(ug[g % 2], pu, AF.Gelu_apprx_tanh, [bpu], [b_ug[g % 2]])
                pf, bpf = bank()
                for j in range(4):
                    MM_(pf[:, j * P:(j + 1) * P], v_n[:, j, g * P:(g + 1) * P], WcT[:, g, :], True, True, [b_vnmg, b_WcT], [bpf])
                TT_("dve", ftmp[g % 2].rearrange("p (j t) -> p j t", j=4), pf.rearrange("p (j t) -> p j t", j=4),
                    biasbc[:, g, :].unsqueeze(1).broadcast_to([P, 4, P]), ALU.add, [bpf, b_biasbc], [b_ftmp[g % 2]])
                TT_("pool", y_gmlp[:, g, :], ftmp[g % 2], ug[g % 2], ALU.mult, [b_ftmp[g % 2], b_ug[g % 2]], [b_ygmlp])

            slabD = [get_slab(), get_slab()]
            for h in range(4):
                sl, sb_ = slabD[h // 2]
                q, bq = qT[h % 2], b_qT[h % 2]
                for dc in range(2):
                    cc = (h % 2) * 2 + dc
                    pb, bb = bank()
                    for k in range(8):
                        MM_(pb, sl[:, k, cc * P:(cc + 1) * P], xT_bf[:, k, :], k == 0, k == 7, [sb_, b_xT], [bb])
                    CP_("act", q[:, dc, :], pb, [bb], [bq])
                pt, bpt = PT[h % 2], b_PT[h % 2]
                for mc in range(2):
                    pb, bb = bank()
                    for dc in range(2):
                        MM_(pb, KT[:, h * 2 + dc, mc * P:(mc + 1) * P], q[:, dc, :], dc == 0, dc == 1, [b_KT, bq], [bb])
                    ACT_(pt[:, mc, :], pb, AF.Exp, [bb], [bpt], scale=0.0625)
                pden, bden = bank()
                for mc in range(2):
                    MM_(pden, onesb, pt[:, mc, :], mc == 0, mc == 1, [b_onesb, bpt], [bden])
                rd, brd = rden[h % 2], b_rden[h % 2]
                S.op("dve", lambda e, rd=rd, pden=pden: e.reciprocal(out=rd, in_=pden), [bden], [brd])
                for dc in range(2):
                    po, bpo = bank()
                    for mc in range(2):
                        MM_(po, Vt[:, mc, (h * 2 + dc) * P:(h * 2 + dc + 1) * P], pt[:, mc, :], mc == 0, mc == 1, [b_Vt, bpt], [bpo])
                    TT_("dve", y_xa[:, h * 2 + dc, :], rd, po, ALU.mult, [brd, bpo], [b_yxa])

            ys = [(y_conv, b_yconv), (y_gmlp, b_ygmlp), (y_xa, b_yxa)]
            for dc in range(8):
                slg, sbg = get_slab()
                slp, sbp = get_slab()
                sg, bsg = sig[dc % 2], b_sig[dc % 2]
                for r in range(3):
                    pb, bb = bank()
                    for k in range(8):
                        MM_(pb, slg[:, k, r * P:(r + 1) * P], xT_bf[:, k, :], k == 0, k == 7, [sbg, b_xT], [bb])
                    ACT_(sg[:, r, :], pb, AF.Sigmoid, [bb, b_bgate], [bsg], bias=bgate[:, r * 8 + dc:r * 8 + dc + 1])
                for r in range(3):
                    pb, bb = bank()
                    yr, byr = ys[r]
                    for k in range(8):
                        MM_(pb, slp[:, k, r * P:(r + 1) * P], yr[:, k, :], k == 0, k == 7, [sbp, byr], [bb])
                    TT_("dve", mt[:, r, :], sg[:, r, :], pb, ALU.mult, [bsg, bb], [b_mt])
                TT_("pool", mt[:, 0, :], mt[:, 0, :], mt[:, 1, :], ALU.add, [b_mt], [b_mt])
                TT_("pool", merged[:, dc, :], mt[:, 0, :], mt[:, 2, :], ALU.add, [b_mt], [b_vnmg])

            if ti + 1 < n_tiles:
                DMA_("pool", xT_bf, xT_d[:, t0 + TT:t0 + 2 * TT].rearrange("(k p) t -> p k t", p=P), (), [b_xT])

            slabF = [get_slab(), get_slab()]

            wbanks = {}

            def stage1_mm(j):
                ci = ti * 4 + j
                xcj, bxc = xc[j % 2], b_xc[j % 2]
                DMA_("sp", xcj, x_d[ci * P:(ci + 1) * P, :], (), [bxc])
                wbanks[j] = []
                for h in range(2):
                    pb, bb = bank()
                    sl, sb_ = slabF[h]
                    for k in range(8):
                        MM_(pb, merged[:, k, j * P:(j + 1) * P], sl[:, k, :], k == 0, k == 7, [b_vnmg, sb_], [bb])
                    wbanks[j].append((pb, bb))

            def stage1_ew(j):
                ci = ti * 4 + j
                r_t, b_r = r2[j % 2], b_r2[j % 2]
                xcj, bxc = xc[j % 2], b_xc[j % 2]
                for h in range(2):
                    pb, bb = wbanks[j][h]
                    STT_("dve", r_t[:, h * 512:(h + 1) * 512], xcj[:, h * 512:(h + 1) * 512], ALPHA, pb, ALU.mult, ALU.add,
                         [bxc, bb], [b_r])
                layer_norm(r_t, b_r, l1_g, l1_b, b_l1, r_t, b_r, lnt, b_lnt)
                DMA_("sp", x1_scr[ci * P:(ci + 1) * P, :], r_t, [b_r], [b_x1scr[ci]])
                xb, bxb = x1bf[j % 2], b_x1bf[j % 2]
                CP_("act", xb, r_t, [b_r], [bxb])

            def stage2(j):
                ci = ti * 4 + j
                r_t, b_r = r2[j % 2], b_r2[j % 2]
                xb, bxb = x1bf[j % 2], b_x1bf[j % 2]
                for hh in range(2):
                    pb, bb = bank()
                    for kk in range(4):
                        k = hh * 4 + kk
                        TR_(pb[:, kk * P:(kk + 1) * P], r_t[:, k * P:(k + 1) * P], identf, [b_r, b_identf], [bb])
                    CP_("act", x1T[:, hh * 4:(hh + 1) * 4, :], pb.rearrange("p (a b) -> p a b", a=4), [bb], [b_x1T])
                pl, bpl = bank()
                for k in range(8):
                    MM_(pl[:, 0:NE], x1T[:, k, :], wr_sb[:, k, :], k == 0, k == 7, [b_x1T, b_wr], [bpl])
                TT_("dve", lgt, pl[:, 0:NE], br_bc, ALU.add, [bpl, b_br], [b_rt])
                S.op("dve", lambda e: e.max(out=mx8, in_=lgt), [b_rt], [b_rt])
                S.op("dve", lambda e: e.max_index(out=mi8, in_max=mx8, in_values=lgt), [b_rt], [b_rt])
                TS_("dve", negmax, mx8[:, 0:1], -1.0, None, ALU.mult, None, [b_rt], [b_rt])
                MS_("pool", ssum, 0.0, [b_rt])
                MS_("pool", pos4, 0.0, [b_rt])
                ACT_(ex4, mx8[:, 0:4], AF.Exp, [b_rt], [b_rt], bias=negmax, accum=ssum)
                S.op("dve", lambda e: e.reciprocal(out=ssum, in_=ssum), [b_rt], [b_rt])
                TS_("dve", gate_all[:, ci, :], ex4, ssum, None, ALU.mult, None, [b_rt], [b_gate_all[ci]])
                TS_("dve", Mbf, lgt, mx8[:, 3:4], None, ALU.is_ge, None, [b_rt], [b_rt])
                pp, bpp = bank()
                MM_(pp[:, 0:NE], ustrb, Mbf, True, True, [b_ustr, b_rt], [bpp])
                MM_(pp[:, NE:2 * NE], onesb, Mbf, True, True, [b_onesb, b_rt], [bpp])
                TT_("dve", pos, pp[:, 0:NE], tot_bc, ALU.add, [bpp, b_tot], [b_rt])
                TT_("dve", tot_bc, pp[:, NE:2 * NE], tot_bc, ALU.add, [bpp, b_tot], [b_tot])
                CP_("dve", ef4, mi8[:, 0:4], [b_rt], [b_rt])
                for k in range(4):
                    STT_("dve", junk, iota32, ef4[:, k:k + 1], pos, ALU.is_equal, ALU.mult, [b_iota, b_rt], [b_rt],
                         accum=pos4[:, k:k + 1])
                STT_("dve", dstf, ef4, float(CAP), pos4, ALU.mult, ALU.add, [b_rt], [b_rt])
                TS_("dve", keep, pos4, float(CAP), None, ALU.is_lt, None, [b_rt], [b_rt])
                TS_("dve", dstf, dstf, -float(ZROW), None, ALU.add, None, [b_rt], [b_rt])
                TT_("dve", dstf, dstf, keep, ALU.mult, [b_rt], [b_rt])
                TS_("dve", dstf, dstf, float(ZROW), None, ALU.add, None, [b_rt], [b_rt])
                CP_("dve", dst_all[:, ci, :], dstf, [b_rt], [b_dst_all[ci]])
                for k in range(4):
                    def scat(e, ci=ci, k=k, xb=xb):
                        if "r" not in bc_reg:
                            bc_reg["r"] = e.to_reg(NROWS - 1)
                        return e.indirect_dma_start(
                            out=xs_scr, out_offset=bass.IndirectOffsetOnAxis(ap=dst_all[:, ci, k:k + 1], axis=0),
                            in_=xb, in_offset=None, bounds_check=bc_reg["r"], oob_is_err=False)
                    S.op("pool", scat, [bxb, b_dst_all[ci]], [b_xs], dma=True)

            stage1_mm(0)
            stage1_ew(0)
            for j in range(4):
                if j + 1 < 4:
                    stage1_mm(j + 1)
                stage2(j)
                if j + 1 < 4:
                    stage1_ew(j + 1)

        S.barrier()

        top[0] = persist_top
        DMA_("sp", lnC_g, l2g_d.partition_broadcast(P), (), [b_lnC])
        DMA_("sp", lnC_b, l2b_d.partition_broadcast(P), (), [b_lnC])
        wgu = [alloc(8192, BF16, (8, 2 * D)) for _ in range(2)]
        b_wgu = [[Buf() for _ in range(8)] for _ in range(2)]
        wdn = [alloc(4096, BF16, (8, D)) for _ in range(2)]
        b_wdn = [[Buf() for _ in range(8)] for _ in range(2)]
        xs_sm = [alloc(512, BF16) for _ in range(NST)]; b_xssm = [Buf() for _ in range(NST)]
        xsT2 = [alloc(4 * CAP, BF16, (8, CAP)) for _ in range(2)]; b_xsT2 = [[Buf() for _ in range(NST)] for _ in range(2)]
        actT = alloc(4 * CAP, BF16, (8, CAP)); b_actT = Buf()
        g_sb = [alloc(512) for _ in range(2)]; b_g = [Buf() for _ in range(2)]
        s_sb = [alloc(512) for _ in range(2)]; b_s = [Buf() for _ in range(2)]
        u_sb = [alloc(512) for _ in range(2)]; b_us = [Buf() for _ in range(2)]
        eo_sb = [alloc(D) for _ in range(2)]; b_eosb = [Buf() for _ in range(2)]
        bdbc = [alloc(D) for _ in range(2)]; b_bdbc = [Buf() for _ in range(2)]
        bgu1 = alloc(NE * 16); b_bgu1 = Buf()
        TS_("dve", bgu1, bgu_sb, 1.0, None, ALU.add, None, [b_bgu], [b_bgu1])
        bankbf2 = [banks[6][:, :].bitcast(BF16), banks[7][:, :].bitcast(BF16)]

        def load_expert(e):
            bi = e % 2
            for k in range(8):
                DMA_("pool", wgu[bi][:, k, :], wgu_d[e, k * P:(k + 1) * P, :], (), [b_wgu[bi][k]])
            for k in range(8):
                DMA_("pool", wdn[bi][:, k, :], wd_d[e, k * P:(k + 1) * P, :], (), [b_wdn[bi][k]])
            DMA_("sp", bdbc[bi], bd_d[e:e + 1, :].partition_broadcast(P), (), [b_bdbc[bi]])

        def prefetch_xs(e):
            for s in range(NST):
                DMA_("sp", xs_sm[s], xs_scr[e * CAP + s * P:e * CAP + (s + 1) * P, :], [b_xs], [b_xssm[s]])

        def transposes(e):
            xsT, b_xsT = xsT2[e % 2], b_xsT2[e % 2]
            for s in range(NST):
                xm, bxm = xs_sm[s], b_xssm[s]
                bankbf, bbf = bankbf2[s % 2], bank_bufs[6 + s % 2]
                for k in range(8):
                    TR_(bankbf[:, k * P:(k + 1) * P], xm[:, k * P:(k + 1) * P], identb, [bxm, b_identb], [bbf])
                CP_("act", xsT[:, :, s * P:(s + 1) * P], bankbf.rearrange("p (a b) -> p a b", a=8), [bbf], [b_xsT[s]])

        splits = [(0, 512), (512, CAP)]
        split_tiles = [[s for s in range(NST) if n0 <= s * P < n1] for (n0, n1) in splits]
        if n_exp > 0:
            load_expert(0)
            prefetch_xs(0)
            transposes(0)
        cnt = 0
        for e in range(n_exp):
            bi = e % 2
            if e + 1 < n_exp:
                load_expert(e + 1)
                prefetch_xs(e + 1)
            xsT, b_xsT = xsT2[bi], b_xsT2[bi]
            for (n0, n1), tiles in zip(splits, split_tiles):
                w = n1 - n0
                rb = [b_xsT[s] for s in tiles]
                for f in range(8):
                    pgs = []
                    for m in (f, 8 + f):
                        i = bank_rr[0] % 6
                        bank_rr[0] += 1
                        pb, bb = banks[i][:, :], bank_bufs[i]
                        for k in range(8):
                            MM_(pb[:, 0:w], wgu[bi][:, k, m * P:(m + 1) * P], xsT[:, k, n0:n1], k == 0, k == 7,
                                [b_wgu[bi][k]] + rb, [bb])
                        pgs.append((pb, bb))
                    (pg, bpg), (pu, bpu) = pgs
                    gi = cnt % 2
                    cnt += 1
                    gs, us, ss = g_sb[gi][:, 0:w], u_sb[gi][:, 0:w], s_sb[gi][:, 0:w]
                    TS_("dve", gs, pg[:, 0:w], bgu_sb[:, e * 16 + f:e * 16 + f + 1], 7.0, ALU.add, ALU.min, [bpg, b_bgu], [b_g[gi]])
                    ACT_(ss, gs, AF.Sigmoid, [b_g[gi]], [b_s[gi]], scale=1.702)
                    TS_("dve", us, pu[:, 0:w], bgu1[:, e * 16 + 8 + f:e * 16 + 9 + f], 8.0, ALU.add, ALU.min, [bpu, b_bgu1], [b_us[gi]])
                    TT_("dve", gs, gs, ss, ALU.mult, [b_g[gi], b_s[gi]], [b_g[gi]])
                    STT_("dve", actT[:, f, n0:n1], us, -6.0, gs, ALU.max, ALU.mult, [b_us[gi], b_g[gi]], [b_actT])
            if e + 1 < n_exp:
                transposes(e + 1)
            for s in range(NST):
                eo, beo = eo_sb[s % 2], b_eosb[s % 2]
                for h in range(2):
                    i = bank_rr[0] % 6
                    bank_rr[0] += 1
                    pb, bb = banks[i][:, :], bank_bufs[i]
                    for f in range(8):
                        MM_(pb, actT[:, f, s * P:(s + 1) * P], wdn[bi][:, f, h * 512:(h + 1) * 512], f == 0, f == 7,
                            [b_actT, b_wdn[bi][f]], [bb])
                    TT_("dve", eo[:, h * 512:(h + 1) * 512], pb, bdbc[bi][:, h * 512:(h + 1) * 512], ALU.add, [bb, b_bdbc[bi]], [beo])
                DMA_("sp", eo_scr[e * CAP + s * P:e * CAP + (s + 1) * P, :], eo, [beo], [b_eo])

        S.barrier()

        top[0] = persist_top
        NB4 = 3
        eo4 = [alloc(4 * D, F32, (4, D)) for _ in range(NB4)]; b_eo4 = [Buf() for _ in range(NB4)]
        x1c = [alloc(D) for _ in range(NB4)]; b_x1c = [Buf() for _ in range(NB4)]
        accd = [alloc(D) for _ in range(2)]; b_accd = [Buf() for _ in range(2)]
        lnt2 = (alloc(12, F32, (2, 6)), alloc(2), alloc(1)); b_lnt2 = Buf()
        n_ch = n_tiles * 4

        def fetch(ci):
            e4, be4 = eo4[ci % NB4], b_eo4[ci % NB4]
            for k in range(4):
                S.op("pool", lambda e, ci=ci, k=k, e4=e4: e.indirect_dma_start(
                    out=e4[:, k, :], out_offset=None, in_=eo_scr,
                    in_offset=bass.IndirectOffsetOnAxis(ap=dst_all[:, ci, k:k + 1], axis=0)),
                    [b_eo, b_dst_all[ci]], [be4], dma=True)
            DMA_("sp", x1c[ci % NB4], x1_scr[ci * P:(ci + 1) * P, :], [b_x1scr[ci]], [b_x1c[ci % NB4]])

        for ci in range(min(NB4 - 1, n_ch)):
            fetch(ci)
        for ci in range(n_ch):
            if ci + NB4 - 1 < n_ch:
                fetch(ci + NB4 - 1)
            e4, be4 = eo4[ci % NB4], b_eo4[ci % NB4]
            xx, bxx = x1c[ci % NB4], b_x1c[ci % NB4]
            a, ba = accd[ci % 2], b_accd[ci % 2]
            TS_("dve", a, e4[:, 0, :], gate_all[:, ci, 0:1], None, ALU.mult, None, [be4, b_gate_all[ci]], [ba])
            for k in range(1, 4):
                STT_("dve", a, e4[:, k, :], gate_all[:, ci, k:k + 1], a, ALU.mult, ALU.add, [be4, b_gate_all[ci], ba], [ba])
            STT_("dve", a, xx, ALPHA, a, ALU.mult, ALU.add, [bxx, ba], [ba])
            layer_norm(a, ba, lnC_g, lnC_b, b_lnC, a, ba, lnt2, b_lnt2, aff="dve")
            out_dmas.append(DMA_("sp", out_d[ci * P:(ci + 1) * P, :], a, [ba], ()))

        S.emit_all(final_waits=[("sp", d) for d in out_dmas])
    return nc


def _host_layout(inp, b):
    f = np.float32
    w_in = inp["w_in"][0]
    cb, cc, ch, gu, gv, q, gates = np.split(w_in, np.cumsum([1024, 1024, 1024, 1024, 1024, 1024])[:], axis=1)
    segs = [inp["w_kv"][0], gv, gu]
    for c in range(8):
        sl = slice(c * 128, (c + 1) * 128)
        segs += [cb[:, sl], cc[:, sl], ch[:, sl]]
    segs.append(q)
    wcp, wgp, wxp = inp["w_conv_proj"][0], inp["w_gmlp_proj"][0], inp["w_xa_proj"][0]
    for dc in range(8):
        sl = slice(dc * 128, (dc + 1) * 128)
        segs += [gates[:, 0:1024][:, sl], gates[:, 1024:2048][:, sl], gates[:, 2048:3072][:, sl], wcp[:, sl], wgp[:, sl], wxp[:, sl]]
    segs.append(inp["w_out"][0])
    wmix = np.ascontiguousarray(np.concatenate(segs, axis=1), dtype=f)
    assert wmix.shape == (D, WCOLS)
    shared = {
        "wmix": wmix,
        "b_gate_T": np.ascontiguousarray(inp["b_gate"][0].reshape(24, 128).T, dtype=f),
        "conv_w_T": np.ascontiguousarray(inp["conv_w"][0].reshape(3, 8, 128).transpose(2, 0, 1).reshape(128, 24), dtype=f),
        "gmlp_wsT": np.ascontiguousarray(inp["gmlp_ws"][0].transpose(2, 0, 1).reshape(128, 8 * 128), dtype=f),
        "gmlp_b": np.ascontiguousarray(inp["gmlp_b"][0].reshape(1, D), dtype=f),
        "gmlp_ln_g": np.ascontiguousarray(inp["gmlp_ln_g"], dtype=f), "gmlp_ln_b": np.ascontiguousarray(inp["gmlp_ln_b"], dtype=f),
        "mem_ln_g": np.ascontiguousarray(inp["mem_ln_g"], dtype=f), "mem_ln_b": np.ascontiguousarray(inp["mem_ln_b"], dtype=f),
        "ln1_g": np.ascontiguousarray(inp["ln1_g"], dtype=f), "ln1_b": np.ascontiguousarray(inp["ln1_b"], dtype=f),
        "ln2_g": np.ascontiguousarray(inp["ln2_g"], dtype=f), "ln2_b": np.ascontiguousarray(inp["ln2_b"], dtype=f),
        "w_router": np.ascontiguousarray(inp["w_router"][0], dtype=f),
        "b_router": np.ascontiguousarray(inp["b_router"], dtype=f),
        "w_gate_up": np.ascontiguousarray(inp["w_gate_up"][0], dtype=f),
        "b_gate_up_T": np.ascontiguousarray(inp["b_gate_up"][0].reshape(32, 16, 128).transpose(2, 0, 1).reshape(128, 512), dtype=f),
        "w_down": np.ascontiguousarray(inp["w_down"][0], dtype=f),
        "b_down": np.ascontiguousarray(inp["b_down"][0], dtype=f),
    }
    return shared


def _core_inputs(inp, shared, b):
    m = dict(shared)
    xb = np.ascontiguousarray(inp["x"][b], dtype=np.float32)
    m["x"] = xb
    m["xT"] = np.ascontiguousarray(xb.T)
    m["mem"] = np.ascontiguousarray(inp["mem"][b], dtype=np.float32)
    return m


def kernel(**inputs):
    inp = {k: np.asarray(v) for k, v in inputs.items()}
    nb = inp["x"].shape[0]
    shared = _host_layout(inp, 0)
    in_maps = [_core_inputs(inp, shared, b) for b in range(nb)]
    nc = build()
    res = run_bass_kernel_spmd(nc, in_maps, core_ids=list(range(nb)))
    out = np.stack([np.asarray(r["out"]) for r in res.results], axis=0)
    return out.astype(np.float32, copy=False)
```

```python
import contextlib
import numpy as np
import concourse.bass as bass
import concourse.mybir as mybir
from concourse.bass_utils import run_bass_kernel_spmd

F32 = mybir.dt.float32
BF16 = mybir.dt.bfloat16
I32 = mybir.dt.int32
U32 = mybir.dt.uint32
AF = mybir.ActivationFunctionType
ALU = mybir.AluOpType

ENGS = ("pe", "act", "dve", "pool", "sp")


class Buf:
    __slots__ = ("name", "writers", "readers")

    def __init__(self, name=""):
        self.name = name
        self.writers = []
        self.readers = []


class Op:
    __slots__ = ("eng", "emit", "deps", "is_dma", "signal", "val", "sem", "idx")

    def __init__(self, eng, emit, is_dma):
        self.eng = eng
        self.emit = emit
        self.deps = []
        self.is_dma = is_dma
        self.signal = False
        self.val = None
        self.sem = None
        self.idx = None


class Sched:
    def __init__(self, nc, n_dma_sems=None):
        self.nc = nc
        self.streams = {e: [] for e in ENGS}
        self.n_dma_sems = n_dma_sems or {"sp": 8, "act": 2, "pool": 4}
        self.last = {}
        self.dmas_since_barrier = []

    @staticmethod
    def _same_inorder(a, b):
        return (not a.is_dma) and (not b.is_dma) and a.eng == b.eng

    def op(self, eng, emit, reads=(), writes=(), dma=False, extra_deps=()):
        o = Op(eng, emit, dma)
        o.idx = len(self.streams[eng])
        deps = list(extra_deps)
        for b in reads:
            deps.extend(b.writers)
        for b in writes:
            deps.extend(b.readers)
            deps.extend(b.writers)
        seen = set()
        for d in deps:
            if d is o or id(d) in seen:
                continue
            seen.add(id(d))
            if eng == "pe" and d.eng == "pe" and not d.is_dma and not dma:
                continue
            o.deps.append(d)
            d.signal = True
        for b in reads:
            b.readers = [r for r in b.readers if not self._same_inorder(r, o)] + [o]
        for b in writes:
            if b.readers:
                b.readers = []
                b.writers = []
            b.writers = [w for w in b.writers if not self._same_inorder(w, o)] + [o]
        self.streams[eng].append(o)
        if dma:
            self.dmas_since_barrier.append(o)
        else:
            self.last[eng] = o
        return o

    def barrier(self):
        deps = list(self.last.values()) + list(self.dmas_since_barrier)
        self.dmas_since_barrier = []
        for e in ENGS:
            self.op(e, lambda eng: eng.nop(), extra_deps=deps)

    def emit_all(self, final_waits=()):
        nc = self.nc
        with contextlib.ExitStack() as st:
            csem = {e: st.enter_context(nc.semaphore("c_" + e)) for e in ("pe", "act", "dve", "pool", "sp")}
            dsems = {e: [st.enter_context(nc.semaphore("d_%s%d" % (e, i))) for i in range(n)]
                     for e, n in self.n_dma_sems.items()}
            for e in ENGS:
                cnt = 0
                nd = self.n_dma_sems.get(e, 0)
                dcnt = [0] * max(nd, 1)
                rr = 0
                for o in self.streams[e]:
                    if o.is_dma:
                        s = rr % nd
                        rr += 1
                        dcnt[s] += 1
                        o.sem = dsems[e][s]
                        o.val = 16 * dcnt[s]
                    elif o.signal:
                        cnt += 1
                        o.sem = csem[e]
                        o.val = cnt
            fw = {}
            for (e, d) in final_waits:
                fw.setdefault(e, []).append(d)
            block = st.enter_context(nc.Block())
            streams = self.streams

            def make(e):
                def body(eng):
                    seen = {}

                    def wait(sem, val):
                        k = id(sem)
                        if seen.get(k, 0) < val:
                            eng.wait_ge(sem, val)
                            seen[k] = val

                    for o in streams[e]:
                        if o.is_dma and o.val > 16:
                            wait(o.sem, o.val - 16)
                        mx = {}
                        for d in o.deps:
                            k = id(d.sem)
                            if k not in mx or mx[k][1] < d.val:
                                mx[k] = (d.sem, d.val)
                        for sem_, val_ in mx.values():
                            wait(sem_, val_)
                        ins = o.emit(eng)
                        if o.is_dma:
                            ins.then_inc(o.sem, 16)
                        elif o.signal:
                            ins.then_inc(o.sem, 1)
                    for d in fw.get(e, ()):
                        wait(d.sem, d.val)
                return body

            for e, deco in (("pe", block.tensor), ("act", block.scalar), ("dve", block.vector),
                            ("pool", block.gpsimd), ("sp", block.sync)):
                if streams[e] or e in fw:
                    deco(make(e))


P = 128
D = 1024
SEQ = 4096
TT = 512
NCH = SEQ // P
NE = 32
CAP = 768
NST = CAP // P
NROWS = NE * CAP
ZROW = NROWS
ALPHA = 2.0 ** 0.25
EPS = 1e-5
SEG_KV = 0
SEG_A = 2048
SEG_B = SEG_A + 1024
SEG_C = SEG_B + 1024
SEG_D = SEG_C + 3072
SEG_E = SEG_D + 1024
SEG_F = SEG_E + 8 * 768
WCOLS = SEG_F + 1024


def build(n_tiles=SEQ // TT, n_exp=NE, debug=False):
    nc = bass.Bass("TRN2", target_bir_lowering=False)

    def din(name, shape, dt=F32):
        return nc.dram_tensor(name, list(shape), dt, kind="ExternalInput").ap()

    xT_d = din("xT", [D, SEQ])
    x_d = din("x", [SEQ, D])
    mem_d = din("mem", [256, D])
    wmix_d = din("wmix", [D, WCOLS])
    bgate_d = din("b_gate_T", [P, 24])
    convw_d = din("conv_w_T", [P, 24])
    wsT_d = din("gmlp_wsT", [P, 8 * P])
    gmlpb_d = din("gmlp_b", [1, D])
    glng_d = din("gmlp_ln_g", [1, D])
    glnb_d = din("gmlp_ln_b", [1, D])
    mlng_d = din("mem_ln_g", [1, D])
    mlnb_d = din("mem_ln_b", [1, D])
    l1g_d = din("ln1_g", [1, D])
    l1b_d = din("ln1_b", [1, D])
    l2g_d = din("ln2_g", [1, D])
    l2b_d = din("ln2_b", [1, D])
    wr_d = din("w_router", [D, NE])
    br_d = din("b_router", [1, NE])
    wgu_d = din("w_gate_up", [NE, D, 2 * D])
    bgu_d = din("b_gate_up_T", [P, NE * 16])
    wd_d = din("w_down", [NE, D, D])
    bd_d = din("b_down", [NE, D])
    out_d = nc.dram_tensor("out", [SEQ, D], F32, kind="ExternalOutput").ap()
    if debug:
        x1_scr = nc.dram_tensor("x1_dbg", [SEQ, D], F32, kind="ExternalOutput").ap()
    else:
        x1_scr = nc.dram_tensor("x1_scr", [SEQ, D], F32).ap()
    wmix_bf = nc.dram_tensor("wmix_bf", [D, WCOLS], BF16).ap()
    xs_scr = nc.dram_tensor("xs_scr", [NROWS, D], BF16).ap()
    eo_scr = nc.dram_tensor("eo_scr", [NROWS + 1, D], F32).ap()

    S = Sched(nc)
    out_dmas = []

    with contextlib.ExitStack() as st:
        NF = 52224
        big = st.enter_context(nc.sbuf_tensor("arena", [P, NF], F32))
        banks = [st.enter_context(nc.psum_tensor("bank%d" % i, [P, 512], F32)) for i in range(8)]
        bank_bufs = [Buf("bank%d" % i) for i in range(8)]
        bank_rr = [0]

        def bank():
            i = bank_rr[0] % 8
            bank_rr[0] += 1
            return banks[i][:, :], bank_bufs[i]

        top = [0]

        def alloc(ncols, dt=F32, shape=None):
            a = big[:, top[0]:top[0] + ncols]
            top[0] += ncols
            assert top[0] <= NF, "SBUF arena overflow %d" % top[0]
            if dt != F32:
                a = a.bitcast(dt)
            if shape is not None:
                names = " ".join("d%d" % i for i in range(len(shape)))
                kw = {"d%d" % i: s for i, s in enumerate(shape)}
                a = a.rearrange("p (%s) -> p %s" % (names, names), **kw)
            return a

        def TT_(eng, out, in0, in1, op, R, W):
            return S.op(eng, lambda e: e.tensor_tensor(out=out, in0=in0, in1=in1, op=op), R, W)

        def TS_(eng, out, in0, s1, s2, op0, op1, R, W):
            if op1 is None:
                return S.op(eng, lambda e: e.tensor_scalar(out=out, in0=in0, scalar1=s1, scalar2=None, op0=op0), R, W)
            return S.op(eng, lambda e: e.tensor_scalar(out=out, in0=in0, scalar1=s1, scalar2=s2, op0=op0, op1=op1), R, W)

        def STT_(eng, out, in0, sc, in1, op0, op1, R, W, accum=None):
            if accum is None:
                return S.op(eng, lambda e: e.scalar_tensor_tensor(out=out, in0=in0, scalar=sc, in1=in1, op0=op0, op1=op1), R, W)
            return S.op(eng, lambda e: e.scalar_tensor_tensor(out=out, in0=in0, scalar=sc, in1=in1, op0=op0, op1=op1,
                                                              accum_out=accum), R, W)

        def ACT_(out, in_, func, R, W, bias=None, scale=1.0, accum=None):
            kw = {}
            if bias is not None:
                kw["bias"] = bias
            if accum is not None:
                kw["accum_out"] = accum
            return S.op("act", lambda e: e.activation(out=out, in_=in_, func=func, scale=scale, **kw), R, W)

        def CP_(eng, out, in_, R, W):
            if eng == "act":
                return S.op("act", lambda e: e.copy(out=out, in_=in_), R, W)
            return S.op(eng, lambda e: e.tensor_copy(out=out, in_=in_), R, W)

        def MS_(eng, ap, val, W):
            return S.op(eng, lambda e: e.memset(ap, val), (), W)

        def MM_(out, lhsT, rhs, start, stop, R, W):
            return S.op("pe", lambda e: e.matmul(out, lhsT=lhsT, rhs=rhs, start=start, stop=stop), R, W)

        def TR_(out, in_, ident, R, W):
            return S.op("pe", lambda e: e.transpose(out=out, in_=in_, identity=ident), R, W)

        def DMA_(eng, out, in_, R, W):
            return S.op(eng, lambda e: e.dma_start(out=out, in_=in_), R, W, dma=True)

        def layer_norm(xin, b_x, g_bc, b_bc, b_par, out, b_out, tmp, b_tmp, aff="pool"):
            st6, mv, rstd = tmp
            for h in range(2):
                S.op("dve", lambda e, h=h: e.bn_stats(out=st6[:, h, :], in_=xin[:, h * 512:(h + 1) * 512]), [b_x], [b_tmp])
            S.op("dve", lambda e: e.bn_aggr(out=mv, in_=st6.rearrange("p a b -> p (a b)")), [b_tmp], [b_tmp])
            ACT_(rstd, mv[:, 1:2], AF.Sqrt, [b_tmp, b_eps], [b_tmp], bias=eps_t)
            S.op("dve", lambda e: e.reciprocal(out=rstd, in_=rstd), [b_tmp], [b_tmp])
            TS_("dve", xin, xin, mv[:, 0:1], rstd, ALU.subtract, ALU.mult, [b_x, b_tmp], [b_x])
            TT_("dve", xin, xin, g_bc, ALU.mult, [b_x, b_par], [b_x])
            TT_(aff, out, xin, b_bc, ALU.add, [b_x, b_par], [b_out])

        identf = alloc(128); b_identf = Buf()
        identb = alloc(64, BF16); b_identb = Buf()
        onesb = alloc(64, BF16); b_onesb = Buf()
        ustrb = alloc(64, BF16); b_ustr = Buf()
        eps_t = alloc(1); b_eps = Buf()
        iota32 = alloc(32); b_iota = Buf()
        wr_sb = alloc(8 * NE, F32, (8, NE)); b_wr = Buf()
        br_bc = alloc(NE); b_br = Buf()
        gate_all = alloc(NCH * 4, F32, (NCH, 4)); b_gate_all = [Buf() for _ in range(NCH)]
        dst_all = alloc(NCH * 4, I32, (NCH, 4)); b_dst_all = [Buf() for _ in range(NCH)]
        tot_bc = alloc(NE); b_tot = Buf()
        bgu_sb = alloc(NE * 16); b_bgu = Buf()
        lnC_g = alloc(D); lnC_b = alloc(D); b_lnC = Buf()
        zrow = alloc(D); b_zrow = Buf()
        lnt = (alloc(12, F32, (2, 6)), alloc(2), alloc(1)); b_lnt = Buf()
        persist_top = top[0]
        WcT = alloc(512, BF16, (8, P)); b_WcT = Buf()
        biasbc = alloc(D, F32, (8, P)); b_biasbc = Buf()
        KT = alloc(1024, BF16, (8, 256)); b_KT = Buf()
        Vt = alloc(1024, BF16, (2, D)); b_Vt = Buf()
        bgate = alloc(24); b_bgate = Buf()
        convw = alloc(24); b_convw = Buf()
        halo = alloc(16, F32, (8, 2)); b_halo = [Buf() for _ in range(8)]
        gln_g = alloc(D); gln_b = alloc(D); b_gln = Buf()
        l1_g = alloc(D); l1_b = alloc(D); b_l1 = Buf()
        mixer_base = top[0]

        MS_("pool", identf, 0.0, [b_identf])
        S.op("pool", lambda e: e.affine_select(out=identf, in_=identf, pattern=[[-1, P]], compare_op=ALU.not_equal,
                                               fill=1.0, base=0, channel_multiplier=1), [b_identf], [b_identf])
        CP_("dve", identb, identf, [b_identf], [b_identb])
        MS_("pool", onesb, 1.0, [b_onesb])
        ustrf = alloc(128); b_ustrf = Buf()
        MS_("pool", ustrf, 1.0, [b_ustrf])
        S.op("pool", lambda e: e.affine_select(out=ustrf, in_=ustrf, pattern=[[1, P]], compare_op=ALU.is_gt,
                                               fill=0.0, base=0, channel_multiplier=-1), [b_ustrf], [b_ustrf])
        CP_("dve", ustrb, ustrf, [b_ustrf], [b_ustr])
        MS_("pool", eps_t, EPS, [b_eps])
        S.op("pool", lambda e: e.iota(iota32, pattern=[[1, NE]], base=0, channel_multiplier=0,
                                      allow_small_or_imprecise_dtypes=True), (), [b_iota])
        MS_("pool", tot_bc, 0.0, [b_tot])
        MS_("pool", halo.rearrange("p a b -> p (a b)"), 0.0, b_halo)
        MS_("pool", zrow, 0.0, [b_zrow])
        b_eo = Buf("eo_scr")
        DMA_("sp", eo_scr[ZROW:ZROW + 1, :], zrow[0:1, :], [b_zrow], [b_eo])
        DMA_("sp", wr_sb, wr_d.rearrange("(k p) n -> p k n", p=P), (), [b_wr])
        DMA_("sp", br_bc, br_d.partition_broadcast(P), (), [b_br])
        DMA_("sp", bgu_sb, bgu_d, (), [b_bgu])
        DMA_("sp", lnC_g, mlng_d.partition_broadcast(P), (), [b_lnC])
        DMA_("sp", lnC_b, mlnb_d.partition_broadcast(P), (), [b_lnC])
        DMA_("sp", biasbc.rearrange("p a b -> p (a b)"), gmlpb_d.partition_broadcast(P), (), [b_biasbc])
        DMA_("sp", bgate, bgate_d, (), [b_bgate])
        DMA_("sp", convw, convw_d, (), [b_convw])
        DMA_("sp", gln_g, glng_d.partition_broadcast(P), (), [b_gln])
        DMA_("sp", gln_b, glnb_d.partition_broadcast(P), (), [b_gln])
        DMA_("sp", l1_g, l1g_d.partition_broadcast(P), (), [b_l1])
        DMA_("sp", l1_b, l1b_d.partition_broadcast(P), (), [b_l1])
        wsTf = alloc(8 * P, F32, (8, P)); b_wsTf = Buf()
        DMA_("sp", wsTf.rearrange("p a b -> p (a b)"), wsT_d, (), [b_wsTf])
        S.op("pool", lambda e: e.affine_select(out=wsTf, in_=wsTf, pattern=[[0, 8], [1, P]], compare_op=ALU.is_ge,
                                               fill=0.0, base=0, channel_multiplier=-1), [b_wsTf], [b_wsTf])
        CP_("dve", WcT, wsTf, [b_wsTf], [b_WcT])
        const_top = top[0]

        NSLOT = 4
        ring = [alloc(2048, BF16, (8, 512)) for _ in range(NSLOT)]
        ring_bufs = [Buf("slab%d" % i) for i in range(NSLOT)]

        slab_list = []
        for c in range(4):
            slab_list.append((SEG_KV + c * 512, 512))
        for _ in range(n_tiles):
            slab_list += [(SEG_A, 512), (SEG_A + 512, 512)]
            slab_list += [(SEG_C + c * 384, 384) for c in range(8)]
            slab_list += [(SEG_B, 512), (SEG_B + 512, 512)]
            slab_list += [(SEG_D, 512), (SEG_D + 512, 512)]
            for dc in range(8):
                slab_list += [(SEG_E + dc * 768, 384), (SEG_E + dc * 768 + 384, 384)]
            slab_list += [(SEG_F, 512), (SEG_F + 512, 512)]
        slab_issued = [0]
        slab_next = [0]

        b_conv = {}

        def issue_slab():
            i = slab_issued[0]
            if i >= len(slab_list):
                return
            c0, w = slab_list[i]
            sl = i % NSLOT
            if c0 not in b_conv:
                DMA_("pool", ring[sl][:, :, 0:w], wmix_d[:, c0:c0 + w].rearrange("(k p) n -> p k n", p=P), (), [ring_bufs[sl]])
                b_conv[c0] = Buf()
                if c0 >= SEG_A and n_tiles > 1:
                    DMA_("sp", wmix_bf[:, c0:c0 + w].rearrange("(k p) n -> p k n", p=P), ring[sl][:, :, 0:w],
                         [ring_bufs[sl]], [b_conv[c0]])
            else:
                DMA_("sp", ring[sl][:, :, 0:w], wmix_bf[:, c0:c0 + w].rearrange("(k p) n -> p k n", p=P), [b_conv[c0]], [ring_bufs[sl]])
            slab_issued[0] += 1

        def get_slab():
            i = slab_next[0]
            slab_next[0] += 1
            while slab_issued[0] < min(i + NSLOT - 1, len(slab_list)):
                issue_slab()
            sl = i % NSLOT
            return ring[sl], ring_bufs[sl]

        memx = [alloc(D) for _ in range(2)]; b_memx = [Buf() for _ in range(2)]
        memnT = alloc(1024, BF16, (8, 256)); b_memnT = Buf()
        for j in range(2):
            DMA_("sp", memx[j], mem_d[j * P:(j + 1) * P, :], (), [b_memx[j]])
            layer_norm(memx[j], b_memx[j], lnC_g, lnC_b, b_lnC, memx[j], b_memx[j], lnt, b_lnt)
            for hh in range(2):
                pb, bb = bank()
                for kk in range(4):
                    k = hh * 4 + kk
                    TR_(pb[:, kk * P:(kk + 1) * P], memx[j][:, k * P:(k + 1) * P], identf, [b_memx[j], b_identf], [bb])
                CP_("act", memnT[:, hh * 4:(hh + 1) * 4, j * P:(j + 1) * P], pb.rearrange("p (a b) -> p a b", a=4), [bb], [b_memnT])
        for c in range(2):
            slab, sb_ = get_slab()
            for dd in range(4):
                dchunk = c * 4 + dd
                pb, bb = bank()
                for k in range(8):
                    MM_(pb[:, 0:256], slab[:, k, dd * P:(dd + 1) * P], memnT[:, k, :], k == 0, k == 7, [sb_, b_memnT], [bb])
                CP_("act", KT[:, dchunk, :], pb[:, 0:256], [bb], [b_KT])
        for c in range(2):
            slab, sb_ = get_slab()
            for mc in range(2):
                pb, bb = bank()
                for k in range(8):
                    MM_(pb, memnT[:, k, mc * P:(mc + 1) * P], slab[:, k, :], k == 0, k == 7, [sb_, b_memnT], [bb])
                CP_("dve", Vt[:, mc, c * 512:(c + 1) * 512], pb, [bb], [b_Vt])

        top[0] = const_top + NSLOT * 2048
        xT_bf = alloc(2048, BF16, (8, TT)); b_xT = Buf()
        vn_mg = alloc(2048, BF16)
        v_n = vn_mg.rearrange("p (a b) -> p a b", a=4)
        merged = vn_mg.rearrange("p (a b) -> p a b", a=8)
        b_vnmg = Buf()
        vg = [alloc(D) for _ in range(2)]; b_vg = [Buf() for _ in range(2)]
        ug = [alloc(TT) for _ in range(2)]; b_ug = [Buf() for _ in range(2)]
        ftmp = [alloc(TT) for _ in range(2)]; b_ftmp = [Buf() for _ in range(2)]
        y_conv = alloc(2048, BF16, (8, TT)); b_yconv = Buf()
        y_gmlp = alloc(2048, BF16, (8, TT)); b_ygmlp = Buf()
        y_xa = alloc(2048, BF16, (8, TT)); b_yxa = Buf()
        cc_sb = alloc(TT); b_cc = Buf()
        u_t = [alloc(TT + 4) for _ in range(2)]; b_u = [Buf() for _ in range(2)]
        acc = alloc(TT); b_acc = Buf()
        qT = [alloc(512, BF16, (2, TT)) for _ in range(2)]; b_qT = [Buf() for _ in range(2)]
        PT = [alloc(512, BF16, (2, TT)) for _ in range(2)]; b_PT = [Buf() for _ in range(2)]
        rden = [alloc(TT) for _ in range(2)]; b_rden = [Buf() for _ in range(2)]
        sig = [alloc(3 * TT, F32, (3, TT)) for _ in range(2)]; b_sig = [Buf() for _ in range(2)]
        mt = alloc(3 * TT, F32, (3, TT)); b_mt = Buf()
        xc = [alloc(D) for _ in range(2)]; b_xc = [Buf() for _ in range(2)]
        r2 = [alloc(D) for _ in range(2)]; b_r2 = [Buf() for _ in range(2)]
        x1bf = [alloc(512, BF16) for _ in range(2)]; b_x1bf = [Buf() for _ in range(2)]
        x1T = alloc(D, F32, (8, P)); b_x1T = Buf()
        lgt = alloc(NE); mx8 = alloc(8); mi8 = alloc(8, U32); ef4 = alloc(4); negmax = alloc(1)
        ex4 = alloc(4); ssum = alloc(1); Mbf = alloc(NE // 2, BF16); pos = alloc(NE); junk = alloc(NE)
        pos4 = alloc(4); dstf = alloc(4); keep = alloc(4)
        b_rt = Buf()
        b_x1scr = [Buf() for _ in range(NCH)]
        b_xs = Buf("xs_scr")
        bc_reg = {}

        S.barrier()

        for ti in range(n_tiles):
            t0 = ti * TT
            if ti == 0:
                DMA_("pool", xT_bf, xT_d[:, t0:t0 + TT].rearrange("(k p) t -> p k t", p=P), (), [b_xT])

            slabA = [get_slab(), get_slab()]
            for j in range(4):
                vgj, bvg = vg[j % 2], b_vg[j % 2]
                for h in range(2):
                    pb, bb = bank()
                    sl, sb_ = slabA[h]
                    for k in range(8):
                        MM_(pb, xT_bf[:, k, j * P:(j + 1) * P], sl[:, k, :], k == 0, k == 7, [b_xT, sb_], [bb])
                    ACT_(vgj[:, h * 512:(h + 1) * 512], pb, AF.Gelu_apprx_tanh, [bb], [bvg])
                layer_norm(vgj, bvg, gln_g, gln_b, b_gln, v_n[:, j, :], b_vnmg, lnt, b_lnt)

            for c in range(8):
                sl, sb_ = get_slab()
                pcs = []
                for r in range(3):
                    pb, bb = bank()
                    for k in range(8):
                        MM_(pb, sl[:, k, r * P:(r + 1) * P], xT_bf[:, k, :], k == 0, k == 7, [sb_, b_xT], [bb])
                    pcs.append((pb, bb))
                (pcb, bcb), (pcc, bcc), (pch, bch) = pcs
                u, bu = u_t[c % 2], b_u[c % 2]
                CP_("act", cc_sb, pcc, [bcc], [b_cc])
                CP_("pool", u[:, 0:2], halo[:, c, :], [b_halo[c]], [bu])
                TT_("dve", u[:, 2:TT + 2], cc_sb, pch, ALU.mult, [b_cc, bch], [bu])
                CP_("pool", halo[:, c, :], u[:, TT:TT + 2], [bu], [b_halo[c]])
                TS_("dve", acc, u[:, 2:TT + 2], convw[:, 16 + c:17 + c], None, ALU.mult, None, [bu, b_convw], [b_acc])
                STT_("dve", acc, u[:, 1:TT + 1], convw[:, 8 + c:9 + c], acc, ALU.mult, ALU.add, [bu, b_convw, b_acc], [b_acc])
                STT_("dve", acc, u[:, 0:TT], convw[:, c:c + 1], acc, ALU.mult, ALU.add, [bu, b_convw, b_acc], [b_acc])
                TT_("dve", y_conv[:, c, :], acc, pcb, ALU.mult, [b_acc, bcb], [b_yconv])

            slabB = [get_slab(), get_slab()]
            for g in range(8):
                sl, sb_ = slabB[g // 4]
                gg = g % 4
                pu, bpu = bank()
                for k in range(8):
                    MM_(pu, sl[:, k, gg * P:(gg + 1) * P], xT_bf[:, k, :], k == 0, k == 7, [sb_, b_xT], [bpu])
                ACT_(ug[g % 2], pu, AF.Gelu_apprx_tanh, [bpu], [b_ug[g % 2]])
                pf, bpf = bank()
                for j in range(4):
                    MM_(pf[:, j * P:(j + 1) * P], v_n[:, j, g * P:(g + 1) * P], WcT[:, g, :], True, True, [b_vnmg, b_WcT], [bpf])
                TT_("dve", ftmp[g % 2].rearrange("p (j t) -> p j t", j=4), pf.rearrange("p (j t) -> p j t", j=4),
                    biasbc[:, g, :].unsqueeze(1).broadcast_to([P, 4, P]), ALU.add, [bpf, b_biasbc], [b_ftmp[g % 2]])
                TT_("pool", y_gmlp[:, g, :], ftmp[g % 2], ug[g % 2], ALU.mult, [b_ftmp[g % 2], b_ug[g % 2]], [b_ygmlp])

            slabD = [get_slab(), get_slab()]
            for h in range(4):
                sl, sb_ = slabD[h // 2]
                q, bq = qT[h % 2], b_qT[h % 2]
                for dc in range(2):
                    cc = (h % 2) * 2 + dc
                    pb, bb = bank()
                    for k in range(8):
                        MM_(pb, sl[:, k, cc * P:(cc + 1) * P], xT_bf[:, k, :], k == 0, k == 7, [sb_, b_xT], [bb])
                    CP_("act", q[:, dc, :], pb, [bb], [bq])
                pt, bpt = PT[h % 2], b_PT[h % 2]
                for mc in range(2):
                    pb, bb = bank()
                    for dc in range(2):
                        MM_(pb, KT[:, h * 2 + dc, mc * P:(mc + 1) * P], q[:, dc, :], dc == 0, dc == 1, [b_KT, bq], [bb])
                    ACT_(pt[:, mc, :], pb, AF.Exp, [bb], [bpt], scale=0.0625)
                pden, bden = bank()
                for mc in range(2):
                    MM_(pden, onesb, pt[:, mc, :], mc == 0, mc == 1, [b_onesb, bpt], [bden])
                rd, brd = rden[h % 2], b_rden[h % 2]
                S.op("dve", lambda e, rd=rd, pden=pden: e.reciprocal(out=rd, in_=pden), [bden], [brd])
                for dc in range(2):
                    po, bpo = bank()
                    for mc in range(2):
                        MM_(po, Vt[:, mc, (h * 2 + dc) * P:(h * 2 + dc + 1) * P], pt[:, mc, :], mc == 0, mc == 1, [b_Vt, bpt], [bpo])
                    TT_("dve", y_xa[:, h * 2 + dc, :], rd, po, ALU.mult, [brd, bpo], [b_yxa])

            ys = [(y_conv, b_yconv), (y_gmlp, b_ygmlp), (y_xa, b_yxa)]
            for dc in range(8):
                slg, sbg = get_slab()
                slp, sbp = get_slab()
                sg, bsg = sig[dc % 2], b_sig[dc % 2]
                for r in range(3):
                    pb, bb = bank()
                    for k in range(8):
                        MM_(pb, slg[:, k, r * P:(r + 1) * P], xT_bf[:, k, :], k == 0, k == 7, [sbg, b_xT], [bb])
                    ACT_(sg[:, r, :], pb, AF.Sigmoid, [bb, b_bgate], [bsg], bias=bgate[:, r * 8 + dc:r * 8 + dc + 1])
                for r in range(3):
                    pb, bb = bank()
                    yr, byr = ys[r]
                    for k in range(8):
                        MM_(pb, slp[:, k, r * P:(r + 1) * P], yr[:, k, :], k == 0, k == 7, [sbp, byr], [bb])
                    TT_("dve", mt[:, r, :], sg[:, r, :], pb, ALU.mult, [bsg, bb], [b_mt])
                TT_("pool", mt[:, 0, :], mt[:, 0, :], mt[:, 1, :], ALU.add, [b_mt], [b_mt])
                TT_("pool", merged[:, dc, :], mt[:, 0, :], mt[:, 2, :], ALU.add, [b_mt], [b_vnmg])

            if ti + 1 < n_tiles:
                DMA_("pool", xT_bf, xT_d[:, t0 + TT:t0 + 2 * TT].rearrange("(k p) t -> p k t", p=P), (), [b_xT])

            slabF = [get_slab(), get_slab()]

            wbanks = {}

            def stage1_mm(j):
                ci = ti * 4 + j
                xcj, bxc = xc[j % 2], b_xc[j % 2]
                DMA_("sp", xcj, x_d[ci * P:(ci + 1) * P, :], (), [bxc])
                wbanks[j] = []
                for h in range(2):
                    pb, bb = bank()
                    sl, sb_ = slabF[h]
                    for k in range(8):
                        MM_(pb, merged[:, k, j * P:(j + 1) * P], sl[:, k, :], k == 0, k == 7, [b_vnmg, sb_], [bb])
                    wbanks[j].append((pb, bb))

            def stage1_ew(j):
                ci = ti * 4 + j
                r_t, b_r = r2[j % 2], b_r2[j % 2]
                xcj, bxc = xc[j % 2], b_xc[j % 2]
                for h in range(2):
                    pb, bb = wbanks[j][h]
                    STT_("dve", r_t[:, h * 512:(h + 1) * 512], xcj[:, h * 512:(h + 1) * 512], ALPHA, pb, ALU.mult, ALU.add,
                         [bxc, bb], [b_r])
                layer_norm(r_t, b_r, l1_g, l1_b, b_l1, r_t, b_r, lnt, b_lnt)
                DMA_("sp", x1_scr[ci * P:(ci + 1) * P, :], r_t, [b_r], [b_x1scr[ci]])
                xb, bxb = x1bf[j % 2], b_x1bf[j % 2]
                CP_("act", xb, r_t, [b_r], [bxb])

            def stage2(j):
                ci = ti * 4 + j
                r_t, b_r = r2[j % 2], b_r2[j % 2]
                xb, bxb = x1bf[j % 2], b_x1bf[j % 2]
                for hh in range(2):
                    pb, bb = bank()
                    for kk in range(4):
                        k = hh * 4 + kk
                        TR_(pb[:, kk * P:(kk + 1) * P], r_t[:, k * P:(k + 1) * P], identf, [b_r, b_identf], [bb])
                    CP_("act", x1T[:, hh * 4:(hh + 1) * 4, :], pb.rearrange("p (a b) -> p a b", a=4), [bb], [b_x1T])
                pl, bpl = bank()
                for k in range(8):
                    MM_(pl[:, 0:NE], x1T[:, k, :], wr_sb[:, k, :], k == 0, k == 7, [b_x1T, b_wr], [bpl])
                TT_("dve", lgt, pl[:, 0:NE], br_bc, ALU.add, [bpl, b_br], [b_rt])
                S.op("dve", lambda e: e.max(out=mx8, in_=lgt), [b_rt], [b_rt])
                S.op("dve", lambda e: e.max_index(out=mi8, in_max=mx8, in_values=lgt), [b_rt], [b_rt])
                TS_("dve", negmax, mx8[:, 0:1], -1.0, None, ALU.mult, None, [b_rt], [b_rt])
                MS_("dve", ssum, 0.0, [b_rt])
                MS_("dve", pos4, 0.0, [b_rt])
                ACT_(ex4, mx8[:, 0:4], AF.Exp, [b_rt], [b_rt], bias=negmax, accum=ssum)
                S.op("dve", lambda e: e.reciprocal(out=ssum, in_=ssum), [b_rt], [b_rt])
                TS_("dve", gate_all[:, ci, :], ex4, ssum, None, ALU.mult, None, [b_rt], [b_gate_all[ci]])
                TS_("dve", Mbf, lgt, mx8[:, 3:4], None, ALU.is_ge, None, [b_rt], [b_rt])
                pp, bpp = bank()
                MM_(pp[:, 0:NE], ustrb, Mbf, True, True, [b_ustr, b_rt], [bpp])
                MM_(pp[:, NE:2 * NE], onesb, Mbf, True, True, [b_onesb, b_rt], [bpp])
                TT_("dve", pos, pp[:, 0:NE], tot_bc, ALU.add, [bpp, b_tot], [b_rt])
                TT_("dve", tot_bc, pp[:, NE:2 * NE], tot_bc, ALU.add, [bpp, b_tot], [b_tot])
                CP_("dve", ef4, mi8[:, 0:4], [b_rt], [b_rt])
                for k in range(4):
                    STT_("dve", junk, iota32, ef4[:, k:k + 1], pos, ALU.is_equal, ALU.mult, [b_iota, b_rt], [b_rt],
                         accum=pos4[:, k:k + 1])
                STT_("dve", dstf, ef4, float(CAP), pos4, ALU.mult, ALU.add, [b_rt], [b_rt])
                TS_("dve", keep, pos4, float(CAP), None, ALU.is_lt, None, [b_rt], [b_rt])
                TS_("dve", dstf, dstf, -float(ZROW), None, ALU.add, None, [b_rt], [b_rt])
                TT_("dve", dstf, dstf, keep, ALU.mult, [b_rt], [b_rt])
                TS_("dve", dstf, dstf, float(ZROW), None, ALU.add, None, [b_rt], [b_rt])
                CP_("dve", dst_all[:, ci, :], dstf, [b_rt], [b_dst_all[ci]])
                for k in range(4):
                    def scat(e, ci=ci, k=k, xb=xb):
                        if "r" not in bc_reg:
                            bc_reg["r"] = e.to_reg(NROWS - 1)
                        return e.indirect_dma_start(
                            out=xs_scr, out_offset=bass.IndirectOffsetOnAxis(ap=dst_all[:, ci, k:k + 1], axis=0),
                            in_=xb, in_offset=None, bounds_check=bc_reg["r"], oob_is_err=False)
                    S.op("pool", scat, [bxb, b_dst_all[ci]], [b_xs], dma=True)

            stage1_mm(0)
            stage1_ew(0)
            for j in range(4):
                if j + 1 < 4:
                    stage1_mm(j + 1)
                stage2(j)
                if j + 1 < 4:
                    stage1_ew(j + 1)

        S.barrier()

        top[0] = persist_top
        DMA_("sp", lnC_g, l2g_d.partition_broadcast(P), (), [b_lnC])
        DMA_("sp", lnC_b, l2b_d.partition_broadcast(P), (), [b_lnC])
        wgu = [alloc(8192, BF16, (8, 2 * D)) for _ in range(2)]
        b_wgu = [[Buf() for _ in range(8)] for _ in range(2)]
        wdn = [alloc(4096, BF16, (8, D)) for _ in range(2)]
        b_wdn = [[Buf() for _ in range(8)] for _ in range(2)]
        xs_sm = [alloc(512, BF16) for _ in range(NST)]; b_xssm = [Buf() for _ in range(NST)]
        xsT2 = [alloc(4 * CAP, BF16, (8, CAP)) for _ in range(2)]; b_xsT2 = [[Buf() for _ in range(NST)] for _ in range(2)]
        actT = alloc(4 * CAP, BF16, (8, CAP)); b_actT = Buf()
        g_sb = [alloc(512) for _ in range(2)]; b_g = [Buf() for _ in range(2)]
        s_sb = [alloc(512) for _ in range(2)]; b_s = [Buf() for _ in range(2)]
        u_sb = [alloc(512) for _ in range(2)]; b_us = [Buf() for _ in range(2)]
        eo_sb = [alloc(D) for _ in range(2)]; b_eosb = [Buf() for _ in range(2)]
        bdbc = [alloc(D) for _ in range(2)]; b_bdbc = [Buf() for _ in range(2)]
        bgu1 = alloc(NE * 16); b_bgu1 = Buf()
        TS_("dve", bgu1, bgu_sb, 1.0, None, ALU.add, None, [b_bgu], [b_bgu1])
        bankbf2 = [banks[6][:, :].bitcast(BF16), banks[7][:, :].bitcast(BF16)]

        def load_expert(e):
            bi = e % 2
            for k in range(8):
                DMA_("pool", wgu[bi][:, k, :], wgu_d[e, k * P:(k + 1) * P, :], (), [b_wgu[bi][k]])
            for k in range(8):
                DMA_("pool", wdn[bi][:, k, :], wd_d[e, k * P:(k + 1) * P, :], (), [b_wdn[bi][k]])
            DMA_("sp", bdbc[bi], bd_d[e:e + 1, :].partition_broadcast(P), (), [b_bdbc[bi]])

        def prefetch_xs(e):
            for s in range(NST):
                DMA_("sp", xs_sm[s], xs_scr[e * CAP + s * P:e * CAP + (s + 1) * P, :], [b_xs], [b_xssm[s]])

        def transposes(e):
            xsT, b_xsT = xsT2[e % 2], b_xsT2[e % 2]
            for s in range(NST):
                xm, bxm = xs_sm[s], b_xssm[s]
                bankbf, bbf = bankbf2[s % 2], bank_bufs[6 + s % 2]
                for k in range(8):
                    TR_(bankbf[:, k * P:(k + 1) * P], xm[:, k * P:(k + 1) * P], identb, [bxm, b_identb], [bbf])
                CP_("act", xsT[:, :, s * P:(s + 1) * P], bankbf.rearrange("p (a b) -> p a b", a=8), [bbf], [b_xsT[s]])

        splits = [(0, 512), (512, CAP)]
        split_tiles = [[s for s in range(NST) if n0 <= s * P < n1] for (n0, n1) in splits]
        if n_exp > 0:
            load_expert(0)
            prefetch_xs(0)
            transposes(0)
        cnt = 0
        for e in range(n_exp):
            bi = e % 2
            if e + 1 < n_exp:
                load_expert(e + 1)
                prefetch_xs(e + 1)
            xsT, b_xsT = xsT2[bi], b_xsT2[bi]
            for (n0, n1), tiles in zip(splits, split_tiles):
                w = n1 - n0
                rb = [b_xsT[s] for s in tiles]
                for f in range(8):
                    pgs = []
                    for m in (f, 8 + f):
                        i = bank_rr[0] % 6
                        bank_rr[0] += 1
                        pb, bb = banks[i][:, :], bank_bufs[i]
                        for k in range(8):
                            MM_(pb[:, 0:w], wgu[bi][:, k, m * P:(m + 1) * P], xsT[:, k, n0:n1], k == 0, k == 7,
                                [b_wgu[bi][k]] + rb, [bb])
                        pgs.append((pb, bb))
                    (pg, bpg), (pu, bpu) = pgs
                    gi = cnt % 2
                    cnt += 1
                    gs, us, ss = g_sb[gi][:, 0:w], u_sb[gi][:, 0:w], s_sb[gi][:, 0:w]
                    TS_("dve", gs, pg[:, 0:w], bgu_sb[:, e * 16 + f:e * 16 + f + 1], 7.0, ALU.add, ALU.min, [bpg, b_bgu], [b_g[gi]])
                    ACT_(ss, gs, AF.Sigmoid, [b_g[gi]], [b_s[gi]], scale=1.702)
                    TS_("dve", us, pu[:, 0:w], bgu1[:, e * 16 + 8 + f:e * 16 + 9 + f], 8.0, ALU.add, ALU.min, [bpu, b_bgu1], [b_us[gi]])
                    TT_("dve", gs, gs, ss, ALU.mult, [b_g[gi], b_s[gi]], [b_g[gi]])
                    STT_("dve", actT[:, f, n0:n1], us, -6.0, gs, ALU.max, ALU.mult, [b_us[gi], b_g[gi]], [b_actT])
            if e + 1 < n_exp:
                transposes(e + 1)
            for s in range(NST):
                eo, beo = eo_sb[s % 2], b_eosb[s % 2]
                for h in range(2):
                    i = bank_rr[0] % 6
                    bank_rr[0] += 1
                    pb, bb = banks[i][:, :], bank_bufs[i]
                    for f in range(8):
                        MM_(pb, actT[:, f, s * P:(s + 1) * P], wdn[bi][:, f, h * 512:(h + 1) * 512], f == 0, f == 7,
                            [b_actT, b_wdn[bi][f]], [bb])
                    TT_("dve", eo[:, h * 512:(h + 1) * 512], pb, bdbc[bi][:, h * 512:(h + 1) * 512], ALU.add, [bb, b_bdbc[bi]], [beo])
                DMA_("sp", eo_scr[e * CAP + s * P:e * CAP + (s + 1) * P, :], eo, [beo], [b_eo])

        S.barrier()

        top[0] = persist_top
        NB4 = 3
        eo4 = [alloc(4 * D, F32, (4, D)) for _ in range(NB4)]; b_eo4 = [Buf() for _ in range(NB4)]
        x1c = [alloc(D) for _ in range(NB4)]; b_x1c = [Buf() for _ in range(NB4)]
        accd = [alloc(D) for _ in range(2)]; b_accd = [Buf() for _ in range(2)]
        lnt2 = (alloc(12, F32, (2, 6)), alloc(2), alloc(1)); b_lnt2 = Buf()
        n_ch = n_tiles * 4

        def fetch(ci):
            e4, be4 = eo4[ci % NB4], b_eo4[ci % NB4]
            for k in range(4):
                S.op("pool", lambda e, ci=ci, k=k, e4=e4: e.indirect_dma_start(
                    out=e4[:, k, :], out_offset=None, in_=eo_scr,
                    in_offset=bass.IndirectOffsetOnAxis(ap=dst_all[:, ci, k:k + 1], axis=0)),
                    [b_eo, b_dst_all[ci]], [be4], dma=True)
            DMA_("sp", x1c[ci % NB4], x1_scr[ci * P:(ci + 1) * P, :], [b_x1scr[ci]], [b_x1c[ci % NB4]])

        for ci in range(min(NB4 - 1, n_ch)):
            fetch(ci)
        for ci in range(n_ch):
            if ci + NB4 - 1 < n_ch:
                fetch(ci + NB4 - 1)
            e4, be4 = eo4[ci % NB4], b_eo4[ci % NB4]
            xx, bxx = x1c[ci % NB4], b_x1c[ci % NB4]
            a, ba = accd[ci % 2], b_accd[ci % 2]
            TS_("dve", a, e4[:, 0, :], gate_all[:, ci, 0:1], None, ALU.mult, None, [be4, b_gate_all[ci]], [ba])
            for k in range(1, 4):
                STT_("dve", a, e4[:, k, :], gate_all[:, ci, k:k + 1], a, ALU.mult, ALU.add, [be4, b_gate_all[ci], ba], [ba])
            STT_("dve", a, xx, ALPHA, a, ALU.mult, ALU.add, [bxx, ba], [ba])
            layer_norm(a, ba, lnC_g, lnC_b, b_lnC, a, ba, lnt2, b_lnt2, aff="dve")
            out_dmas.append(DMA_("sp", out_d[ci * P:(ci + 1) * P, :], a, [ba], ()))

        S.emit_all(final_waits=[("sp", d) for d in out_dmas])
    return nc


def _host_layout(inp, b):
    f = np.float32
    w_in = inp["w_in"][0]
    cb, cc, ch, gu, gv, q, gates = np.split(w_in, np.cumsum([1024, 1024, 1024, 1024, 1024, 1024])[:], axis=1)
    segs = [inp["w_kv"][0], gv, gu]
    for c in range(8):
        sl = slice(c * 128, (c + 1) * 128)
        segs += [cb[:, sl], cc[:, sl], ch[:, sl]]
    segs.append(q)
    wcp, wgp, wxp = inp["w_conv_proj"][0], inp["w_gmlp_proj"][0], inp["w_xa_proj"][0]
    for dc in range(8):
        sl = slice(dc * 128, (dc + 1) * 128)
        segs += [gates[:, 0:1024][:, sl], gates[:, 1024:2048][:, sl], gates[:, 2048:3072][:, sl], wcp[:, sl], wgp[:, sl], wxp[:, sl]]
    segs.append(inp["w_out"][0])
    wmix = np.ascontiguousarray(np.concatenate(segs, axis=1), dtype=f)
    assert wmix.shape == (D, WCOLS)
    shared = {
        "wmix": wmix,
        "b_gate_T": np.ascontiguousarray(inp["b_gate"][0].reshape(24, 128).T, dtype=f),
        "conv_w_T": np.ascontiguousarray(inp["conv_w"][0].reshape(3, 8, 128).transpose(2, 0, 1).reshape(128, 24), dtype=f),
        "gmlp_wsT": np.ascontiguousarray(inp["gmlp_ws"][0].transpose(2, 0, 1).reshape(128, 8 * 128), dtype=f),
        "gmlp_b": np.ascontiguousarray(inp["gmlp_b"][0].reshape(1, D), dtype=f),
        "gmlp_ln_g": np.ascontiguousarray(inp["gmlp_ln_g"], dtype=f), "gmlp_ln_b": np.ascontiguousarray(inp["gmlp_ln_b"], dtype=f),
        "mem_ln_g": np.ascontiguousarray(inp["mem_ln_g"], dtype=f), "mem_ln_b": np.ascontiguousarray(inp["mem_ln_b"], dtype=f),
        "ln1_g": np.ascontiguousarray(inp["ln1_g"], dtype=f), "ln1_b": np.ascontiguousarray(inp["ln1_b"], dtype=f),
        "ln2_g": np.ascontiguousarray(inp["ln2_g"], dtype=f), "ln2_b": np.ascontiguousarray(inp["ln2_b"], dtype=f),
        "w_router": np.ascontiguousarray(inp["w_router"][0], dtype=f),
        "b_router": np.ascontiguousarray(inp["b_router"], dtype=f),
        "w_gate_up": np.ascontiguousarray(inp["w_gate_up"][0], dtype=f),
        "b_gate_up_T": np.ascontiguousarray(inp["b_gate_up"][0].reshape(32, 16, 128).transpose(2, 0, 1).reshape(128, 512), dtype=f),
        "w_down": np.ascontiguousarray(inp["w_down"][0], dtype=f),
        "b_down": np.ascontiguousarray(inp["b_down"][0], dtype=f),
    }
    return shared


def _core_inputs(inp, shared, b):
    m = dict(shared)
    xb = np.ascontiguousarray(inp["x"][b], dtype=np.float32)
    m["x"] = xb
    m["xT"] = np.ascontiguousarray(xb.T)
    m["mem"] = np.ascontiguousarray(inp["mem"][b], dtype=np.float32)
    return m


def kernel(**inputs):
    inp = {k: np.asarray(v) for k, v in inputs.items()}
    nb = inp["x"].shape[0]
    shared = _host_layout(inp, 0)
    in_maps = [_core_inputs(inp, shared, b) for b in range(nb)]
    nc = build()
    res = run_bass_kernel_spmd(nc, in_maps, core_ids=list(range(nb)))
    out = np.stack([np.asarray(r["out"]) for r in res.results], axis=0)
    return out.astype(np.float32, copy=False)
```

```python
import contextlib
import numpy as np
import concourse.bass as bass
import concourse.mybir as mybir
from concourse.bass_utils import run_bass_kernel_spmd

F32 = mybir.dt.float32
BF16 = mybir.dt.bfloat16
I32 = mybir.dt.int32
U32 = mybir.dt.uint32
AF = mybir.ActivationFunctionType
ALU = mybir.AluOpType

ENGS = ("pe", "act", "dve", "pool", "sp")


class Buf:
    __slots__ = ("name", "writers", "readers")

    def __init__(self, name=""):
        self.name = name
        self.writers = []
        self.readers = []


class Op:
    __slots__ = ("eng", "emit", "deps", "is_dma", "signal", "val", "sem", "idx")

    def __init__(self, eng, emit, is_dma):
        self.eng = eng
        self.emit = emit
        self.deps = []
        self.is_dma = is_dma
        self.signal = False
        self.val = None
        self.sem = None
        self.idx = None


class Sched:
    def __init__(self, nc, n_dma_sems=None):
        self.nc = nc
        self.streams = {e: [] for e in ENGS}
        self.n_dma_sems = n_dma_sems or {"sp": 8, "act": 2, "pool": 6}
        self.last = {}
        self.dmas_since_barrier = []

    @staticmethod
    def _same_inorder(a, b):
        return (not a.is_dma) and (not b.is_dma) and a.eng == b.eng

    def op(self, eng, emit, reads=(), writes=(), dma=False, extra_deps=()):
        o = Op(eng, emit, dma)
        o.idx = len(self.streams[eng])
        deps = list(extra_deps)
        for b in reads:
            deps.extend(b.writers)
        for b in writes:
            deps.extend(b.readers)
            deps.extend(b.writers)
        seen = set()
        for d in deps:
            if d is o or id(d) in seen:
                continue
            seen.add(id(d))
            if eng == "pe" and d.eng == "pe" and not d.is_dma and not dma:
                continue
            o.deps.append(d)
            d.signal = True
        for b in reads:
            b.readers = [r for r in b.readers if not self._same_inorder(r, o)] + [o]
        for b in writes:
            if b.readers:
                b.readers = []
                b.writers = []
            b.writers = [w for w in b.writers if not self._same_inorder(w, o)] + [o]
        self.streams[eng].append(o)
        if dma:
            self.dmas_since_barrier.append(o)
        else:
            self.last[eng] = o
        return o

    def barrier(self):
        deps = list(self.last.values()) + list(self.dmas_since_barrier)
        self.dmas_since_barrier = []
        for e in ENGS:
            self.op(e, lambda eng: eng.nop(), extra_deps=deps)

    def emit_all(self, final_waits=()):
        nc = self.nc
        with contextlib.ExitStack() as st:
            csem = {e: st.enter_context(nc.semaphore("c_" + e)) for e in ("pe", "act", "dve", "pool", "sp")}
            dsems = {e: [st.enter_context(nc.semaphore("d_%s%d" % (e, i))) for i in range(n)]
                     for e, n in self.n_dma_sems.items()}
            for e in ENGS:
                cnt = 0
                nd = self.n_dma_sems.get(e, 0)
                dcnt = [0] * max(nd, 1)
                rr = 0
                for o in self.streams[e]:
                    if o.is_dma:
                        s = rr % nd
                        rr += 1
                        dcnt[s] += 1
                        o.sem = dsems[e][s]
                        o.val = 16 * dcnt[s]
                    elif o.signal:
                        cnt += 1
                        o.sem = csem[e]
                        o.val = cnt
            fw = {}
            for (e, d) in final_waits:
                fw.setdefault(e, []).append(d)
            block = st.enter_context(nc.Block())
            streams = self.streams

            def make(e):
                def body(eng):
                    seen = {}

                    def wait(sem, val):
                        k = id(sem)
                        if seen.get(k, 0) < val:
                            eng.wait_ge(sem, val)
                            seen[k] = val

                    for o in streams[e]:
                        if o.is_dma and o.val > 16:
                            wait(o.sem, o.val - 16)
                        mx = {}
                        for d in o.deps:
                            k = id(d.sem)
                            if k not in mx or mx[k][1] < d.val:
                                mx[k] = (d.sem, d.val)
                        for sem_, val_ in mx.values():
                            wait(sem_, val_)
                        ins = o.emit(eng)
                        if o.is_dma:
                            ins.then_inc(o.sem, 16)
                        elif o.signal:
                            ins.then_inc(o.sem, 1)
                    for d in fw.get(e, ()):
                        wait(d.sem, d.val)
                return body

            for e, deco in (("pe", block.tensor), ("act", block.scalar), ("dve", block.vector),
                            ("pool", block.gpsimd), ("sp", block.sync)):
                if streams[e] or e in fw:
                    deco(make(e))


P = 128
D = 1024
SEQ = 4096
TT = 512
NCH = SEQ // P
NE = 32
CAP = 768
NST = CAP // P
NROWS = NE * CAP
ZROW = NROWS
ALPHA = 2.0 ** 0.25
EPS = 1e-5
SEG_KV = 0
SEG_A = 2048
SEG_B = SEG_A + 1024
SEG_C = SEG_B + 1024
SEG_D = SEG_C + 3072
SEG_E = SEG_D + 1024
SEG_F = SEG_E + 8 * 768
WCOLS = SEG_F + 1024


def build(n_tiles=SEQ // TT, n_exp=NE, debug=False):
    nc = bass.Bass("TRN2", target_bir_lowering=False)

    def din(name, shape, dt=F32):
        return nc.dram_tensor(name, list(shape), dt, kind="ExternalInput").ap()

    xT_d = din("xT", [D, SEQ])
    x_d = din("x", [SEQ, D])
    mem_d = din("mem", [256, D])
    wmix_d = din("wmix", [D, WCOLS])
    bgate_d = din("b_gate_T", [P, 24])
    convw_d = din("conv_w_T", [P, 24])
    wsT_d = din("gmlp_wsT", [P, 8 * P])
    gmlpb_d = din("gmlp_b", [1, D])
    glng_d = din("gmlp_ln_g", [1, D])
    glnb_d = din("gmlp_ln_b", [1, D])
    mlng_d = din("mem_ln_g", [1, D])
    mlnb_d = din("mem_ln_b", [1, D])
    l1g_d = din("ln1_g", [1, D])
    l1b_d = din("ln1_b", [1, D])
    l2g_d = din("ln2_g", [1, D])
    l2b_d = din("ln2_b", [1, D])
    wr_d = din("w_router", [D, NE])
    br_d = din("b_router", [1, NE])
    wgu_d = din("w_gate_up", [NE, D, 2 * D])
    bgu_d = din("b_gate_up_T", [P, NE * 16])
    wd_d = din("w_down", [NE, D, D])
    bd_d = din("b_down", [NE, D])
    out_d = nc.dram_tensor("out", [SEQ, D], F32, kind="ExternalOutput").ap()
    if debug:
        x1_scr = nc.dram_tensor("x1_dbg", [SEQ, D], F32, kind="ExternalOutput").ap()
    else:
        x1_scr = nc.dram_tensor("x1_scr", [SEQ, D], F32).ap()
    wmix_bf = nc.dram_tensor("wmix_bf", [D, WCOLS], BF16).ap()
    xs_scr = nc.dram_tensor("xs_scr", [NROWS, D], BF16).ap()
    eo_scr = nc.dram_tensor("eo_scr", [NROWS + 1, D], F32).ap()

    S = Sched(nc)
    out_dmas = []

    with contextlib.ExitStack() as st:
        NF = 52224
        big = st.enter_context(nc.sbuf_tensor("arena", [P, NF], F32))
        banks = [st.enter_context(nc.psum_tensor("bank%d" % i, [P, 512], F32)) for i in range(8)]
        bank_bufs = [Buf("bank%d" % i) for i in range(8)]
        bank_rr = [0]

        def bank():
            i = bank_rr[0] % 8
            bank_rr[0] += 1
            return banks[i][:, :], bank_bufs[i]

        top = [0]

        def alloc(ncols, dt=F32, shape=None):
            a = big[:, top[0]:top[0] + ncols]
            top[0] += ncols
            assert top[0] <= NF, "SBUF arena overflow %d" % top[0]
            if dt != F32:
                a = a.bitcast(dt)
            if shape is not None:
                names = " ".join("d%d" % i for i in range(len(shape)))
                kw = {"d%d" % i: s for i, s in enumerate(shape)}
                a = a.rearrange("p (%s) -> p %s" % (names, names), **kw)
            return a

        def TT_(eng, out, in0, in1, op, R, W):
            return S.op(eng, lambda e: e.tensor_tensor(out=out, in0=in0, in1=in1, op=op), R, W)

        def TS_(eng, out, in0, s1, s2, op0, op1, R, W):
            if op1 is None:
                return S.op(eng, lambda e: e.tensor_scalar(out=out, in0=in0, scalar1=s1, scalar2=None, op0=op0), R, W)
            return S.op(eng, lambda e: e.tensor_scalar(out=out, in0=in0, scalar1=s1, scalar2=s2, op0=op0, op1=op1), R, W)

        def STT_(eng, out, in0, sc, in1, op0, op1, R, W, accum=None):
            if accum is None:
                return S.op(eng, lambda e: e.scalar_tensor_tensor(out=out, in0=in0, scalar=sc, in1=in1, op0=op0, op1=op1), R, W)
            return S.op(eng, lambda e: e.scalar_tensor_tensor(out=out, in0=in0, scalar=sc, in1=in1, op0=op0, op1=op1,
                                                              accum_out=accum), R, W)

        def ACT_(out, in_, func, R, W, bias=None, scale=1.0, accum=None):
            kw = {}
            if bias is not None:
                kw["bias"] = bias
            if accum is not None:
                kw["accum_out"] = accum
            return S.op("act", lambda e: e.activation(out=out, in_=in_, func=func, scale=scale, **kw), R, W)

        def CP_(eng, out, in_, R, W):
            if eng == "act":
                return S.op("act", lambda e: e.copy(out=out, in_=in_), R, W)
            return S.op(eng, lambda e: e.tensor_copy(out=out, in_=in_), R, W)

        def MS_(eng, ap, val, W):
            return S.op(eng, lambda e: e.memset(ap, val), (), W)

        def MM_(out, lhsT, rhs, start, stop, R, W):
            return S.op("pe", lambda e: e.matmul(out, lhsT=lhsT, rhs=rhs, start=start, stop=stop), R, W)

        def TR_(out, in_, ident, R, W):
            return S.op("pe", lambda e: e.transpose(out=out, in_=in_, identity=ident), R, W)

        def DMA_(eng, out, in_, R, W):
            return S.op(eng, lambda e: e.dma_start(out=out, in_=in_), R, W, dma=True)

        def layer_norm(xin, b_x, g_bc, b_bc, b_par, out, b_out, tmp, b_tmp, aff="dve"):
            st6, mv, rstd = tmp
            for h in range(2):
                S.op("dve", lambda e, h=h: e.bn_stats(out=st6[:, h, :], in_=xin[:, h * 512:(h + 1) * 512]), [b_x], [b_tmp])
            S.op("dve", lambda e: e.bn_aggr(out=mv, in_=st6.rearrange("p a b -> p (a b)")), [b_tmp], [b_tmp])
            ACT_(rstd, mv[:, 1:2], AF.Sqrt, [b_tmp, b_eps], [b_tmp], bias=eps_t)
            S.op("dve", lambda e: e.reciprocal(out=rstd, in_=rstd), [b_tmp], [b_tmp])
            TS_("dve", xin, xin, mv[:, 0:1], rstd, ALU.subtract, ALU.mult, [b_x, b_tmp], [b_x])
            TT_("dve", xin, xin, g_bc, ALU.mult, [b_x, b_par], [b_x])
            TT_(aff, out, xin, b_bc, ALU.add, [b_x, b_par], [b_out])

        identf = alloc(128); b_identf = Buf()
        identb = alloc(64, BF16); b_identb = Buf()
        onesb = alloc(64, BF16); b_onesb = Buf()
        ustrb = alloc(64, BF16); b_ustr = Buf()
        eps_t = alloc(1); b_eps = Buf()
        iota32 = alloc(32); b_iota = Buf()
        wr_sb = alloc(8 * NE, F32, (8, NE)); b_wr = Buf()
        br_bc = alloc(NE); b_br = Buf()
        gate_all = alloc(NCH * 4, F32, (NCH, 4)); b_gate_all = [Buf() for _ in range(NCH)]
        dst_all = alloc(NCH * 4, I32, (NCH, 4)); b_dst_all = [Buf() for _ in range(NCH)]
        tot_bc = alloc(NE); b_tot = Buf()
        bgu_sb = alloc(NE * 16); b_bgu = Buf()
        lnC_g = alloc(D); lnC_b = alloc(D); b_lnC = Buf()
        zrow = alloc(D); b_zrow = Buf()
        lnt = (alloc(12, F32, (2, 6)), alloc(2), alloc(1)); b_lnt = Buf()
        persist_top = top[0]
        WcT = alloc(512, BF16, (8, P)); b_WcT = Buf()
        biasbc = alloc(D, F32, (8, P)); b_biasbc = Buf()
        KT = alloc(1024, BF16, (8, 256)); b_KT = Buf()
        Vt = alloc(1024, BF16, (2, D)); b_Vt = Buf()
        bgate = alloc(24); b_bgate = Buf()
        convw = alloc(24); b_convw = Buf()
        halo = alloc(16, F32, (8, 2)); b_halo = [Buf() for _ in range(8)]
        gln_g = alloc(D); gln_b = alloc(D); b_gln = Buf()
        l1_g = alloc(D); l1_b = alloc(D); b_l1 = Buf()
        mixer_base = top[0]

        MS_("pool", identf, 0.0, [b_identf])
        S.op("pool", lambda e: e.affine_select(out=identf, in_=identf, pattern=[[-1, P]], compare_op=ALU.not_equal,
                                               fill=1.0, base=0, channel_multiplier=1), [b_identf], [b_identf])
        CP_("dve", identb, identf, [b_identf], [b_identb])
        MS_("pool", onesb, 1.0, [b_onesb])
        ustrf = alloc(128); b_ustrf = Buf()
        MS_("pool", ustrf, 1.0, [b_ustrf])
        S.op("pool", lambda e: e.affine_select(out=ustrf, in_=ustrf, pattern=[[1, P]], compare_op=ALU.is_gt,
                                               fill=0.0, base=0, channel_multiplier=-1), [b_ustrf], [b_ustrf])
        CP_("dve", ustrb, ustrf, [b_ustrf], [b_ustr])
        MS_("pool", eps_t, EPS, [b_eps])
        S.op("pool", lambda e: e.iota(iota32, pattern=[[1, NE]], base=0, channel_multiplier=0,
                                      allow_small_or_imprecise_dtypes=True), (), [b_iota])
        MS_("pool", tot_bc, 0.0, [b_tot])
        MS_("pool", halo.rearrange("p a b -> p (a b)"), 0.0, b_halo)
        MS_("pool", zrow, 0.0, [b_zrow])
        b_eo = Buf("eo_scr")
        DMA_("sp", eo_scr[ZROW:ZROW + 1, :], zrow[0:1, :], [b_zrow], [b_eo])
        DMA_("sp", wr_sb, wr_d.rearrange("(k p) n -> p k n", p=P), (), [b_wr])
        DMA_("sp", br_bc, br_d.partition_broadcast(P), (), [b_br])
        DMA_("sp", bgu_sb, bgu_d, (), [b_bgu])
        DMA_("sp", lnC_g, mlng_d.partition_broadcast(P), (), [b_lnC])
        DMA_("sp", lnC_b, mlnb_d.partition_broadcast(P), (), [b_lnC])
        DMA_("sp", biasbc.rearrange("p a b -> p (a b)"), gmlpb_d.partition_broadcast(P), (), [b_biasbc])
        DMA_("sp", bgate, bgate_d, (), [b_bgate])
        DMA_("sp", convw, convw_d, (), [b_convw])
        DMA_("sp", gln_g, glng_d.partition_broadcast(P), (), [b_gln])
        DMA_("sp", gln_b, glnb_d.partition_broadcast(P), (), [b_gln])
        DMA_("sp", l1_g, l1g_d.partition_broadcast(P), (), [b_l1])
        DMA_("sp", l1_b, l1b_d.partition_broadcast(P), (), [b_l1])
        wsTf = alloc(8 * P, F32, (8, P)); b_wsTf = Buf()
        DMA_("sp", wsTf.rearrange("p a b -> p (a b)"), wsT_d, (), [b_wsTf])
        S.op("pool", lambda e: e.affine_select(out=wsTf, in_=wsTf, pattern=[[0, 8], [1, P]], compare_op=ALU.is_ge,
                                               fill=0.0, base=0, channel_multiplier=-1), [b_wsTf], [b_wsTf])
        CP_("dve", WcT, wsTf, [b_wsTf], [b_WcT])
        const_top = top[0]

        NSLOT = 4
        ring = [alloc(2048, BF16, (8, 512)) for _ in range(NSLOT)]
        ring_bufs = [Buf("slab%d" % i) for i in range(NSLOT)]

        slab_list = []
        for c in range(4):
            slab_list.append((SEG_KV + c * 512, 512))
        for _ in range(n_tiles):
            slab_list += [(SEG_A, 512), (SEG_A + 512, 512)]
            slab_list += [(SEG_C + c * 384, 384) for c in range(8)]
            slab_list += [(SEG_B, 512), (SEG_B + 512, 512)]
            slab_list += [(SEG_D, 512), (SEG_D + 512, 512)]
            for dc in range(8):
                slab_list += [(SEG_E + dc * 768, 384), (SEG_E + dc * 768 + 384, 384)]
            slab_list += [(SEG_F, 512), (SEG_F + 512, 512)]
        slab_issued = [0]
        slab_next = [0]

        b_conv = {}

        def issue_slab():
            i = slab_issued[0]
            if i >= len(slab_list):
                return
            c0, w = slab_list[i]
            sl = i % NSLOT
            if c0 not in b_conv:
                DMA_("pool", ring[sl][:, :, 0:w], wmix_d[:, c0:c0 + w].rearrange("(k p) n -> p k n", p=P), (), [ring_bufs[sl]])
                b_conv[c0] = Buf()
                if c0 >= SEG_A and n_tiles > 1:
                    DMA_("sp", wmix_bf[:, c0:c0 + w].rearrange("(k p) n -> p k n", p=P), ring[sl][:, :, 0:w],
                         [ring_bufs[sl]], [b_conv[c0]])
            else:
                DMA_("sp", ring[sl][:, :, 0:w], wmix_bf[:, c0:c0 + w].rearrange("(k p) n -> p k n", p=P), [b_conv[c0]], [ring_bufs[sl]])
            slab_issued[0] += 1

        def get_slab():
            i = slab_next[0]
            slab_next[0] += 1
            while slab_issued[0] < min(i + NSLOT - 1, len(slab_list)):
                issue_slab()
            sl = i % NSLOT
            return ring[sl], ring_bufs[sl]

        memx = [alloc(D) for _ in range(2)]; b_memx = [Buf() for _ in range(2)]
        memnT = alloc(1024, BF16, (8, 256)); b_memnT = Buf()
        for j in range(2):
            DMA_("sp", memx[j], mem_d[j * P:(j + 1) * P, :], (), [b_memx[j]])
            layer_norm(memx[j], b_memx[j], lnC_g, lnC_b, b_lnC, memx[j], b_memx[j], lnt, b_lnt)
            for hh in range(2):
                pb, bb = bank()
                for kk in range(4):
                    k = hh * 4 + kk
                    TR_(pb[:, kk * P:(kk + 1) * P], memx[j][:, k * P:(k + 1) * P], identf, [b_memx[j], b_identf], [bb])
                CP_("act", memnT[:, hh * 4:(hh + 1) * 4, j * P:(j + 1) * P], pb.rearrange("p (a b) -> p a b", a=4), [bb], [b_memnT])
        for c in range(2):
            slab, sb_ = get_slab()
            for dd in range(4):
                dchunk = c * 4 + dd
                pb, bb = bank()
                for k in range(8):
                    MM_(pb[:, 0:256], slab[:, k, dd * P:(dd + 1) * P], memnT[:, k, :], k == 0, k == 7, [sb_, b_memnT], [bb])
                CP_("act", KT[:, dchunk, :], pb[:, 0:256], [bb], [b_KT])
        for c in range(2):
            slab, sb_ = get_slab()
            for mc in range(2):
                pb, bb = bank()
                for k in range(8):
                    MM_(pb, memnT[:, k, mc * P:(mc + 1) * P], slab[:, k, :], k == 0, k == 7, [sb_, b_memnT], [bb])
                CP_("dve", Vt[:, mc, c * 512:(c + 1) * 512], pb, [bb], [b_Vt])

        top[0] = const_top + NSLOT * 2048
        xT_bf = alloc(2048, BF16, (8, TT)); b_xT = Buf()
        vn_mg = alloc(2048, BF16)
        v_n = vn_mg.rearrange("p (a b) -> p a b", a=4)
        merged = vn_mg.rearrange("p (a b) -> p a b", a=8)
        b_vnmg = Buf()
        vg = [alloc(D) for _ in range(2)]; b_vg = [Buf() for _ in range(2)]
        ug = [alloc(TT) for _ in range(2)]; b_ug = [Buf() for _ in range(2)]
        ftmp = [alloc(TT) for _ in range(2)]; b_ftmp = [Buf() for _ in range(2)]
        y_conv = alloc(2048, BF16, (8, TT)); b_yconv = Buf()
        y_gmlp = alloc(2048, BF16, (8, TT)); b_ygmlp = Buf()
        y_xa = alloc(2048, BF16, (8, TT)); b_yxa = Buf()
        cc_sb = alloc(TT); b_cc = Buf()
        u_t = [alloc(TT + 4) for _ in range(2)]; b_u = [Buf() for _ in range(2)]
        acc = alloc(TT); b_acc = Buf()
        qT = [alloc(512, BF16, (2, TT)) for _ in range(2)]; b_qT = [Buf() for _ in range(2)]
        PT = [alloc(512, BF16, (2, TT)) for _ in range(2)]; b_PT = [Buf() for _ in range(2)]
        rden = [alloc(TT) for _ in range(2)]; b_rden = [Buf() for _ in range(2)]
        sig = [alloc(3 * TT, F32, (3, TT)) for _ in range(2)]; b_sig = [Buf() for _ in range(2)]
        mt = alloc(3 * TT, F32, (3, TT)); b_mt = Buf()
        xc = [alloc(D) for _ in range(2)]; b_xc = [Buf() for _ in range(2)]
        r2 = [alloc(D) for _ in range(2)]; b_r2 = [Buf() for _ in range(2)]
        x1bf = [alloc(512, BF16) for _ in range(2)]; b_x1bf = [Buf() for _ in range(2)]
        x1T = alloc(D, F32, (8, P)); b_x1T = Buf()
        lgt = alloc(NE); mx8 = alloc(8); mi8 = alloc(8, U32); ef4 = alloc(4); negmax = alloc(1)
        ex4 = alloc(4); ssum = alloc(1); Mbf = alloc(NE // 2, BF16); pos = alloc(NE); junk = alloc(NE)
        pos4 = alloc(4); dstf = alloc(4); keep = alloc(4)
        b_rt = Buf()
        b_mbf = Buf()
        b_x1scr = [Buf() for _ in range(NCH)]
        b_xs = Buf("xs_scr")
        bc_reg = {}

        S.barrier()

        for ti in range(n_tiles):
            t0 = ti * TT
            if ti == 0:
                DMA_("pool", xT_bf, xT_d[:, t0:t0 + TT].rearrange("(k p) t -> p k t", p=P), (), [b_xT])

            slabA = [get_slab(), get_slab()]
            for j in range(4):
                vgj, bvg = vg[j % 2], b_vg[j % 2]
                for h in range(2):
                    pb, bb = bank()
                    sl, sb_ = slabA[h]
                    for k in range(8):
                        MM_(pb, xT_bf[:, k, j * P:(j + 1) * P], sl[:, k, :], k == 0, k == 7, [b_xT, sb_], [bb])
                    ACT_(vgj[:, h * 512:(h + 1) * 512], pb, AF.Gelu_apprx_tanh, [bb], [bvg])
                layer_norm(vgj, bvg, gln_g, gln_b, b_gln, v_n[:, j, :], b_vnmg, lnt, b_lnt)

            for c in range(8):
                sl, sb_ = get_slab()
                pcs = []
                for r in range(3):
                    pb, bb = bank()
                    for k in range(8):
                        MM_(pb, sl[:, k, r * P:(r + 1) * P], xT_bf[:, k, :], k == 0, k == 7, [sb_, b_xT], [bb])
                    pcs.append((pb, bb))
                (pcb, bcb), (pcc, bcc), (pch, bch) = pcs
                u, bu = u_t[c % 2], b_u[c % 2]
                CP_("act", cc_sb, pcc, [bcc], [b_cc])
                CP_("pool", u[:, 0:2], halo[:, c, :], [b_halo[c]], [bu])
                TT_("dve", u[:, 2:TT + 2], cc_sb, pch, ALU.mult, [b_cc, bch], [bu])
                CP_("pool", halo[:, c, :], u[:, TT:TT + 2], [bu], [b_halo[c]])
                TS_("dve", acc, u[:, 2:TT + 2], convw[:, 16 + c:17 + c], None, ALU.mult, None, [bu, b_convw], [b_acc])
                STT_("dve", acc, u[:, 1:TT + 1], convw[:, 8 + c:9 + c], acc, ALU.mult, ALU.add, [bu, b_convw, b_acc], [b_acc])
                STT_("dve", acc, u[:, 0:TT], convw[:, c:c + 1], acc, ALU.mult, ALU.add, [bu, b_convw, b_acc], [b_acc])
                TT_("dve", y_conv[:, c, :], acc, pcb, ALU.mult, [b_acc, bcb], [b_yconv])

            slabB = [get_slab(), get_slab()]
            for g in range(8):
                sl, sb_ = slabB[g // 4]
                gg = g % 4
                pu, bpu = bank()
                for k in range(8):
                    MM_(pu, sl[:, k, gg * P:(gg + 1) * P], xT_bf[:, k, :], k == 0, k == 7, [sb_, b_xT], [bpu])
                ACT_(ug[g % 2], pu, AF.Gelu_apprx_tanh, [bpu], [b_ug[g % 2]])
                pf, bpf = bank()
                for j in range(4):
                    MM_(pf[:, j * P:(j + 1) * P], v_n[:, j, g * P:(g + 1) * P], WcT[:, g, :], True, True, [b_vnmg, b_WcT], [bpf])
                TT_("dve", ftmp[g % 2].rearrange("p (j t) -> p j t", j=4), pf.rearrange("p (j t) -> p j t", j=4),
                    biasbc[:, g, :].unsqueeze(1).broadcast_to([P, 4, P]), ALU.add, [bpf, b_biasbc], [b_ftmp[g % 2]])
                TT_("pool", y_gmlp[:, g, :], ftmp[g % 2], ug[g % 2], ALU.mult, [b_ftmp[g % 2], b_ug[g % 2]], [b_ygmlp])

            slabD = [get_slab(), get_slab()]
            for h in range(4):
                sl, sb_ = slabD[h // 2]
                q, bq = qT[h % 2], b_qT[h % 2]
                for dc in range(2):
                    cc = (h % 2) * 2 + dc
                    pb, bb = bank()
                    for k in range(8):
                        MM_(pb, sl[:, k, cc * P:(cc + 1) * P], xT_bf[:, k, :], k == 0, k == 7, [sb_, b_xT], [bb])
                    CP_("act", q[:, dc, :], pb, [bb], [bq])
                pt, bpt = PT[h % 2], b_PT[h % 2]
                for mc in range(2):
                    pb, bb = bank()
                    for dc in range(2):
                        MM_(pb, KT[:, h * 2 + dc, mc * P:(mc + 1) * P], q[:, dc, :], dc == 0, dc == 1, [b_KT, bq], [bb])
                    ACT_(pt[:, mc, :], pb, AF.Exp, [bb], [bpt], scale=0.0625)
                pden, bden = bank()
                for mc in range(2):
                    MM_(pden, onesb, pt[:, mc, :], mc == 0, mc == 1, [b_onesb, bpt], [bden])
                rd, brd = rden[h % 2], b_rden[h % 2]
                S.op("dve", lambda e, rd=rd, pden=pden: e.reciprocal(out=rd, in_=pden), [bden], [brd])
                for dc in range(2):
                    po, bpo = bank()
                    for mc in range(2):
                        MM_(po, Vt[:, mc, (h * 2 + dc) * P:(h * 2 + dc + 1) * P], pt[:, mc, :], mc == 0, mc == 1, [b_Vt, bpt], [bpo])
                    TT_("dve", y_xa[:, h * 2 + dc, :], rd, po, ALU.mult, [brd, bpo], [b_yxa])

            ys = [(y_conv, b_yconv), (y_gmlp, b_ygmlp), (y_xa, b_yxa)]
            for dc in range(8):
                slg, sbg = get_slab()
                slp, sbp = get_slab()
                sg, bsg = sig[dc % 2], b_sig[dc % 2]
                for r in range(3):
                    pb, bb = bank()
                    for k in range(8):
                        MM_(pb, slg[:, k, r * P:(r + 1) * P], xT_bf[:, k, :], k == 0, k == 7, [sbg, b_xT], [bb])
                    ACT_(sg[:, r, :], pb, AF.Sigmoid, [bb, b_bgate], [bsg], bias=bgate[:, r * 8 + dc:r * 8 + dc + 1])
                for r in range(3):
                    pb, bb = bank()
                    yr, byr = ys[r]
                    for k in range(8):
                        MM_(pb, slp[:, k, r * P:(r + 1) * P], yr[:, k, :], k == 0, k == 7, [sbp, byr], [bb])
                    TT_("dve", mt[:, r, :], sg[:, r, :], pb, ALU.mult, [bsg, bb], [b_mt])
                TT_("pool", mt[:, 0, :], mt[:, 0, :], mt[:, 1, :], ALU.add, [b_mt], [b_mt])
                TT_("pool", merged[:, dc, :], mt[:, 0, :], mt[:, 2, :], ALU.add, [b_mt], [b_vnmg])

            if ti + 1 < n_tiles:
                DMA_("pool", xT_bf, xT_d[:, t0 + TT:t0 + 2 * TT].rearrange("(k p) t -> p k t", p=P), (), [b_xT])

            slabF = [get_slab(), get_slab()]

            wbanks = {}

            def stage1_mm(j):
                ci = ti * 4 + j
                xcj, bxc = xc[j % 2], b_xc[j % 2]
                DMA_("sp", xcj, x_d[ci * P:(ci + 1) * P, :], (), [bxc])
                wbanks[j] = []
                for h in range(2):
                    pb, bb = bank()
                    sl, sb_ = slabF[h]
                    for k in range(8):
                        MM_(pb, merged[:, k, j * P:(j + 1) * P], sl[:, k, :], k == 0, k == 7, [b_vnmg, sb_], [bb])
                    wbanks[j].append((pb, bb))

            def stage1_ew(j):
                ci = ti * 4 + j
                r_t, b_r = r2[j % 2], b_r2[j % 2]
                xcj, bxc = xc[j % 2], b_xc[j % 2]
                for h in range(2):
                    pb, bb = wbanks[j][h]
                    STT_("dve", r_t[:, h * 512:(h + 1) * 512], xcj[:, h * 512:(h + 1) * 512], ALPHA, pb, ALU.mult, ALU.add,
                         [bxc, bb], [b_r])
                layer_norm(r_t, b_r, l1_g, l1_b, b_l1, r_t, b_r, lnt, b_lnt)
                DMA_("sp", x1_scr[ci * P:(ci + 1) * P, :], r_t, [b_r], [b_x1scr[ci]])
                xb, bxb = x1bf[j % 2], b_x1bf[j % 2]
                CP_("act", xb, r_t, [b_r], [bxb])

            def stage2(j):
                ci = ti * 4 + j
                r_t, b_r = r2[j % 2], b_r2[j % 2]
                xb, bxb = x1bf[j % 2], b_x1bf[j % 2]
                for hh in range(2):
                    pb, bb = bank()
                    for kk in range(4):
                        k = hh * 4 + kk
                        TR_(pb[:, kk * P:(kk + 1) * P], r_t[:, k * P:(k + 1) * P], identf, [b_r, b_identf], [bb])
                    CP_("act", x1T[:, hh * 4:(hh + 1) * 4, :], pb.rearrange("p (a b) -> p a b", a=4), [bb], [b_x1T])
                pl, bpl = bank()
                for k in range(8):
                    MM_(pl[:, 0:NE], x1T[:, k, :], wr_sb[:, k, :], k == 0, k == 7, [b_x1T, b_wr], [bpl])
                TT_("dve", lgt, pl[:, 0:NE], br_bc, ALU.add, [bpl, b_br], [b_rt])
                S.op("dve", lambda e: e.max(out=mx8, in_=lgt), [b_rt], [b_rt])
                TS_("dve", Mbf, lgt, mx8[:, 3:4], None, ALU.is_ge, None, [b_rt], [b_mbf])
                S.op("dve", lambda e: e.max_index(out=mi8, in_max=mx8, in_values=lgt), [b_rt], [b_rt])
                TS_("dve", negmax, mx8[:, 0:1], -1.0, None, ALU.mult, None, [b_rt], [b_rt])
                MS_("dve", ssum, 0.0, [b_rt])
                MS_("dve", pos4, 0.0, [b_rt])
                ACT_(ex4, mx8[:, 0:4], AF.Exp, [b_rt], [b_rt], bias=negmax, accum=ssum)
                S.op("dve", lambda e: e.reciprocal(out=ssum, in_=ssum), [b_rt], [b_rt])
                TS_("dve", gate_all[:, ci, :], ex4, ssum, None, ALU.mult, None, [b_rt], [b_gate_all[ci]])
                pp, bpp = bank()
                MM_(pp[:, 0:NE], ustrb, Mbf, True, True, [b_ustr, b_mbf], [bpp])
                MM_(pp[:, NE:2 * NE], onesb, Mbf, True, True, [b_onesb, b_mbf], [bpp])
                TT_("dve", pos, pp[:, 0:NE], tot_bc, ALU.add, [bpp, b_tot], [b_rt])
                TT_("dve", tot_bc, pp[:, NE:2 * NE], tot_bc, ALU.add, [bpp, b_tot], [b_tot])
                CP_("dve", ef4, mi8[:, 0:4], [b_rt], [b_rt])
                for k in range(4):
                    STT_("dve", junk, iota32, ef4[:, k:k + 1], pos, ALU.is_equal, ALU.mult, [b_iota, b_rt], [b_rt],
                         accum=pos4[:, k:k + 1])
                STT_("dve", dstf, ef4, float(CAP), pos4, ALU.mult, ALU.add, [b_rt], [b_rt])
                TS_("dve", keep, pos4, float(CAP), None, ALU.is_lt, None, [b_rt], [b_rt])
                TS_("dve", dstf, dstf, -float(ZROW), None, ALU.add, None, [b_rt], [b_rt])
                TT_("dve", dstf, dstf, keep, ALU.mult, [b_rt], [b_rt])
                TS_("dve", dstf, dstf, float(ZROW), None, ALU.add, None, [b_rt], [b_rt])
                CP_("dve", dst_all[:, ci, :], dstf, [b_rt], [b_dst_all[ci]])
                for k in range(4):
                    def scat(e, ci=ci, k=k, xb=xb):
                        if "r" not in bc_reg:
                            bc_reg["r"] = e.to_reg(NROWS - 1)
                        return e.indirect_dma_start(
                            out=xs_scr, out_offset=bass.IndirectOffsetOnAxis(ap=dst_all[:, ci, k:k + 1], axis=0),
                            in_=xb, in_offset=None, bounds_check=bc_reg["r"], oob_is_err=False)
                    S.op("pool", scat, [bxb, b_dst_all[ci]], [b_xs], dma=True)

            stage1_mm(0)
            stage1_ew(0)
            for j in range(4):
                if j + 1 < 4:
                    stage1_mm(j + 1)
                stage2(j)
                if j + 1 < 4:
                    stage1_ew(j + 1)

        S.barrier()

        top[0] = persist_top
        DMA_("sp", lnC_g, l2g_d.partition_broadcast(P), (), [b_lnC])
        DMA_("sp", lnC_b, l2b_d.partition_broadcast(P), (), [b_lnC])
        wgu = [alloc(8192, BF16, (8, 2 * D)) for _ in range(2)]
        b_wgu = [[Buf() for _ in range(8)] for _ in range(2)]
        wdn = [alloc(4096, BF16, (8, D)) for _ in range(2)]
        b_wdn = [[Buf() for _ in range(8)] for _ in range(2)]
        xs_sm = [alloc(512, BF16) for _ in range(NST)]; b_xssm = [Buf() for _ in range(NST)]
        xsT2 = [alloc(4 * CAP, BF16, (8, CAP)) for _ in range(2)]; b_xsT2 = [[Buf() for _ in range(NST)] for _ in range(2)]
        actT = alloc(4 * CAP, BF16, (8, CAP)); b_actT = Buf()
        g_sb = [alloc(512) for _ in range(2)]; b_g = [Buf() for _ in range(2)]
        s_sb = [alloc(512) for _ in range(2)]; b_s = [Buf() for _ in range(2)]
        u_sb = [alloc(512) for _ in range(2)]; b_us = [Buf() for _ in range(2)]
        eo_sb = [alloc(D) for _ in range(2)]; b_eosb = [Buf() for _ in range(2)]
        bdbc = [alloc(D) for _ in range(2)]; b_bdbc = [Buf() for _ in range(2)]
        bgu1 = alloc(NE * 16); b_bgu1 = Buf()
        TS_("dve", bgu1, bgu_sb, 1.0, None, ALU.add, None, [b_bgu], [b_bgu1])
        bankbf2 = [banks[6][:, :].bitcast(BF16), banks[7][:, :].bitcast(BF16)]

        def load_expert(e):
            bi = e % 2
            for k in range(8):
                DMA_("pool", wgu[bi][:, k, :], wgu_d[e, k * P:(k + 1) * P, :], (), [b_wgu[bi][k]])
            for k in range(8):
                DMA_("pool", wdn[bi][:, k, :], wd_d[e, k * P:(k + 1) * P, :], (), [b_wdn[bi][k]])
            DMA_("sp", bdbc[bi], bd_d[e:e + 1, :].partition_broadcast(P), (), [b_bdbc[bi]])

        def prefetch_xs(e):
            for s in range(NST):
                DMA_("sp", xs_sm[s], xs_scr[e * CAP + s * P:e * CAP + (s + 1) * P, :], [b_xs], [b_xssm[s]])

        def transposes(e):
            xsT, b_xsT = xsT2[e % 2], b_xsT2[e % 2]
            for s in range(NST):
                xm, bxm = xs_sm[s], b_xssm[s]
                bankbf, bbf = bankbf2[s % 2], bank_bufs[6 + s % 2]
                for k in range(8):
                    TR_(bankbf[:, k * P:(k + 1) * P], xm[:, k * P:(k + 1) * P], identb, [bxm, b_identb], [bbf])
                CP_("act", xsT[:, :, s * P:(s + 1) * P], bankbf.rearrange("p (a b) -> p a b", a=8), [bbf], [b_xsT[s]])

        splits = [(0, 512), (512, CAP)]
        split_tiles = [[s for s in range(NST) if n0 <= s * P < n1] for (n0, n1) in splits]
        if n_exp > 0:
            load_expert(0)
            prefetch_xs(0)
            transposes(0)
        cnt = 0
        for e in range(n_exp):
            bi = e % 2
            if e + 1 < n_exp:
                load_expert(e + 1)
                prefetch_xs(e + 1)
            xsT, b_xsT = xsT2[bi], b_xsT2[bi]
            for (n0, n1), tiles in zip(splits, split_tiles):
                w = n1 - n0
                rb = [b_xsT[s] for s in tiles]
                for f in range(8):
                    pgs = []
                    for m in (f, 8 + f):
                        i = bank_rr[0] % 6
                        bank_rr[0] += 1
                        pb, bb = banks[i][:, :], bank_bufs[i]
                        for k in range(8):
                            MM_(pb[:, 0:w], wgu[bi][:, k, m * P:(m + 1) * P], xsT[:, k, n0:n1], k == 0, k == 7,
                                [b_wgu[bi][k]] + rb, [bb])
                        pgs.append((pb, bb))
                    (pg, bpg), (pu, bpu) = pgs
                    gi = cnt % 2
                    cnt += 1
                    gs, us, ss = g_sb[gi][:, 0:w], u_sb[gi][:, 0:w], s_sb[gi][:, 0:w]
                    TS_("dve", gs, pg[:, 0:w], bgu_sb[:, e * 16 + f:e * 16 + f + 1], 7.0, ALU.add, ALU.min, [bpg, b_bgu], [b_g[gi]])
                    ACT_(ss, gs, AF.Sigmoid, [b_g[gi]], [b_s[gi]], scale=1.702)
                    TS_("dve", us, pu[:, 0:w], bgu1[:, e * 16 + 8 + f:e * 16 + 9 + f], 8.0, ALU.add, ALU.min, [bpu, b_bgu1], [b_us[gi]])
                    TT_("dve", gs, gs, ss, ALU.mult, [b_g[gi], b_s[gi]], [b_g[gi]])
                    STT_("dve", actT[:, f, n0:n1], us, -6.0, gs, ALU.max, ALU.mult, [b_us[gi], b_g[gi]], [b_actT])
            if e + 1 < n_exp:
                transposes(e + 1)
            for s in range(NST):
                eo, beo = eo_sb[s % 2], b_eosb[s % 2]
                for h in range(2):
                    i = bank_rr[0] % 6
                    bank_rr[0] += 1
                    pb, bb = banks[i][:, :], bank_bufs[i]
                    for f in range(8):
                        MM_(pb, actT[:, f, s * P:(s + 1) * P], wdn[bi][:, f, h * 512:(h + 1) * 512], f == 0, f == 7,
                            [b_actT, b_wdn[bi][f]], [bb])
                    TT_("dve", eo[:, h * 512:(h + 1) * 512], pb, bdbc[bi][:, h * 512:(h + 1) * 512], ALU.add, [bb, b_bdbc[bi]], [beo])
                DMA_("sp", eo_scr[e * CAP + s * P:e * CAP + (s + 1) * P, :], eo, [beo], [b_eo])

        S.barrier()

        top[0] = persist_top
        NB4 = 3
        eo4 = [alloc(4 * D, F32, (4, D)) for _ in range(NB4)]; b_eo4 = [Buf() for _ in range(NB4)]
        x1c = [alloc(D) for _ in range(NB4)]; b_x1c = [Buf() for _ in range(NB4)]
        accd = [alloc(D) for _ in range(2)]; b_accd = [Buf() for _ in range(2)]
        lnt2 = (alloc(12, F32, (2, 6)), alloc(2), alloc(1)); b_lnt2 = Buf()
        n_ch = n_tiles * 4

        def fetch(ci):
            e4, be4 = eo4[ci % NB4], b_eo4[ci % NB4]
            for k in range(4):
                S.op("pool", lambda e, ci=ci, k=k, e4=e4: e.indirect_dma_start(
                    out=e4[:, k, :], out_offset=None, in_=eo_scr,
                    in_offset=bass.IndirectOffsetOnAxis(ap=dst_all[:, ci, k:k + 1], axis=0)),
                    [b_eo, b_dst_all[ci]], [be4], dma=True)
            DMA_("sp", x1c[ci % NB4], x1_scr[ci * P:(ci + 1) * P, :], [b_x1scr[ci]], [b_x1c[ci % NB4]])

        for ci in range(min(NB4 - 1, n_ch)):
            fetch(ci)
        for ci in range(n_ch):
            if ci + NB4 - 1 < n_ch:
                fetch(ci + NB4 - 1)
            e4, be4 = eo4[ci % NB4], b_eo4[ci % NB4]
            xx, bxx = x1c[ci % NB4], b_x1c[ci % NB4]
            a, ba = accd[ci % 2], b_accd[ci % 2]
            TS_("dve", a, e4[:, 0, :], gate_all[:, ci, 0:1], None, ALU.mult, None, [be4, b_gate_all[ci]], [ba])
            for k in range(1, 4):
                STT_("dve", a, e4[:, k, :], gate_all[:, ci, k:k + 1], a, ALU.mult, ALU.add, [be4, b_gate_all[ci], ba], [ba])
            STT_("dve", a, xx, ALPHA, a, ALU.mult, ALU.add, [bxx, ba], [ba])
            layer_norm(a, ba, lnC_g, lnC_b, b_lnC, a, ba, lnt2, b_lnt2, aff="dve")
            out_dmas.append(DMA_("sp", out_d[ci * P:(ci + 1) * P, :], a, [ba], ()))

        S.emit_all(final_waits=[("sp", d) for d in out_dmas])
    return nc


def _host_layout(inp, b):
    f = np.float32
    w_in = inp["w_in"][0]
    cb, cc, ch, gu, gv, q, gates = np.split(w_in, np.cumsum([1024, 1024, 1024, 1024, 1024, 1024])[:], axis=1)
    segs = [inp["w_kv"][0], gv, gu]
    for c in range(8):
        sl = slice(c * 128, (c + 1) * 128)
        segs += [cb[:, sl], cc[:, sl], ch[:, sl]]
    segs.append(q)
    wcp, wgp, wxp = inp["w_conv_proj"][0], inp["w_gmlp_proj"][0], inp["w_xa_proj"][0]
    for dc in range(8):
        sl = slice(dc * 128, (dc + 1) * 128)
        segs += [gates[:, 0:1024][:, sl], gates[:, 1024:2048][:, sl], gates[:, 2048:3072][:, sl], wcp[:, sl], wgp[:, sl], wxp[:, sl]]
    segs.append(inp["w_out"][0])
    wmix = np.ascontiguousarray(np.concatenate(segs, axis=1), dtype=f)
    assert wmix.shape == (D, WCOLS)
    shared = {
        "wmix": wmix,
        "b_gate_T": np.ascontiguousarray(inp["b_gate"][0].reshape(24, 128).T, dtype=f),
        "conv_w_T": np.ascontiguousarray(inp["conv_w"][0].reshape(3, 8, 128).transpose(2, 0, 1).reshape(128, 24), dtype=f),
        "gmlp_wsT": np.ascontiguousarray(inp["gmlp_ws"][0].transpose(2, 0, 1).reshape(128, 8 * 128), dtype=f),
        "gmlp_b": np.ascontiguousarray(inp["gmlp_b"][0].reshape(1, D), dtype=f),
        "gmlp_ln_g": np.ascontiguousarray(inp["gmlp_ln_g"], dtype=f), "gmlp_ln_b": np.ascontiguousarray(inp["gmlp_ln_b"], dtype=f),
        "mem_ln_g": np.ascontiguousarray(inp["mem_ln_g"], dtype=f), "mem_ln_b": np.ascontiguousarray(inp["mem_ln_b"], dtype=f),
        "ln1_g": np.ascontiguousarray(inp["ln1_g"], dtype=f), "ln1_b": np.ascontiguousarray(inp["ln1_b"], dtype=f),
        "ln2_g": np.ascontiguousarray(inp["ln2_g"], dtype=f), "ln2_b": np.ascontiguousarray(inp["ln2_b"], dtype=f),
        "w_router": np.ascontiguousarray(inp["w_router"][0], dtype=f),
        "b_router": np.ascontiguousarray(inp["b_router"], dtype=f),
        "w_gate_up": np.ascontiguousarray(inp["w_gate_up"][0], dtype=f),
        "b_gate_up_T": np.ascontiguousarray(inp["b_gate_up"][0].reshape(32, 16, 128).transpose(2, 0, 1).reshape(128, 512), dtype=f),
        "w_down": np.ascontiguousarray(inp["w_down"][0], dtype=f),
        "b_down": np.ascontiguousarray(inp["b_down"][0], dtype=f),
    }
    return shared


def _core_inputs(inp, shared, b):
    m = dict(shared)
    xb = np.ascontiguousarray(inp["x"][b], dtype=np.float32)
    m["x"] = xb
    m["xT"] = np.ascontiguousarray(xb.T)
    m["mem"] = np.ascontiguousarray(inp["mem"][b], dtype=np.float32)
    return m


def kernel(**inputs):
    inp = {k: np.asarray(v) for k, v in inputs.items()}
    nb = inp["x"].shape[0]
    shared = _host_layout(inp, 0)
    in_maps = [_core_inputs(inp, shared, b) for b in range(nb)]
    nc = build()
    res = run_bass_kernel_spmd(nc, in_maps, core_ids=list(range(nb)))
    out = np.stack([np.asarray(r["out"]) for r in res.results], axis=0)
    return out.astype(np.float32, copy=False)
```

```python
import contextlib
import numpy as np
import concourse.bass as bass
import concourse.mybir as mybir
from concourse.bass_utils import run_bass_kernel_spmd

F32 = mybir.dt.float32
BF16 = mybir.dt.bfloat16
I32 = mybir.dt.int32
U32 = mybir.dt.uint32
AF = mybir.ActivationFunctionType
ALU = mybir.AluOpType

ENGS = ("pe", "act", "dve", "pool", "sp")


class Buf:
    __slots__ = ("name", "writers", "readers")

    def __init__(self, name=""):
        self.name = name
        self.writers = []
        self.readers = []


class Op:
    __slots__ = ("eng", "emit", "deps", "is_dma", "signal", "val", "sem", "idx")

    def __init__(self, eng, emit, is_dma):
        self.eng = eng
        self.emit = emit
        self.deps = []
        self.is_dma = is_dma
        self.signal = False
        self.val = None
        self.sem = None
        self.idx = None


class Sched:
    def __init__(self, nc, n_dma_sems=None):
        self.nc = nc
        self.streams = {e: [] for e in ENGS}
        self.n_dma_sems = n_dma_sems or {"sp": 8, "act": 2, "pool": 4}
        self.last = {}
        self.dmas_since_barrier = []

    @staticmethod
    def _same_inorder(a, b):
        return (not a.is_dma) and (not b.is_dma) and a.eng == b.eng

    def op(self, eng, emit, reads=(), writes=(), dma=False, extra_deps=()):
        o = Op(eng, emit, dma)
        o.idx = len(self.streams[eng])
        deps = list(extra_deps)
        for b in reads:
            deps.extend(b.writers)
        for b in writes:
            deps.extend(b.readers)
            deps.extend(b.writers)
        seen = set()
        for d in deps:
            if d is o or id(d) in seen:
                continue
            seen.add(id(d))
            if eng == "pe" and d.eng == "pe" and not d.is_dma and not dma:
                continue
            o.deps.append(d)
            d.signal = True
        for b in reads:
            b.readers = [r for r in b.readers if not self._same_inorder(r, o)] + [o]
        for b in writes:
            if b.readers:
                b.readers = []
                b.writers = []
            b.writers = [w for w in b.writers if not self._same_inorder(w, o)] + [o]
        self.streams[eng].append(o)
        if dma:
            self.dmas_since_barrier.append(o)
        else:
            self.last[eng] = o
        return o

    def barrier(self):
        deps = list(self.last.values()) + list(self.dmas_since_barrier)
        self.dmas_since_barrier = []
        for e in ENGS:
            self.op(e, lambda eng: eng.nop(), extra_deps=deps)

    def emit_all(self, final_waits=()):
        nc = self.nc
        with contextlib.ExitStack() as st:
            csem = {e: st.enter_context(nc.semaphore("c_" + e)) for e in ("pe", "act", "dve", "pool", "sp")}
            dsems = {e: [st.enter_context(nc.semaphore("d_%s%d" % (e, i))) for i in range(n)]
                     for e, n in self.n_dma_sems.items()}
            for e in ENGS:
                cnt = 0
                nd = self.n_dma_sems.get(e, 0)
                dcnt = [0] * max(nd, 1)
                rr = 0
                for o in self.streams[e]:
                    if o.is_dma:
                        s = rr % nd
                        rr += 1
                        dcnt[s] += 1
                        o.sem = dsems[e][s]
                        o.val = 16 * dcnt[s]
                    elif o.signal:
                        cnt += 1
                        o.sem = csem[e]
                        o.val = cnt
            fw = {}
            for (e, d) in final_waits:
                fw.setdefault(e, []).append(d)
            block = st.enter_context(nc.Block())
            streams = self.streams

            def make(e):
                def body(eng):
                    seen = {}

                    def wait(sem, val):
                        k = id(sem)
                        if seen.get(k, 0) < val:
                            eng.wait_ge(sem, val)
                            seen[k] = val

                    for o in streams[e]:
                        if o.is_dma and o.val > 16:
                            wait(o.sem, o.val - 16)
                        mx = {}
                        for d in o.deps:
                            k = id(d.sem)
                            if k not in mx or mx[k][1] < d.val:
                                mx[k] = (d.sem, d.val)
                        for sem_, val_ in mx.values():
                            wait(sem_, val_)
                        ins = o.emit(eng)
                        if o.is_dma:
                            ins.then_inc(o.sem, 16)
                        elif o.signal:
                            ins.then_inc(o.sem, 1)
                    for d in fw.get(e, ()):
                        wait(d.sem, d.val)
                return body

            for e, deco in (("pe", block.tensor), ("act", block.scalar), ("dve", block.vector),
                            ("pool", block.gpsimd), ("sp", block.sync)):
                if streams[e] or e in fw:
                    deco(make(e))


P = 128
D = 1024
SEQ = 4096
TT = 512
NCH = SEQ // P
NE = 32
CAP = 768
NST = CAP // P
NROWS = NE * CAP
ZROW = NROWS
ALPHA = 2.0 ** 0.25
EPS = 1e-5
SEG_KV = 0
SEG_A = 2048
SEG_B = SEG_A + 1024
SEG_C = SEG_B + 1024
SEG_D = SEG_C + 3072
SEG_E = SEG_D + 1024
SEG_F = SEG_E + 8 * 768
WCOLS = SEG_F + 1024


def build(n_tiles=SEQ // TT, n_exp=NE, debug=False):
    nc = bass.Bass("TRN2", target_bir_lowering=False)

    def din(name, shape, dt=F32):
        return nc.dram_tensor(name, list(shape), dt, kind="ExternalInput").ap()

    xT_d = din("xT", [D, SEQ])
    x_d = din("x", [SEQ, D])
    mem_d = din("mem", [256, D])
    wmix_d = din("wmix", [D, WCOLS])
    bgate_d = din("b_gate_T", [P, 24])
    convw_d = din("conv_w_T", [P, 24])
    wsT_d = din("gmlp_wsT", [P, 8 * P])
    gmlpb_d = din("gmlp_b", [1, D])
    glng_d = din("gmlp_ln_g", [1, D])
    glnb_d = din("gmlp_ln_b", [1, D])
    mlng_d = din("mem_ln_g", [1, D])
    mlnb_d = din("mem_ln_b", [1, D])
    l1g_d = din("ln1_g", [1, D])
    l1b_d = din("ln1_b", [1, D])
    l2g_d = din("ln2_g", [1, D])
    l2b_d = din("ln2_b", [1, D])
    wr_d = din("w_router", [D, NE])
    br_d = din("b_router", [1, NE])
    wgu_d = din("w_gate_up", [NE, D, 2 * D])
    bgu_d = din("b_gate_up_T", [P, NE * 16])
    wd_d = din("w_down", [NE, D, D])
    bd_d = din("b_down", [NE, D])
    out_d = nc.dram_tensor("out", [SEQ, D], F32, kind="ExternalOutput").ap()
    if debug:
        x1_scr = nc.dram_tensor("x1_dbg", [SEQ, D], F32, kind="ExternalOutput").ap()
    else:
        x1_scr = nc.dram_tensor("x1_scr", [SEQ, D], F32).ap()
    wmix_bf = nc.dram_tensor("wmix_bf", [D, WCOLS], BF16).ap()
    xs_scr = nc.dram_tensor("xs_scr", [NROWS, D], BF16).ap()
    eo_scr = nc.dram_tensor("eo_scr", [NROWS + 1, D], F32).ap()

    S = Sched(nc)
    out_dmas = []

    with contextlib.ExitStack() as st:
        NF = 52224
        big = st.enter_context(nc.sbuf_tensor("arena", [P, NF], F32))
        banks = [st.enter_context(nc.psum_tensor("bank%d" % i, [P, 512], F32)) for i in range(8)]
        bank_bufs = [Buf("bank%d" % i) for i in range(8)]
        bank_rr = [0]

        def bank():
            i = bank_rr[0] % 8
            bank_rr[0] += 1
            return banks[i][:, :], bank_bufs[i]

        top = [0]

        def alloc(ncols, dt=F32, shape=None):
            a = big[:, top[0]:top[0] + ncols]
            top[0] += ncols
            assert top[0] <= NF, "SBUF arena overflow %d" % top[0]
            if dt != F32:
                a = a.bitcast(dt)
            if shape is not None:
                names = " ".join("d%d" % i for i in range(len(shape)))
                kw = {"d%d" % i: s for i, s in enumerate(shape)}
                a = a.rearrange("p (%s) -> p %s" % (names, names), **kw)
            return a

        def TT_(eng, out, in0, in1, op, R, W):
            return S.op(eng, lambda e: e.tensor_tensor(out=out, in0=in0, in1=in1, op=op), R, W)

        def TS_(eng, out, in0, s1, s2, op0, op1, R, W):
            if op1 is None:
                return S.op(eng, lambda e: e.tensor_scalar(out=out, in0=in0, scalar1=s1, scalar2=None, op0=op0), R, W)
            return S.op(eng, lambda e: e.tensor_scalar(out=out, in0=in0, scalar1=s1, scalar2=s2, op0=op0, op1=op1), R, W)

        def STT_(eng, out, in0, sc, in1, op0, op1, R, W, accum=None):
            if accum is None:
                return S.op(eng, lambda e: e.scalar_tensor_tensor(out=out, in0=in0, scalar=sc, in1=in1, op0=op0, op1=op1), R, W)
            return S.op(eng, lambda e: e.scalar_tensor_tensor(out=out, in0=in0, scalar=sc, in1=in1, op0=op0, op1=op1,
                                                              accum_out=accum), R, W)

        def ACT_(out, in_, func, R, W, bias=None, scale=1.0, accum=None):
            kw = {}
            if bias is not None:
                kw["bias"] = bias
            if accum is not None:
                kw["accum_out"] = accum
            return S.op("act", lambda e: e.activation(out=out, in_=in_, func=func, scale=scale, **kw), R, W)

        def CP_(eng, out, in_, R, W):
            if eng == "act":
                return S.op("act", lambda e: e.copy(out=out, in_=in_), R, W)
            return S.op(eng, lambda e: e.tensor_copy(out=out, in_=in_), R, W)

        def MS_(eng, ap, val, W):
            return S.op(eng, lambda e: e.memset(ap, val), (), W)

        def MM_(out, lhsT, rhs, start, stop, R, W):
            return S.op("pe", lambda e: e.matmul(out, lhsT=lhsT, rhs=rhs, start=start, stop=stop), R, W)

        def TR_(out, in_, ident, R, W):
            return S.op("pe", lambda e: e.transpose(out=out, in_=in_, identity=ident), R, W)

        def DMA_(eng, out, in_, R, W):
            return S.op(eng, lambda e: e.dma_start(out=out, in_=in_), R, W, dma=True)

        def layer_norm(xin, b_x, g_bc, b_bc, b_par, out, b_out, tmp, b_tmp, aff="dve"):
            st6, mv, rstd = tmp
            for h in range(2):
                S.op("dve", lambda e, h=h: e.bn_stats(out=st6[:, h, :], in_=xin[:, h * 512:(h + 1) * 512]), [b_x], [b_tmp])
            S.op("dve", lambda e: e.bn_aggr(out=mv, in_=st6.rearrange("p a b -> p (a b)")), [b_tmp], [b_tmp])
            ACT_(rstd, mv[:, 1:2], AF.Sqrt, [b_tmp, b_eps], [b_tmp], bias=eps_t)
            S.op("dve", lambda e: e.reciprocal(out=rstd, in_=rstd), [b_tmp], [b_tmp])
            TS_("dve", xin, xin, mv[:, 0:1], rstd, ALU.subtract, ALU.mult, [b_x, b_tmp], [b_x])
            TT_("dve", xin, xin, g_bc, ALU.mult, [b_x, b_par], [b_x])
            TT_(aff, out, xin, b_bc, ALU.add, [b_x, b_par], [b_out])

        identf = alloc(128); b_identf = Buf()
        identb = alloc(64, BF16); b_identb = Buf()
        onesb = alloc(64, BF16); b_onesb = Buf()
        ustrb = alloc(64, BF16); b_ustr = Buf()
        eps_t = alloc(1); b_eps = Buf()
        iota32 = alloc(32); b_iota = Buf()
        wr_sb = alloc(8 * NE, F32, (8, NE)); b_wr = Buf()
        br_bc = alloc(NE); b_br = Buf()
        gate_all = alloc(NCH * 4, F32, (NCH, 4)); b_gate_all = [Buf() for _ in range(NCH)]
        dst_all = alloc(NCH * 4, I32, (NCH, 4)); b_dst_all = [Buf() for _ in range(NCH)]
        tot_bc = alloc(NE); b_tot = Buf()
        bgu_sb = alloc(NE * 16); b_bgu = Buf()
        lnC_g = alloc(D); lnC_b = alloc(D); b_lnC = Buf()
        zrow = alloc(D); b_zrow = Buf()
        lnt = (alloc(12, F32, (2, 6)), alloc(2), alloc(1)); b_lnt = Buf()
        persist_top = top[0]
        WcT = alloc(512, BF16, (8, P)); b_WcT = Buf()
        biasbc = alloc(D, F32, (8, P)); b_biasbc = Buf()
        KT = alloc(1024, BF16, (8, 256)); b_KT = Buf()
        Vt = alloc(1024, BF16, (2, D)); b_Vt = Buf()
        bgate = alloc(24); b_bgate = Buf()
        convw = alloc(24); b_convw = Buf()
        halo = alloc(16, F32, (8, 2)); b_halo = [Buf() for _ in range(8)]
        gln_g = alloc(D); gln_b = alloc(D); b_gln = Buf()
        l1_g = alloc(D); l1_b = alloc(D); b_l1 = Buf()
        mixer_base = top[0]

        MS_("pool", identf, 0.0, [b_identf])
        S.op("pool", lambda e: e.affine_select(out=identf, in_=identf, pattern=[[-1, P]], compare_op=ALU.not_equal,
                                               fill=1.0, base=0, channel_multiplier=1), [b_identf], [b_identf])
        CP_("dve", identb, identf, [b_identf], [b_identb])
        MS_("pool", onesb, 1.0, [b_onesb])
        ustrf = alloc(128); b_ustrf = Buf()
        MS_("pool", ustrf, 1.0, [b_ustrf])
        S.op("pool", lambda e: e.affine_select(out=ustrf, in_=ustrf, pattern=[[1, P]], compare_op=ALU.is_gt,
                                               fill=0.0, base=0, channel_multiplier=-1), [b_ustrf], [b_ustrf])
        CP_("dve", ustrb, ustrf, [b_ustrf], [b_ustr])
        MS_("pool", eps_t, EPS, [b_eps])
        S.op("pool", lambda e: e.iota(iota32, pattern=[[1, NE]], base=0, channel_multiplier=0,
                                      allow_small_or_imprecise_dtypes=True), (), [b_iota])
        MS_("pool", tot_bc, 0.0, [b_tot])
        MS_("pool", halo.rearrange("p a b -> p (a b)"), 0.0, b_halo)
        MS_("pool", zrow, 0.0, [b_zrow])
        b_eo = Buf("eo_scr")
        DMA_("sp", eo_scr[ZROW:ZROW + 1, :], zrow[0:1, :], [b_zrow], [b_eo])
        DMA_("sp", wr_sb, wr_d.rearrange("(k p) n -> p k n", p=P), (), [b_wr])
        DMA_("sp", br_bc, br_d.partition_broadcast(P), (), [b_br])
        DMA_("sp", bgu_sb, bgu_d, (), [b_bgu])
        DMA_("sp", lnC_g, mlng_d.partition_broadcast(P), (), [b_lnC])
        DMA_("sp", lnC_b, mlnb_d.partition_broadcast(P), (), [b_lnC])
        DMA_("sp", biasbc.rearrange("p a b -> p (a b)"), gmlpb_d.partition_broadcast(P), (), [b_biasbc])
        DMA_("sp", bgate, bgate_d, (), [b_bgate])
        DMA_("sp", convw, convw_d, (), [b_convw])
        DMA_("sp", gln_g, glng_d.partition_broadcast(P), (), [b_gln])
        DMA_("sp", gln_b, glnb_d.partition_broadcast(P), (), [b_gln])
        DMA_("sp", l1_g, l1g_d.partition_broadcast(P), (), [b_l1])
        DMA_("sp", l1_b, l1b_d.partition_broadcast(P), (), [b_l1])
        wsTf = alloc(8 * P, F32, (8, P)); b_wsTf = Buf()
        DMA_("sp", wsTf.rearrange("p a b -> p (a b)"), wsT_d, (), [b_wsTf])
        S.op("pool", lambda e: e.affine_select(out=wsTf, in_=wsTf, pattern=[[0, 8], [1, P]], compare_op=ALU.is_ge,
                                               fill=0.0, base=0, channel_multiplier=-1), [b_wsTf], [b_wsTf])
        CP_("dve", WcT, wsTf, [b_wsTf], [b_WcT])
        const_top = top[0]

        NSLOT = 4
        ring = [alloc(2048, BF16, (8, 512)) for _ in range(NSLOT)]
        ring_bufs = [Buf("slab%d" % i) for i in range(NSLOT)]

        slab_list = []
        for c in range(4):
            slab_list.append((SEG_KV + c * 512, 512))
        for _ in range(n_tiles):
            slab_list += [(SEG_A, 512), (SEG_A + 512, 512)]
            slab_list += [(SEG_C + c * 384, 384) for c in range(8)]
            slab_list += [(SEG_B, 512), (SEG_B + 512, 512)]
            slab_list += [(SEG_D, 512), (SEG_D + 512, 512)]
            for dc in range(8):
                slab_list += [(SEG_E + dc * 768, 384), (SEG_E + dc * 768 + 384, 384)]
            slab_list += [(SEG_F, 512), (SEG_F + 512, 512)]
        slab_issued = [0]
        slab_next = [0]

        b_conv = {}

        def issue_slab():
            i = slab_issued[0]
            if i >= len(slab_list):
                return
            c0, w = slab_list[i]
            sl = i % NSLOT
            if c0 not in b_conv:
                DMA_("pool", ring[sl][:, :, 0:w], wmix_d[:, c0:c0 + w].rearrange("(k p) n -> p k n", p=P), (), [ring_bufs[sl]])
                b_conv[c0] = Buf()
                if c0 >= SEG_A and n_tiles > 1:
                    DMA_("sp", wmix_bf[:, c0:c0 + w].rearrange("(k p) n -> p k n", p=P), ring[sl][:, :, 0:w],
                         [ring_bufs[sl]], [b_conv[c0]])
            else:
                DMA_("sp", ring[sl][:, :, 0:w], wmix_bf[:, c0:c0 + w].rearrange("(k p) n -> p k n", p=P), [b_conv[c0]], [ring_bufs[sl]])
            slab_issued[0] += 1

        def get_slab():
            i = slab_next[0]
            slab_next[0] += 1
            while slab_issued[0] < min(i + NSLOT - 1, len(slab_list)):
                issue_slab()
            sl = i % NSLOT
            return ring[sl], ring_bufs[sl]

        memx = [alloc(D) for _ in range(2)]; b_memx = [Buf() for _ in range(2)]
        memnT = alloc(1024, BF16, (8, 256)); b_memnT = Buf()
        for j in range(2):
            DMA_("sp", memx[j], mem_d[j * P:(j + 1) * P, :], (), [b_memx[j]])
            layer_norm(memx[j], b_memx[j], lnC_g, lnC_b, b_lnC, memx[j], b_memx[j], lnt, b_lnt)
            for hh in range(2):
                pb, bb = bank()
                for kk in range(4):
                    k = hh * 4 + kk
                    TR_(pb[:, kk * P:(kk + 1) * P], memx[j][:, k * P:(k + 1) * P], identf, [b_memx[j], b_identf], [bb])
                CP_("act", memnT[:, hh * 4:(hh + 1) * 4, j * P:(j + 1) * P], pb.rearrange("p (a b) -> p a b", a=4), [bb], [b_memnT])
        for c in range(2):
            slab, sb_ = get_slab()
            for dd in range(4):
                dchunk = c * 4 + dd
                pb, bb = bank()
                for k in range(8):
                    MM_(pb[:, 0:256], slab[:, k, dd * P:(dd + 1) * P], memnT[:, k, :], k == 0, k == 7, [sb_, b_memnT], [bb])
                CP_("act", KT[:, dchunk, :], pb[:, 0:256], [bb], [b_KT])
        for c in range(2):
            slab, sb_ = get_slab()
            for mc in range(2):
                pb, bb = bank()
                for k in range(8):
                    MM_(pb, memnT[:, k, mc * P:(mc + 1) * P], slab[:, k, :], k == 0, k == 7, [sb_, b_memnT], [bb])
                CP_("dve", Vt[:, mc, c * 512:(c + 1) * 512], pb, [bb], [b_Vt])

        top[0] = const_top + NSLOT * 2048
        xT_bf = alloc(2048, BF16, (8, TT)); b_xT = Buf()
        vn_mg = alloc(2048, BF16)
        v_n = vn_mg.rearrange("p (a b) -> p a b", a=4)
        merged = vn_mg.rearrange("p (a b) -> p a b", a=8)
        b_vnmg = Buf()
        vg = [alloc(D) for _ in range(2)]; b_vg = [Buf() for _ in range(2)]
        ug = [alloc(TT) for _ in range(2)]; b_ug = [Buf() for _ in range(2)]
        ftmp = [alloc(TT) for _ in range(2)]; b_ftmp = [Buf() for _ in range(2)]
        y_conv = alloc(2048, BF16, (8, TT)); b_yconv = Buf()
        y_gmlp = alloc(2048, BF16, (8, TT)); b_ygmlp = Buf()
        y_xa = alloc(2048, BF16, (8, TT)); b_yxa = Buf()
        cc_sb = alloc(TT); b_cc = Buf()
        u_t = [alloc(TT + 4) for _ in range(2)]; b_u = [Buf() for _ in range(2)]
        acc = alloc(TT); b_acc = Buf()
        qT = [alloc(512, BF16, (2, TT)) for _ in range(2)]; b_qT = [Buf() for _ in range(2)]
        PT = [alloc(512, BF16, (2, TT)) for _ in range(2)]; b_PT = [Buf() for _ in range(2)]
        rden = [alloc(TT) for _ in range(2)]; b_rden = [Buf() for _ in range(2)]
        sig = [alloc(3 * TT, F32, (3, TT)) for _ in range(2)]; b_sig = [Buf() for _ in range(2)]
        mt = alloc(3 * TT, F32, (3, TT)); b_mt = Buf()
        xc = [alloc(D) for _ in range(2)]; b_xc = [Buf() for _ in range(2)]
        r2 = [alloc(D) for _ in range(2)]; b_r2 = [Buf() for _ in range(2)]
        x1bf = [alloc(512, BF16) for _ in range(2)]; b_x1bf = [Buf() for _ in range(2)]
        x1T = alloc(D, F32, (8, P)); b_x1T = Buf()
        lgt = alloc(NE); mx8 = alloc(8); mi8 = alloc(8, U32); ef4 = alloc(4); negmax = alloc(1)
        ex4 = alloc(4); ssum = alloc(1); Mbf = alloc(NE // 2, BF16); pos = alloc(NE); junk = alloc(NE)
        pos4 = alloc(4); dstf = alloc(4); keep = alloc(4)
        b_rt = Buf()
        b_mbf = Buf()
        b_x1scr = [Buf() for _ in range(NCH)]
        b_xs = Buf("xs_scr")
        bc_reg = {}

        S.barrier()

        for ti in range(n_tiles):
            t0 = ti * TT
            if ti == 0:
                DMA_("pool", xT_bf, xT_d[:, t0:t0 + TT].rearrange("(k p) t -> p k t", p=P), (), [b_xT])

            slabA = [get_slab(), get_slab()]
            for j in range(4):
                vgj, bvg = vg[j % 2], b_vg[j % 2]
                for h in range(2):
                    pb, bb = bank()
                    sl, sb_ = slabA[h]
                    for k in range(8):
                        MM_(pb, xT_bf[:, k, j * P:(j + 1) * P], sl[:, k, :], k == 0, k == 7, [b_xT, sb_], [bb])
                    ACT_(vgj[:, h * 512:(h + 1) * 512], pb, AF.Gelu_apprx_tanh, [bb], [bvg])
                layer_norm(vgj, bvg, gln_g, gln_b, b_gln, v_n[:, j, :], b_vnmg, lnt, b_lnt)

            for c in range(8):
                sl, sb_ = get_slab()
                pcs = []
                for r in range(3):
                    pb, bb = bank()
                    for k in range(8):
                        MM_(pb, sl[:, k, r * P:(r + 1) * P], xT_bf[:, k, :], k == 0, k == 7, [sb_, b_xT], [bb])
                    pcs.append((pb, bb))
                (pcb, bcb), (pcc, bcc), (pch, bch) = pcs
                u, bu = u_t[c % 2], b_u[c % 2]
                CP_("act", cc_sb, pcc, [bcc], [b_cc])
                CP_("dve", u[:, 0:2], halo[:, c, :], [b_halo[c]], [bu])
                TT_("dve", u[:, 2:TT + 2], cc_sb, pch, ALU.mult, [b_cc, bch], [bu])
                CP_("dve", halo[:, c, :], u[:, TT:TT + 2], [bu], [b_halo[c]])
                TS_("dve", acc, u[:, 2:TT + 2], convw[:, 16 + c:17 + c], None, ALU.mult, None, [bu, b_convw], [b_acc])
                STT_("dve", acc, u[:, 1:TT + 1], convw[:, 8 + c:9 + c], acc, ALU.mult, ALU.add, [bu, b_convw, b_acc], [b_acc])
                STT_("dve", acc, u[:, 0:TT], convw[:, c:c + 1], acc, ALU.mult, ALU.add, [bu, b_convw, b_acc], [b_acc])
                TT_("dve", y_conv[:, c, :], acc, pcb, ALU.mult, [b_acc, bcb], [b_yconv])

            slabB = [get_slab(), get_slab()]
            for g in range(8):
                sl, sb_ = slabB[g // 4]
                gg = g % 4
                pu, bpu = bank()
                for k in range(8):
                    MM_(pu, sl[:, k, gg * P:(gg + 1) * P], xT_bf[:, k, :], k == 0, k == 7, [sb_, b_xT], [bpu])
                ACT_(ug[g % 2], pu, AF.Gelu_apprx_tanh, [bpu], [b_ug[g % 2]])
                pf, bpf = bank()
                for j in range(4):
                    MM_(pf[:, j * P:(j + 1) * P], v_n[:, j, g * P:(g + 1) * P], WcT[:, g, :], True, True, [b_vnmg, b_WcT], [bpf])
                TT_("dve", ftmp[g % 2].rearrange("p (j t) -> p j t", j=4), pf.rearrange("p (j t) -> p j t", j=4),
                    biasbc[:, g, :].unsqueeze(1).broadcast_to([P, 4, P]), ALU.add, [bpf, b_biasbc], [b_ftmp[g % 2]])
                TT_("dve", y_gmlp[:, g, :], ftmp[g % 2], ug[g % 2], ALU.mult, [b_ftmp[g % 2], b_ug[g % 2]], [b_ygmlp])

            slabD = [get_slab(), get_slab()]
            for h in range(4):
                sl, sb_ = slabD[h // 2]
                q, bq = qT[h % 2], b_qT[h % 2]
                for dc in range(2):
                    cc = (h % 2) * 2 + dc
                    pb, bb = bank()
                    for k in range(8):
                        MM_(pb, sl[:, k, cc * P:(cc + 1) * P], xT_bf[:, k, :], k == 0, k == 7, [sb_, b_xT], [bb])
                    CP_("act", q[:, dc, :], pb, [bb], [bq])
                pt, bpt = PT[h % 2], b_PT[h % 2]
                for mc in range(2):
                    pb, bb = bank()
                    for dc in range(2):
                        MM_(pb, KT[:, h * 2 + dc, mc * P:(mc + 1) * P], q[:, dc, :], dc == 0, dc == 1, [b_KT, bq], [bb])
                    ACT_(pt[:, mc, :], pb, AF.Exp, [bb], [bpt], scale=0.0625)
                pden, bden = bank()
                for mc in range(2):
                    MM_(pden, onesb, pt[:, mc, :], mc == 0, mc == 1, [b_onesb, bpt], [bden])
                rd, brd = rden[h % 2], b_rden[h % 2]
                S.op("dve", lambda e, rd=rd, pden=pden: e.reciprocal(out=rd, in_=pden), [bden], [brd])
                for dc in range(2):
                    po, bpo = bank()
                    for mc in range(2):
                        MM_(po, Vt[:, mc, (h * 2 + dc) * P:(h * 2 + dc + 1) * P], pt[:, mc, :], mc == 0, mc == 1, [b_Vt, bpt], [bpo])
                    TT_("dve", y_xa[:, h * 2 + dc, :], rd, po, ALU.mult, [brd, bpo], [b_yxa])

            ys = [(y_conv, b_yconv), (y_gmlp, b_ygmlp), (y_xa, b_yxa)]
            for dc in range(8):
                slg, sbg = get_slab()
                slp, sbp = get_slab()
                sg, bsg = sig[dc % 2], b_sig[dc % 2]
                for r in range(3):
                    pb, bb = bank()
                    for k in range(8):
                        MM_(pb, slg[:, k, r * P:(r + 1) * P], xT_bf[:, k, :], k == 0, k == 7, [sbg, b_xT], [bb])
                    ACT_(sg[:, r, :], pb, AF.Sigmoid, [bb, b_bgate], [bsg], bias=bgate[:, r * 8 + dc:r * 8 + dc + 1])
                for r in range(3):
                    pb, bb = bank()
                    yr, byr = ys[r]
                    for k in range(8):
                        MM_(pb, slp[:, k, r * P:(r + 1) * P], yr[:, k, :], k == 0, k == 7, [sbp, byr], [bb])
                    TT_("dve", mt[:, r, :], sg[:, r, :], pb, ALU.mult, [bsg, bb], [b_mt])
                TT_("dve", mt[:, 0, :], mt[:, 0, :], mt[:, 1, :], ALU.add, [b_mt], [b_mt])
                TT_("dve", merged[:, dc, :], mt[:, 0, :], mt[:, 2, :], ALU.add, [b_mt], [b_vnmg])

            if ti + 1 < n_tiles:
                DMA_("pool", xT_bf, xT_d[:, t0 + TT:t0 + 2 * TT].rearrange("(k p) t -> p k t", p=P), (), [b_xT])

            slabF = [get_slab(), get_slab()]

            wbanks = {}

            def stage1_mm(j):
                ci = ti * 4 + j
                xcj, bxc = xc[j % 2], b_xc[j % 2]
                DMA_("sp", xcj, x_d[ci * P:(ci + 1) * P, :], (), [bxc])
                wbanks[j] = []
                for h in range(2):
                    pb, bb = bank()
                    sl, sb_ = slabF[h]
                    for k in range(8):
                        MM_(pb, merged[:, k, j * P:(j + 1) * P], sl[:, k, :], k == 0, k == 7, [b_vnmg, sb_], [bb])
                    wbanks[j].append((pb, bb))

            def stage1_ew(j):
                ci = ti * 4 + j
                r_t, b_r = r2[j % 2], b_r2[j % 2]
                xcj, bxc = xc[j % 2], b_xc[j % 2]
                for h in range(2):
                    pb, bb = wbanks[j][h]
                    STT_("dve", r_t[:, h * 512:(h + 1) * 512], xcj[:, h * 512:(h + 1) * 512], ALPHA, pb, ALU.mult, ALU.add,
                         [bxc, bb], [b_r])
                layer_norm(r_t, b_r, l1_g, l1_b, b_l1, r_t, b_r, lnt, b_lnt)
                DMA_("sp", x1_scr[ci * P:(ci + 1) * P, :], r_t, [b_r], [b_x1scr[ci]])
                xb, bxb = x1bf[j % 2], b_x1bf[j % 2]
                CP_("act", xb, r_t, [b_r], [bxb])

            def stage2(j):
                ci = ti * 4 + j
                r_t, b_r = r2[j % 2], b_r2[j % 2]
                xb, bxb = x1bf[j % 2], b_x1bf[j % 2]
                for hh in range(2):
                    pb, bb = bank()
                    for kk in range(4):
                        k = hh * 4 + kk
                        TR_(pb[:, kk * P:(kk + 1) * P], r_t[:, k * P:(k + 1) * P], identf, [b_r, b_identf], [bb])
                    CP_("act", x1T[:, hh * 4:(hh + 1) * 4, :], pb.rearrange("p (a b) -> p a b", a=4), [bb], [b_x1T])
                pl, bpl = bank()
                for k in range(8):
                    MM_(pl[:, 0:NE], x1T[:, k, :], wr_sb[:, k, :], k == 0, k == 7, [b_x1T, b_wr], [bpl])
                TT_("dve", lgt, pl[:, 0:NE], br_bc, ALU.add, [bpl, b_br], [b_rt])
                S.op("dve", lambda e: e.max(out=mx8, in_=lgt), [b_rt], [b_rt])
                TS_("dve", Mbf, lgt, mx8[:, 3:4], None, ALU.is_ge, None, [b_rt], [b_mbf])
                S.op("dve", lambda e: e.max_index(out=mi8, in_max=mx8, in_values=lgt), [b_rt], [b_rt])
                TS_("dve", negmax, mx8[:, 0:1], -1.0, None, ALU.mult, None, [b_rt], [b_rt])
                MS_("dve", ssum, 0.0, [b_rt])
                MS_("dve", pos4, 0.0, [b_rt])
                ACT_(ex4, mx8[:, 0:4], AF.Exp, [b_rt], [b_rt], bias=negmax, accum=ssum)
                S.op("dve", lambda e: e.reciprocal(out=ssum, in_=ssum), [b_rt], [b_rt])
                TS_("dve", gate_all[:, ci, :], ex4, ssum, None, ALU.mult, None, [b_rt], [b_gate_all[ci]])
                pp, bpp = bank()
                MM_(pp[:, 0:NE], ustrb, Mbf, True, True, [b_ustr, b_mbf], [bpp])
                MM_(pp[:, NE:2 * NE], onesb, Mbf, True, True, [b_onesb, b_mbf], [bpp])
                TT_("dve", pos, pp[:, 0:NE], tot_bc, ALU.add, [bpp, b_tot], [b_rt])
                TT_("dve", tot_bc, pp[:, NE:2 * NE], tot_bc, ALU.add, [bpp, b_tot], [b_tot])
                CP_("dve", ef4, mi8[:, 0:4], [b_rt], [b_rt])
                for k in range(4):
                    STT_("dve", junk, iota32, ef4[:, k:k + 1], pos, ALU.is_equal, ALU.mult, [b_iota, b_rt], [b_rt],
                         accum=pos4[:, k:k + 1])
                STT_("dve", dstf, ef4, float(CAP), pos4, ALU.mult, ALU.add, [b_rt], [b_rt])
                TS_("dve", keep, pos4, float(CAP), None, ALU.is_lt, None, [b_rt], [b_rt])
                TS_("dve", dstf, dstf, -float(ZROW), None, ALU.add, None, [b_rt], [b_rt])
                TT_("dve", dstf, dstf, keep, ALU.mult, [b_rt], [b_rt])
                TS_("dve", dstf, dstf, float(ZROW), None, ALU.add, None, [b_rt], [b_rt])
                CP_("dve", dst_all[:, ci, :], dstf, [b_rt], [b_dst_all[ci]])
                for k in range(4):
                    def scat(e, ci=ci, k=k, xb=xb):
                        if "r" not in bc_reg:
                            bc_reg["r"] = e.to_reg(NROWS - 1)
                        return e.indirect_dma_start(
                            out=xs_scr, out_offset=bass.IndirectOffsetOnAxis(ap=dst_all[:, ci, k:k + 1], axis=0),
                            in_=xb, in_offset=None, bounds_check=bc_reg["r"], oob_is_err=False)
                    S.op("pool", scat, [bxb, b_dst_all[ci]], [b_xs], dma=True)

            stage1_mm(0)
            stage1_ew(0)
            for j in range(4):
                if j + 1 < 4:
                    stage1_mm(j + 1)
                stage2(j)
                if j + 1 < 4:
                    stage1_ew(j + 1)

        S.barrier()

        top[0] = persist_top
        DMA_("sp", lnC_g, l2g_d.partition_broadcast(P), (), [b_lnC])
        DMA_("sp", lnC_b, l2b_d.partition_broadcast(P), (), [b_lnC])
        wgu = [alloc(8192, BF16, (8, 2 * D)) for _ in range(2)]
        b_wgu = [[Buf() for _ in range(8)] for _ in range(2)]
        wdn = [alloc(4096, BF16, (8, D)) for _ in range(2)]
        b_wdn = [[Buf() for _ in range(8)] for _ in range(2)]
        xs_sm = [alloc(512, BF16) for _ in range(NST)]; b_xssm = [Buf() for _ in range(NST)]
        xsT2 = [alloc(4 * CAP, BF16, (8, CAP)) for _ in range(2)]; b_xsT2 = [[Buf() for _ in range(NST)] for _ in range(2)]
        actT = alloc(4 * CAP, BF16, (8, CAP)); b_actT = Buf()
        g_sb = [alloc(512) for _ in range(2)]; b_g = [Buf() for _ in range(2)]
        s_sb = [alloc(512) for _ in range(2)]; b_s = [Buf() for _ in range(2)]
        u_sb = [alloc(512) for _ in range(2)]; b_us = [Buf() for _ in range(2)]
        eo_sb = [alloc(D) for _ in range(2)]; b_eosb = [Buf() for _ in range(2)]
        bdbc = [alloc(D) for _ in range(2)]; b_bdbc = [Buf() for _ in range(2)]
        bgu1 = alloc(NE * 16); b_bgu1 = Buf()
        TS_("dve", bgu1, bgu_sb, 1.0, None, ALU.add, None, [b_bgu], [b_bgu1])
        bankbf2 = [banks[6][:, :].bitcast(BF16), banks[7][:, :].bitcast(BF16)]

        def load_expert(e):
            bi = e % 2
            for k in range(8):
                DMA_("pool", wgu[bi][:, k, :], wgu_d[e, k * P:(k + 1) * P, :], (), [b_wgu[bi][k]])
            for k in range(8):
                DMA_("pool", wdn[bi][:, k, :], wd_d[e, k * P:(k + 1) * P, :], (), [b_wdn[bi][k]])
            DMA_("sp", bdbc[bi], bd_d[e:e + 1, :].partition_broadcast(P), (), [b_bdbc[bi]])

        def prefetch_xs(e):
            for s in range(NST):
                DMA_("sp", xs_sm[s], xs_scr[e * CAP + s * P:e * CAP + (s + 1) * P, :], [b_xs], [b_xssm[s]])

        def transposes(e):
            xsT, b_xsT = xsT2[e % 2], b_xsT2[e % 2]
            for s in range(NST):
                xm, bxm = xs_sm[s], b_xssm[s]
                bankbf, bbf = bankbf2[s % 2], bank_bufs[6 + s % 2]
                for k in range(8):
                    TR_(bankbf[:, k * P:(k + 1) * P], xm[:, k * P:(k + 1) * P], identb, [bxm, b_identb], [bbf])
                CP_("act", xsT[:, :, s * P:(s + 1) * P], bankbf.rearrange("p (a b) -> p a b", a=8), [bbf], [b_xsT[s]])

        splits = [(0, 512), (512, CAP)]
        split_tiles = [[s for s in range(NST) if n0 <= s * P < n1] for (n0, n1) in splits]
        if n_exp > 0:
            load_expert(0)
            prefetch_xs(0)
            transposes(0)
        cnt = 0
        for e in range(n_exp):
            bi = e % 2
            if e + 1 < n_exp:
                load_expert(e + 1)
                prefetch_xs(e + 1)
            xsT, b_xsT = xsT2[bi], b_xsT2[bi]
            for (n0, n1), tiles in zip(splits, split_tiles):
                w = n1 - n0
                rb = [b_xsT[s] for s in tiles]
                for f in range(8):
                    pgs = []
                    for m in (f, 8 + f):
                        i = bank_rr[0] % 6
                        bank_rr[0] += 1
                        pb, bb = banks[i][:, :], bank_bufs[i]
                        for k in range(8):
                            MM_(pb[:, 0:w], wgu[bi][:, k, m * P:(m + 1) * P], xsT[:, k, n0:n1], k == 0, k == 7,
                                [b_wgu[bi][k]] + rb, [bb])
                        pgs.append((pb, bb))
                    (pg, bpg), (pu, bpu) = pgs
                    gi = cnt % 2
                    cnt += 1
                    gs, us, ss = g_sb[gi][:, 0:w], u_sb[gi][:, 0:w], s_sb[gi][:, 0:w]
                    TS_("dve", gs, pg[:, 0:w], bgu_sb[:, e * 16 + f:e * 16 + f + 1], 7.0, ALU.add, ALU.min, [bpg, b_bgu], [b_g[gi]])
                    ACT_(ss, gs, AF.Sigmoid, [b_g[gi]], [b_s[gi]], scale=1.702)
                    TS_("dve", us, pu[:, 0:w], bgu1[:, e * 16 + 8 + f:e * 16 + 9 + f], 8.0, ALU.add, ALU.min, [bpu, b_bgu1], [b_us[gi]])
                    TT_("dve", gs, gs, ss, ALU.mult, [b_g[gi], b_s[gi]], [b_g[gi]])
                    STT_("dve", actT[:, f, n0:n1], us, -6.0, gs, ALU.max, ALU.mult, [b_us[gi], b_g[gi]], [b_actT])
            if e + 1 < n_exp:
                transposes(e + 1)
            for s in range(NST):
                eo, beo = eo_sb[s % 2], b_eosb[s % 2]
                for h in range(2):
                    i = bank_rr[0] % 6
                    bank_rr[0] += 1
                    pb, bb = banks[i][:, :], bank_bufs[i]
                    for f in range(8):
                        MM_(pb, actT[:, f, s * P:(s + 1) * P], wdn[bi][:, f, h * 512:(h + 1) * 512], f == 0, f == 7,
                            [b_actT, b_wdn[bi][f]], [bb])
                    TT_("dve", eo[:, h * 512:(h + 1) * 512], pb, bdbc[bi][:, h * 512:(h + 1) * 512], ALU.add, [bb, b_bdbc[bi]], [beo])
                DMA_("sp", eo_scr[e * CAP + s * P:e * CAP + (s + 1) * P, :], eo, [beo], [b_eo])

        S.barrier()

        top[0] = persist_top
        NB4 = 3
        eo4 = [alloc(4 * D, F32, (4, D)) for _ in range(NB4)]; b_eo4 = [Buf() for _ in range(NB4)]
        x1c = [alloc(D) for _ in range(NB4)]; b_x1c = [Buf() for _ in range(NB4)]
        accd = [alloc(D) for _ in range(2)]; b_accd = [Buf() for _ in range(2)]
        lnt2 = (alloc(12, F32, (2, 6)), alloc(2), alloc(1)); b_lnt2 = Buf()
        n_ch = n_tiles * 4

        def fetch(ci):
            e4, be4 = eo4[ci % NB4], b_eo4[ci % NB4]
            for k in range(4):
                S.op("pool", lambda e, ci=ci, k=k, e4=e4: e.indirect_dma_start(
                    out=e4[:, k, :], out_offset=None, in_=eo_scr,
                    in_offset=bass.IndirectOffsetOnAxis(ap=dst_all[:, ci, k:k + 1], axis=0)),
                    [b_eo, b_dst_all[ci]], [be4], dma=True)
            DMA_("sp", x1c[ci % NB4], x1_scr[ci * P:(ci + 1) * P, :], [b_x1scr[ci]], [b_x1c[ci % NB4]])

        for ci in range(min(NB4 - 1, n_ch)):
            fetch(ci)
        for ci in range(n_ch):
            if ci + NB4 - 1 < n_ch:
                fetch(ci + NB4 - 1)
            e4, be4 = eo4[ci % NB4], b_eo4[ci % NB4]
            xx, bxx = x1c[ci % NB4], b_x1c[ci % NB4]
            a, ba = accd[ci % 2], b_accd[ci % 2]
            TS_("dve", a, e4[:, 0, :], gate_all[:, ci, 0:1], None, ALU.mult, None, [be4, b_gate_all[ci]], [ba])
            for k in range(1, 4):
                STT_("dve", a, e4[:, k, :], gate_all[:, ci, k:k + 1], a, ALU.mult, ALU.add, [be4, b_gate_all[ci], ba], [ba])
            STT_("dve", a, xx, ALPHA, a, ALU.mult, ALU.add, [bxx, ba], [ba])
            layer_norm(a, ba, lnC_g, lnC_b, b_lnC, a, ba, lnt2, b_lnt2, aff="dve")
            out_dmas.append(DMA_("sp", out_d[ci * P:(ci + 1) * P, :], a, [ba], ()))

        S.emit_all(final_waits=[("sp", d) for d in out_dmas])
    return nc


def _host_layout(inp, b):
    f = np.float32
    w_in = inp["w_in"][0]
    cb, cc, ch, gu, gv, q, gates = np.split(w_in, np.cumsum([1024, 1024, 1024, 1024, 1024, 1024])[:], axis=1)
    segs = [inp["w_kv"][0], gv, gu]
    for c in range(8):
        sl = slice(c * 128, (c + 1) * 128)
        segs += [cb[:, sl], cc[:, sl], ch[:, sl]]
    segs.append(q)
    wcp, wgp, wxp = inp["w_conv_proj"][0], inp["w_gmlp_proj"][0], inp["w_xa_proj"][0]
    for dc in range(8):
        sl = slice(dc * 128, (dc + 1) * 128)
        segs += [gates[:, 0:1024][:, sl], gates[:, 1024:2048][:, sl], gates[:, 2048:3072][:, sl], wcp[:, sl], wgp[:, sl], wxp[:, sl]]
    segs.append(inp["w_out"][0])
    wmix = np.ascontiguousarray(np.concatenate(segs, axis=1), dtype=f)
    assert wmix.shape == (D, WCOLS)
    shared = {
        "wmix": wmix,
        "b_gate_T": np.ascontiguousarray(inp["b_gate"][0].reshape(24, 128).T, dtype=f),
        "conv_w_T": np.ascontiguousarray(inp["conv_w"][0].reshape(3, 8, 128).transpose(2, 0, 1).reshape(128, 24), dtype=f),
        "gmlp_wsT": np.ascontiguousarray(inp["gmlp_ws"][0].transpose(2, 0, 1).reshape(128, 8 * 128), dtype=f),
        "gmlp_b": np.ascontiguousarray(inp["gmlp_b"][0].reshape(1, D), dtype=f),
        "gmlp_ln_g": np.ascontiguousarray(inp["gmlp_ln_g"], dtype=f), "gmlp_ln_b": np.ascontiguousarray(inp["gmlp_ln_b"], dtype=f),
        "mem_ln_g": np.ascontiguousarray(inp["mem_ln_g"], dtype=f), "mem_ln_b": np.ascontiguousarray(inp["mem_ln_b"], dtype=f),
        "ln1_g": np.ascontiguousarray(inp["ln1_g"], dtype=f), "ln1_b": np.ascontiguousarray(inp["ln1_b"], dtype=f),
        "ln2_g": np.ascontiguousarray(inp["ln2_g"], dtype=f), "ln2_b": np.ascontiguousarray(inp["ln2_b"], dtype=f),
        "w_router": np.ascontiguousarray(inp["w_router"][0], dtype=f),
        "b_router": np.ascontiguousarray(inp["b_router"], dtype=f),
        "w_gate_up": np.ascontiguousarray(inp["w_gate_up"][0], dtype=f),
        "b_gate_up_T": np.ascontiguousarray(inp["b_gate_up"][0].reshape(32, 16, 128).transpose(2, 0, 1).reshape(128, 512), dtype=f),
        "w_down": np.ascontiguousarray(inp["w_down"][0], dtype=f),
        "b_down": np.ascontiguousarray(inp["b_down"][0], dtype=f),
    }
    return shared


def _core_inputs(inp, shared, b):
    m = dict(shared)
    xb = np.ascontiguousarray(inp["x"][b], dtype=np.float32)
    m["x"] = xb
    m["xT"] = np.ascontiguousarray(xb.T)
    m["mem"] = np.ascontiguousarray(inp["mem"][b], dtype=np.float32)
    return m


def kernel(**inputs):
    inp = {k: np.asarray(v) for k, v in inputs.items()}
    nb = inp["x"].shape[0]
    shared = _host_layout(inp, 0)
    in_maps = [_core_inputs(inp, shared, b) for b in range(nb)]
    nc = build()
    res = run_bass_kernel_spmd(nc, in_maps, core_ids=list(range(nb)))
    out = np.stack([np.asarray(r["out"]) for r in res.results], axis=0)
    return out.astype(np.float32, copy=False)
```
